# Optimizing a Trainium2 kernel written in Bass

```python
import math
import jax
import jax.numpy as jnp
from jax import lax
import numpy as np

D_MODEL = 1024
BATCH = 4
SEQ = 8192
DEPTH = 2

EPS = 1e-6
M_HEADS = 4
M_DH = D_MODEL // 8
M_W = M_HEADS * M_DH
M_CHUNK = 128
CONV_K = 4
B_HEADS = 8
B_DH = D_MODEL // 16
B_W = B_HEADS * B_DH
MOBA_BLOCK = 256
MOBA_TOPK = 3
MOBA_QCHUNK = 64
C_HEADS = 8
C_DH = D_MODEL // 16
C_VDH = 2 * C_DH
C_W = C_HEADS * C_VDH
C_QBLOCK = 128
PEER_HEADS = 8
PEER_NKEYS = 128
PEER_N = PEER_NKEYS * PEER_NKEYS
PEER_DK = 256
PEER_TOPK = 16
PEER_TOK_CHUNK = 128

IN_EVEN = 4 * M_W + 2 * M_HEADS + 3 * B_W
MIX_EVEN = M_W + B_W
IN_ODD = 4 * C_HEADS * C_DH + C_HEADS * C_VDH
N_EVEN = (DEPTH + 1) // 2
N_ODD = DEPTH // 2

kernel_name = 'hybrid_mlstm_moba_diffattn_peer_block'


def rms_norm(x, g):
    xf = x.astype(jnp.float32)
    y = xf * lax.rsqrt(jnp.mean(xf * xf, axis=-1, keepdims=True) + EPS)
    return (y * g.astype(jnp.float32)).astype(x.dtype)


def alibi_slopes(n_heads):
    return jnp.asarray([2.0 ** (-8.0 * (h + 1) / n_heads) for h in range(n_heads)], dtype=jnp.float32)


def causal_depthwise_conv(x, w):
    ch = x.shape[-1]
    return lax.conv_general_dilated(x, w.astype(x.dtype)[:, None, :], window_strides=(1,),
                                    padding=[(CONV_K - 1, 0)],
                                    dimension_numbers=('NWC', 'WIO', 'NWC'),
                                    feature_group_count=ch)


def to_heads(t, n_heads):
    bn, s, _ = t.shape
    return t.reshape(bn, s, n_heads, -1).transpose(0, 2, 1, 3)


def from_heads(t):
    bn, h, s, dh = t.shape
    return t.transpose(0, 2, 1, 3).reshape(bn, s, h * dh)


def mlstm_chunkwise(q, k, v, i_pre, f_pre):
    f32 = jnp.float32
    bn, nh, s, dh = q.shape
    L = M_CHUNK
    nc = s // L
    q = q.astype(f32).reshape(bn, nh, nc, L, dh)
    k = (k.astype(f32) * dh ** -0.5).reshape(bn, nh, nc, L, dh)
    v = v.astype(f32).reshape(bn, nh, nc, L, dh)
    ig = i_pre.astype(f32).reshape(bn, nh, nc, L)
    b = jnp.cumsum(jax.nn.log_sigmoid(f_pre.astype(f32)).reshape(bn, nh, nc, L), axis=-1)
    b_end = b[..., -1]
    w_end = b_end[..., None] - b + ig
    m_loc = jnp.max(w_end, axis=-1)
    e_end = jnp.exp(w_end - m_loc[..., None])
    c_loc = jnp.einsum('bhcl,bhcld,bhcle->bhcde', e_end, k, v)
    n_loc = jnp.einsum('bhcl,bhcld->bhcd', e_end, k)

    def step(carry, inp):
        c_st, n_st, m_st = carry
        be, cl, nl, ml = inp
        m_new = jnp.maximum(be + m_st, ml)
        a = jnp.exp(be + m_st - m_new)
        sc = jnp.exp(ml - m_new)
        c_new = a[..., None, None] * c_st + sc[..., None, None] * cl
        n_new = a[..., None] * n_st + sc[..., None] * nl
        return (c_new, n_new, m_new), (c_st, n_st, m_st)

    init = (jnp.zeros((bn, nh, dh, dh), f32), jnp.zeros((bn, nh, dh), f32), jnp.zeros((bn, nh), f32))
    xs = tuple(jnp.moveaxis(t, 2, 0) for t in (b_end, c_loc, n_loc, m_loc))
    _, (c_prev, n_prev, m_prev) = lax.scan(step, init, xs)
    c_prev = jnp.moveaxis(c_prev, 0, 2)
    n_prev = jnp.moveaxis(n_prev, 0, 2)
    m_prev = jnp.moveaxis(m_prev, 0, 2)
    causal = jnp.tril(jnp.ones((L, L), dtype=bool))
    log_d = jnp.where(causal, b[..., :, None] - b[..., None, :] + ig[..., None, :], -jnp.inf)
    a_inter = b + m_prev[..., None]
    m_t = jnp.maximum(a_inter, jnp.max(log_d, axis=-1))
    w_inter = jnp.exp(a_inter - m_t)
    s_intra = jnp.einsum('bhcld,bhcsd->bhcls', q, k) * jnp.exp(log_d - m_t[..., None])
    num = (w_inter[..., None] * jnp.einsum('bhcld,bhcde->bhcle', q, c_prev)
           + jnp.einsum('bhcls,bhcse->bhcle', s_intra, v))
    den = w_inter * jnp.einsum('bhcld,bhcd->bhcl', q, n_prev) + jnp.sum(s_intra, axis=-1)
    h = num / jnp.maximum(jnp.abs(den), jnp.exp(-m_t))[..., None]
    return h.reshape(bn, nh, s, dh)


def moba_attention(q, k, v, slopes):
    f32 = jnp.float32
    bn, nh, s, dh = q.shape
    bs = MOBA_BLOCK
    nb = -(-s // bs)
    pad = nb * bs - s
    scale = dh ** -0.5
    q = q.astype(f32)
    k_blk = jnp.pad(k.astype(f32), ((0, 0), (0, 0), (0, pad), (0, 0))).reshape(bn, nh, nb, bs, dh)
    v_blk = jnp.pad(v.astype(f32), ((0, 0), (0, 0), (0, pad), (0, 0))).reshape(bn, nh, nb, bs, dh)
    k_sel_n = min(MOBA_TOPK, nb - 1)
    if k_sel_n > 0:
        gate = jnp.einsum('bhsd,bhnd->bhsn', q, jnp.mean(k_blk, axis=3))
        fully_past = jnp.arange(nb)[None, :] < (jnp.arange(s) // bs)[:, None]
        gate = jnp.where(fully_past, gate, -jnp.inf)
        gate_val, sel = lax.top_k(gate, k_sel_n)
        sel_ok = jnp.isfinite(gate_val)
    b_idx = jnp.arange(bn)[:, None, None, None]
    h_idx = jnp.arange(nh)[None, :, None, None]
    qc_n = MOBA_QCHUNK

    def one_chunk(ci):
        t0 = ci * qc_n
        qc = lax.dynamic_slice_in_dim(q, t0, qc_n, axis=2)
        tq = t0 + jnp.arange(qc_n)
        j = t0 // bs
        k_own = lax.dynamic_index_in_dim(k_blk, j, axis=2, keepdims=False)
        v_own = lax.dynamic_index_in_dim(v_blk, j, axis=2, keepdims=False)
        dist_own = (tq[:, None] - (j * bs + jnp.arange(bs))[None, :]).astype(f32)
        logit_own = jnp.einsum('bhqd,bhkd->bhqk', qc, k_own) * scale - slopes[:, None, None] * dist_own
        logit_own = jnp.where(dist_own >= 0, logit_own, -jnp.inf)
        if k_sel_n == 0:
            p = jax.nn.softmax(logit_own, axis=-1)
            return jnp.einsum('bhqk,bhkd->bhqd', p, v_own)
        sc = lax.dynamic_slice_in_dim(sel, t0, qc_n, axis=2)
        ok = lax.dynamic_slice_in_dim(sel_ok, t0, qc_n, axis=2)
        k_g = k_blk[b_idx, h_idx, sc]
        v_g = v_blk[b_idx, h_idx, sc]
        dist_g = (tq[:, None, None] - (sc[..., None] * bs + jnp.arange(bs))).astype(f32)
        logit_g = jnp.einsum('bhqd,bhqnkd->bhqnk', qc, k_g) * scale - slopes[:, None, None, None] * dist_g
        logit_g = jnp.where(ok[..., None], logit_g, -jnp.inf)
        logits = jnp.concatenate([logit_g.reshape(bn, nh, qc_n, k_sel_n * bs), logit_own], axis=-1)
        p = jax.nn.softmax(logits, axis=-1)
        p_g = p[..., :k_sel_n * bs].reshape(bn, nh, qc_n, k_sel_n, bs)
        p_own = p[..., k_sel_n * bs:]
        return jnp.einsum('bhqnk,bhqnkd->bhqd', p_g, v_g) + jnp.einsum('bhqk,bhkd->bhqd', p_own, v_own)

    out = lax.map(one_chunk, jnp.arange(s // qc_n))
    return out.transpose(1, 2, 0, 3, 4).reshape(bn, nh, s, dh)


def diff_attention(q, k, v, lam, slopes):
    f32 = jnp.float32
    bn, nh, _, s, dh = q.shape
    scale = dh ** -0.5
    qf = q.astype(f32)
    kf = k.astype(f32)
    vf = v.astype(f32)
    kpos = jnp.arange(s)
    qb_n = C_QBLOCK

    def one_block(ci):
        t0 = ci * qb_n
        qb = lax.dynamic_slice_in_dim(qf, t0, qb_n, axis=3)
        dist = ((t0 + jnp.arange(qb_n))[:, None] - kpos[None, :]).astype(f32)
        logits = jnp.einsum('bhcqd,bhckd->bhcqk', qb, kf) * scale - slopes[:, None, None, None] * dist
        logits = jnp.where(dist >= 0, logits, -jnp.inf)
        p = jax.nn.softmax(logits, axis=-1)
        return jnp.einsum('bhqk,bhkd->bhqd', p[:, :, 0] - lam * p[:, :, 1], vf)

    out = lax.map(one_block, jnp.arange(s // qb_n))
    return out.transpose(1, 2, 0, 3, 4).reshape(bn, nh, s, v.shape[-1])


def mlstm_moba_mixer(h, w_in, conv_w, igate_b, fgate_b, mnorm_g, qn_g, kn_g, w_out):
    f32 = jnp.float32
    z = h @ w_in
    offs = np.cumsum([M_W, M_W, M_W, M_W, M_HEADS, M_HEADS, B_W, B_W]).tolist()
    mq, mk, mv, mo, mi, mf, bq, bk, bv = jnp.split(z, offs, axis=-1)
    qk = jax.nn.silu(causal_depthwise_conv(jnp.concatenate([mq, mk], axis=-1), conv_w))
    mq, mk = qk[..., :M_W], qk[..., M_W:]
    i_pre = (mi + igate_b).transpose(0, 2, 1)
    f_pre = (mf + fgate_b).transpose(0, 2, 1)
    hm = mlstm_chunkwise(to_heads(mq, M_HEADS), to_heads(mk, M_HEADS), to_heads(mv, M_HEADS), i_pre, f_pre)
    hm = rms_norm(hm, mnorm_g[:, None, :]) * jax.nn.sigmoid(to_heads(mo, M_HEADS).astype(f32))
    qb = rms_norm(to_heads(bq, B_HEADS), qn_g)
    kb = rms_norm(to_heads(bk, B_HEADS), kn_g)
    hb = moba_attention(qb, kb, to_heads(bv, B_HEADS), alibi_slopes(B_HEADS))
    y = jnp.concatenate([from_heads(hm), from_heads(hb)], axis=-1).astype(h.dtype)
    return y @ w_out


def diff_attention_mixer(h, w_in, qn_g, kn_g, lam_p, onorm_g, w_out, lam_init):
    f32 = jnp.float32
    bn, s, _ = h.shape
    z = h @ w_in
    qk_w = 2 * C_HEADS * C_DH
    q = z[..., :qk_w].reshape(bn, s, C_HEADS, 2, C_DH).transpose(0, 2, 3, 1, 4)
    k = z[..., qk_w:2 * qk_w].reshape(bn, s, C_HEADS, 2, C_DH).transpose(0, 2, 3, 1, 4)
    v = to_heads(z[..., 2 * qk_w:], C_HEADS)
    q = rms_norm(q, qn_g)
    k = rms_norm(k, kn_g)
    lp = lam_p.astype(f32)
    lam = jnp.exp(jnp.sum(lp[0] * lp[1])) - jnp.exp(jnp.sum(lp[2] * lp[3])) + lam_init
    o = diff_attention(q, k, v, lam, alibi_slopes(C_HEADS))
    o = rms_norm(o, onorm_g) * (1.0 - lam_init)
    return from_heads(o).astype(h.dtype) @ w_out


def peer_ffn(x, wq, keys, u, v):
    f32 = jnp.float32
    bn, s, d = x.shape
    t = bn * s
    xt = x.reshape(t, d)
    qry = (xt @ wq).astype(f32).reshape(t, PEER_HEADS, 2, PEER_DK // 2)
    sub = jnp.einsum('thpd,pnd->thpn', qry, keys.astype(f32))
    s_top, i_top = lax.top_k(sub, PEER_TOPK)
    cand_s = (s_top[:, :, 0, :, None] + s_top[:, :, 1, None, :]).reshape(t, PEER_HEADS, PEER_TOPK * PEER_TOPK)
    cand_i = (i_top[:, :, 0, :, None] * PEER_NKEYS + i_top[:, :, 1, None, :]).reshape(t, PEER_HEADS, PEER_TOPK * PEER_TOPK)
    score, pos = lax.top_k(cand_s, PEER_TOPK)
    idx = jnp.take_along_axis(cand_i, pos, axis=-1)
    gate = jax.nn.softmax(score, axis=-1)
    tc = PEER_TOK_CHUNK
    nt = t // tc

    def one_chunk(args):
        xb, ib, gb = args
        act = jax.nn.gelu(jnp.einsum('td,thkd->thk', xb, u[ib]).astype(f32), approximate=False) * gb
        return jnp.einsum('thk,thkd->td', act, v[ib].astype(f32))

    out = lax.map(one_chunk, (xt.reshape(nt, tc, d),
                              idx.reshape(nt, tc, PEER_HEADS, PEER_TOPK),
                              gate.reshape(nt, tc, PEER_HEADS, PEER_TOPK)))
    return out.reshape(bn, s, d).astype(x.dtype)


def setup_inputs(seed: int = 0) -> dict:
    key = jax.random.key(seed)
    ks = jax.random.split(key, 24)
    f32 = jnp.float32
    D = D_MODEL

    def nrm(k, shape, sc):
        return jax.random.normal(k, shape, f32) * sc

    return {
        'x': nrm(ks[0], (BATCH, SEQ, D), 1.0),
        'c': nrm(ks[1], (BATCH, D), 1.0),
        'ada_w': nrm(ks[2], (DEPTH, D, 6 * D), 0.5 * D ** -0.5),
        'ada_b': nrm(ks[3], (DEPTH, 6 * D), 0.01),
        'norm_mix_g': 1.0 + nrm(ks[4], (DEPTH, D), 0.02),
        'norm_ffn_g': 1.0 + nrm(ks[5], (DEPTH, D), 0.02),
        'ev_w_in': nrm(ks[6], (N_EVEN, D, IN_EVEN), D ** -0.5),
        'ev_conv_w': nrm(ks[7], (N_EVEN, CONV_K, 2 * M_W), CONV_K ** -0.5),
        'ev_igate_b': -1.0 + nrm(ks[8], (N_EVEN, M_HEADS), 0.1),
        'ev_fgate_b': jnp.linspace(3.0, 6.0, M_HEADS, dtype=f32) + nrm(ks[9], (N_EVEN, M_HEADS), 0.1),
        'ev_mnorm_g': 1.0 + nrm(ks[10], (N_EVEN, M_HEADS, M_DH), 0.02),
        'ev_qn_g': 1.0 + nrm(ks[11], (N_EVEN, B_DH), 0.02),
        'ev_kn_g': 1.0 + nrm(ks[12], (N_EVEN, B_DH), 0.02),
        'ev_w_out': nrm(ks[13], (N_EVEN, MIX_EVEN, D), MIX_EVEN ** -0.5),
        'od_w_in': nrm(ks[14], (N_ODD, D, IN_ODD), D ** -0.5),
        'od_qn_g': 1.0 + nrm(ks[15], (N_ODD, C_DH), 0.02),
        'od_kn_g': 1.0 + nrm(ks[16], (N_ODD, C_DH), 0.02),
        'od_lam': nrm(ks[17], (N_ODD, 4, C_DH), 0.1),
        'od_onorm_g': 1.0 + nrm(ks[18], (N_ODD, C_VDH), 0.02),
        'od_w_out': nrm(ks[19], (N_ODD, C_W, D), C_W ** -0.5),
        'peer_wq': nrm(ks[20], (DEPTH, D, PEER_HEADS * PEER_DK), D ** -0.5),
        'peer_keys': nrm(ks[21], (DEPTH, 2, PEER_NKEYS, PEER_DK // 2), (PEER_DK // 2) ** -0.5),
        'peer_u': nrm(ks[22], (DEPTH, PEER_N, D), D ** -0.5),
        'peer_v': nrm(ks[23], (DEPTH, PEER_N, D), PEER_HEADS ** -0.5),
    }


def reference(x, c, ada_w, ada_b, norm_mix_g, norm_ffn_g, ev_w_in, ev_conv_w, ev_igate_b, ev_fgate_b,
              ev_mnorm_g, ev_qn_g, ev_kn_g, ev_w_out, od_w_in, od_qn_g, od_kn_g, od_lam, od_onorm_g,
              od_w_out, peer_wq, peer_keys, peer_u, peer_v):
    c_act = jax.nn.silu(c.astype(jnp.float32))
    for layer in range(DEPTH):
        mod = (c_act @ ada_w[layer].astype(jnp.float32) + ada_b[layer].astype(jnp.float32)).astype(x.dtype)
        sh1, sc1, g1, sh2, sc2, g2 = [m[:, None, :] for m in jnp.split(mod, 6, axis=-1)]
        h = rms_norm(x, norm_mix_g[layer]) * (1.0 + sc1) + sh1
        if layer % 2 == 0:
            e = layer // 2
            y = mlstm_moba_mixer(h, ev_w_in[e], ev_conv_w[e], ev_igate_b[e], ev_fgate_b[e],
                                 ev_mnorm_g[e], ev_qn_g[e], ev_kn_g[e], ev_w_out[e])
        else:
            o = layer // 2
            lam_init = 0.8 - 0.6 * math.exp(-0.3 * layer)
            y = diff_attention_mixer(h, od_w_in[o], od_qn_g[o], od_kn_g[o], od_lam[o],
                                     od_onorm_g[o], od_w_out[o], lam_init)
        x = x + g1 * y.astype(x.dtype)
        h = rms_norm(x, norm_ffn_g[layer]) * (1.0 + sc2) + sh2
        x = x + g2 * peer_ffn(h, peer_wq[layer], peer_keys[layer], peer_u[layer], peer_v[layer])
    return x
```

```python
import numpy as np
from contextlib import ExitStack
import concourse.bass as bass
import concourse.mybir as mybir
from concourse.bass_utils import run_bass_kernel_spmd

F32 = mybir.dt.float32
BF16 = mybir.dt.bfloat16
AF = mybir.ActivationFunctionType
ALU = mybir.AluOpType
AX = mybir.AxisListType

D = 1024
EPS = 1e-6
NEG = -30000.0
import os
STAGE = int(os.environ.get('KSTAGE', '9'))
SUB = int(os.environ.get('KSUB', '0'))
KTENG = os.environ.get('KTENG', 'dve')


class Prog:
    def __init__(self, nc, stack, n_dma_sems=24, pfx="", sem_stack=None):
        self.nc = nc
        self.st = stack
        self.pfx = pfx
        sem_stack = sem_stack if sem_stack is not None else stack
        self.eng = {"pe": nc.tensor, "act": nc.scalar, "dve": nc.vector, "pool": nc.gpsimd, "sp": nc.sync}
        self.sem = {k: sem_stack.enter_context(nc.semaphore(pfx + "s_" + k)) for k in self.eng}
        self.cnt = {k: 0 for k in self.eng}
        self.dsem = [sem_stack.enter_context(nc.semaphore(pfx + "d%d" % i)) for i in range(n_dma_sems)]
        self.dcnt = [0] * n_dma_sems
        self.dnext = 0
        self.waited = {k: {} for k in self.eng}
        self.lastw = {}
        self.readers = {}
        self.ninst = 0
        self.nwait = 0

    def sb(self, name, shape, dt):
        return self.st.enter_context(self.nc.sbuf_tensor(self.pfx + name, shape, dt))

    def ps(self, name, shape, dt):
        return self.st.enter_context(self.nc.psum_tensor(self.pfx + name, shape, dt))

    def all_tokens(self):
        toks = [("E" + e, self.sem[e], self.cnt[e]) for e in self.eng if self.cnt[e] > 0]
        toks += [("D%d" % j, self.dsem[j], 16 * self.dcnt[j]) for j in range(len(self.dsem)) if self.dcnt[j] > 0]
        return toks

    def wait_all(self, toks, engines=None):
        for e in (engines or self.eng):
            for sid, sm, v in toks:
                self.eng[e].wait_ge(sm, v)
                self.nwait += 1

    def _wait(self, e, tok):
        if tok is None:
            return
        sid, s, v = tok
        if self.waited[e].get(sid, 0) >= v:
            return
        self.eng[e].wait_ge(s, v)
        self.nwait += 1
        self.waited[e][sid] = v

    def _deps(self, e, reads, writes):
        for k in reads:
            self._wait(e, self.lastw.get(k))
        for k in writes:
            self._wait(e, self.lastw.get(k))
            for t in self.readers.get(k, ()):
                self._wait(e, t)

    def _commit(self, tok, reads, writes):
        for k in writes:
            self.lastw[k] = tok
            self.readers[k] = []
        for k in reads:
            lst = self.readers.setdefault(k, [])
            lst.append(tok)
            if len(lst) > 16:
                best = {}
                for t in lst:
                    if t[0] not in best or best[t[0]][2] < t[2]:
                        best[t[0]] = t
                self.readers[k] = list(best.values())

    def op(self, e, ins_fn, reads=(), writes=()):
        self._deps(e, reads, writes)
        ins = ins_fn()
        self.cnt[e] += 1
        ins.then_inc(self.sem[e], 1)
        tok = ("E" + e, self.sem[e], self.cnt[e])
        self.waited[e]["E" + e] = self.cnt[e] - 1
        self._commit(tok, reads, writes)
        self.ninst += 1
        return tok

    def dma(self, q, out, in_, reads=(), writes=(), **kw):
        j = self.dnext
        self.dnext = (self.dnext + 1) % len(self.dsem)
        if self.dcnt[j] > 0:
            self._wait(q, ("D%d" % j, self.dsem[j], 16 * self.dcnt[j]))
        self._deps(q, reads, writes)
        ins = self.eng[q].dma_start(out=out, in_=in_, **kw)
        self.dcnt[j] += 1
        ins.then_inc(self.dsem[j], 16)
        tok = ("D%d" % j, self.dsem[j], 16 * self.dcnt[j])
        self.waited[q]["D%d" % j] = 16 * (self.dcnt[j] - 1)
        self._commit(tok, reads, writes)
        self.ninst += 1
        return tok

    def finish(self, keys, e="sp"):
        for k in keys:
            self._wait(e, self.lastw.get(k))

    def mm(self, out, lhsT, rhs, start, stop, reads, writes):
        nc = self.nc
        return self.op("pe", lambda: nc.tensor.matmul(out, lhsT=lhsT, rhs=rhs, start=start, stop=stop),
                       reads=reads, writes=writes)

    def tr(self, out, in_, ident, reads, writes):
        nc = self.nc
        return self.op("pe", lambda: nc.tensor.transpose(out=out, in_=in_, identity=ident), reads=reads, writes=writes)

    def act(self, out, in_, func, reads, writes, **kw):
        nc = self.nc
        return self.op("act", lambda: nc.scalar.activation(out=out, in_=in_, func=func, **kw), reads=reads, writes=writes)

    def tt(self, out, in0, in1, op, reads, writes, e="dve"):
        eng = self.eng[e]
        return self.op(e, lambda: eng.tensor_tensor(out=out, in0=in0, in1=in1, op=op), reads=reads, writes=writes)

    def ts(self, out, in0, s1, s2, op0, op1, reads, writes, e="dve", **kw):
        eng = self.eng[e]
        if op1 is None:
            return self.op(e, lambda: eng.tensor_scalar(out=out, in0=in0, scalar1=s1, scalar2=None, op0=op0, **kw),
                           reads=reads, writes=writes)
        return self.op(e, lambda: eng.tensor_scalar(out=out, in0=in0, scalar1=s1, scalar2=s2, op0=op0, op1=op1, **kw),
                       reads=reads, writes=writes)

    def stt(self, out, in0, scalar, in1, op0, op1, reads, writes):
        nc = self.nc
        return self.op("dve", lambda: nc.vector.scalar_tensor_tensor(out=out, in0=in0, scalar=scalar, in1=in1, op0=op0, op1=op1),
                       reads=reads, writes=writes)

    def copy(self, e, out, in_, reads, writes):
        nc = self.nc
        if e == "act":
            return self.op("act", lambda: nc.scalar.copy(out=out, in_=in_), reads=reads, writes=writes)
        eng = self.eng[e]
        return self.op(e, lambda: eng.tensor_copy(out=out, in_=in_), reads=reads, writes=writes)

    def memset(self, e, ap, val, writes):
        eng = self.eng[e]
        return self.op(e, lambda: eng.memset(ap, val), writes=writes)


def load_consts(P, ident_d):
    idf = P.sb("idf", [128, 128], F32)
    idb = P.sb("idb", [128, 128], BF16)
    P.dma("sp", idf[:], ident_d[:, :], writes=["idf"])
    P.dma("pool", idb[:], ident_d[:, :], writes=["idb"])
    return idf, idb


def compute_mod(P, c_d, adaw_d, adab_d, ncols, ps_bank, name):
    nc = P.nc
    CW = 256
    ccol = P.sb(name + "_ccol", [128, 8], F32)
    cact = P.sb(name + "_cact", [128, 8], F32)
    crep = P.sb(name + "_crep", [128, 8, 128], F32)
    mod = P.sb(name + "_mod", [128, ncols], F32)
    brep = P.sb(name + "_brep", [128, CW], F32)
    wch = P.sb(name + "_wch", [128, 8, CW], F32)
    P.dma("sp", ccol[:], c_d[:, :], writes=[name + "ccol"])
    P.act(cact[:], ccol[:], AF.Silu, reads=[name + "ccol"], writes=[name + "cact"])
    P.copy("dve", crep[:], cact[:].unsqueeze(2).broadcast_to([128, 8, 128]), reads=[name + "cact"], writes=[name + "crep"])
    for j in range(ncols // CW):
        P.dma("sp", wch[:], adaw_d[:, j * CW:(j + 1) * CW].rearrange("(c p) n -> p c n", p=128), writes=[name + "wch"])
        P.dma("sp", brep[:], adab_d[0:1, j * CW:(j + 1) * CW].partition_broadcast(128), writes=[name + "brep"])
        for c in range(8):
            P.mm(ps_bank[:, 0:CW], crep[:, c, :], wch[:, c, :], c == 0, c == 7,
                 reads=[name + "crep", name + "wch"], writes=[ps_bank.name])
        P.tt(mod[:, j * CW:(j + 1) * CW], ps_bank[:, 0:CW], brep[:], ALU.add,
             reads=[ps_bank.name, name + "brep"], writes=[name + "mod"])
    return mod


def rstd_from_ss(P, ss, rstd, n, key_in, key_out, width=1):
    nc = P.nc
    P.ts(rstd, ss, 1.0 / n, EPS, ALU.mult, ALU.add, reads=[key_in], writes=[key_out])
    P.act(rstd, rstd, AF.Sqrt, reads=[key_out], writes=[key_out])
    P.op("dve", lambda: nc.vector.reciprocal(out=rstd, in_=rstd), reads=[key_out], writes=[key_out])


def norm_mod_T(P, xt, xkey, GM, SH, gkeys, hb, hT, psT, idb, junk, small, tag, act_copy=True):
    nc = P.nc
    ss = small[:, 0:1]
    rstd = small[:, 1:2]
    P.act(junk[:], xt, AF.Square, reads=[xkey], writes=["junk" + tag, "ss" + tag], accum_out=ss)
    rstd_from_ss(P, ss, rstd, D, "ss" + tag, "rstd" + tag)
    P.stt(junk[:], xt, rstd, GM[:], ALU.mult, ALU.mult, reads=[xkey, "rstd" + tag] + gkeys, writes=["junk" + tag])
    P.tt(hb[:], junk[:], SH[:], ALU.add, reads=["junk" + tag] + gkeys, writes=["hb" + tag])
    for c in range(8):
        P.tr(psT[:, c * 128:(c + 1) * 128], hb[:, c * 128:(c + 1) * 128], idb[:], reads=["hb" + tag, "idb"], writes=[psT.name])
    P.copy("act" if act_copy else "dve", hT[:].rearrange("p c t -> p (c t)"), psT[:, :], reads=[psT.name], writes=["hT" + tag])


def build_mix1(S, nc=None, P=None, pfx="", over=None):
    over = over or {}
    standalone = nc is None
    NT = S // 128
    if standalone:
        nc = bass.Bass("TRN2", target_bir_lowering=False)
    dt = lambda n, s: over[n] if n in over else nc.dram_tensor(pfx + n, s, F32, kind="ExternalInput").ap()
    x_d = None if "xtile" in over else dt("x", [S, D])
    c_d = dt("c", [128, 8]); adaw_d = dt("adaw", [D, 2048]); adab_d = dt("adab", [1, 2048])
    ng_d = dt("ng", [1, D]); win_d = dt("win", [D, 4 * 384]); gqk_d = dt("gqk", [1, 256]); lam_d = dt("lam", [1, 256])
    ong_d = dt("ong", [1, 128]); ident_d = dt("ident", [128, 128]); negm_d = dt("negm", [128, 128])
    qab_d = dt("qab", [4, 4 * 128]); qad_d = dt("qad", [4, 4 * 128]); ka_d = dt("ka", [4, S])
    laminit_d = dt("laminit", [1, 2])
    y_d = None if "ytile" in over else nc.dram_tensor("y", [S, 512], F32, kind="ExternalOutput").ap()
    with ExitStack() as st:
        if P is None:
            P = Prog(nc, st)
        P.st = st
        idf, idb = load_consts(P, ident_d)
        banks = [P.ps("bk%d" % i, [128, 512], F32) for i in range(7)]
        psT = P.ps("psT", [128, 1024], BF16)
        mod = compute_mod(P, c_d, adaw_d, adab_d, 2048, banks[0], "m")
        GM = P.sb("GM", [128, D], F32)
        P.dma("sp", GM[:], ng_d[0:1, :].partition_broadcast(128), writes=["GM"])
        P.stt(GM[:], mod[:, 1024:2048], 1.0, GM[:], ALU.add, ALU.mult, reads=["mmod", "GM"], writes=["GM"])
        SH = mod[:, 0:1024]
        gk = ["GM", "mmod"]
        negb = P.sb("negb", [128, 128], BF16)
        P.dma("pool", negb[:], negm_d[:, :], writes=["negb"])
        Gqk = P.sb("Gqk", [128, 256], F32)
        P.dma("sp", Gqk[:], gqk_d[0:1, :].partition_broadcast(128), writes=["Gqk"])
        P.ts(Gqk[:, 0:128], Gqk[:, 0:128], 0.125, None, ALU.mult, None, reads=["Gqk"], writes=["Gqk"])
        ONG = P.sb("ONG", [128, 128], F32)
        P.dma("sp", ONG[:], ong_d[0:1, :].partition_broadcast(128), writes=["ONG"])
        lamt = P.sb("lamt_s", [128, 256], F32)
        lami = P.sb("lami", [128, 2], F32)
        lsm = P.sb("lsm", [128, 4], F32)
        P.dma("sp", lamt[:], lam_d[0:1, :].partition_broadcast(128), writes=["lamt"])
        P.dma("sp", lami[:], laminit_d[0:1, :].partition_broadcast(128), writes=["lami"])
        lv = lamt[:].rearrange("p (a d) -> p a d", a=4)
        P.tt(lamt[:, 0:64], lv[:, 0, :], lv[:, 1, :], ALU.mult, reads=["lamt"], writes=["lamt"])
        P.tt(lamt[:, 128:192], lv[:, 2, :], lv[:, 3, :], ALU.mult, reads=["lamt"], writes=["lamt"])
        P.op("dve", lambda: nc.vector.tensor_reduce(out=lsm[:, 0:1], in_=lamt[:, 0:64], axis=AX.X, op=ALU.add), reads=["lamt"], writes=["lsm"])
        P.op("dve", lambda: nc.vector.tensor_reduce(out=lsm[:, 1:2], in_=lamt[:, 128:192], axis=AX.X, op=ALU.add), reads=["lamt"], writes=["lsm"])
        P.act(lsm[:, 0:2], lsm[:, 0:2], AF.Exp, reads=["lsm"], writes=["lsm"])
        P.tt(lsm[:, 2:3], lsm[:, 1:2], lsm[:, 0:1], ALU.subtract, reads=["lsm"], writes=["lsm"])
        P.tt(lsm[:, 3:4], lsm[:, 2:3], lami[:, 0:1], ALU.subtract, reads=["lsm", "lami"], writes=["neglam"])
        neglam = lsm[:, 3:4]
        P.ts(ONG[:], ONG[:], lami[:, 1:2], None, ALU.mult, None, reads=["ONG", "lami"], writes=["ONG"])

        KT = P.sb("KT", [68, 2, S], BF16)
        QT = P.sb("QT", [68, 2, 128], BF16)
        qab = P.sb("qab_s", [68, 4, 128], F32)
        qad = P.sb("qad_s", [68, 4, 128], F32)
        P.dma("sp", qab[64:68, :, :], qab_d.rearrange("r (h t) -> r h t", h=4), writes=["qab"])
        P.dma("sp", qad[64:68, :, :], qad_d.rearrange("r (h t) -> r h t", h=4), writes=["qad"])
        for c in range(2):
            P.dma("pool", KT[64:68, c, :], ka_d[:, :], writes=["KTaug"])
        Vaug = P.sb("Vaug", [128, NT, 129], BF16)
        P.memset("pool", Vaug[:, :, 128:129], 1.0, writes=["Vones"])
        wsb = P.sb("wsb", [128, 8, 384], BF16)
        xts = [P.sb("xt%d" % i, [128, D], F32) for i in range(2)]
        junk = P.sb("junk", [128, D], F32)
        hb = P.sb("hb", [128, D], BF16)
        hT = P.sb("hT", [128, 8, 128], BF16)
        small = P.sb("small", [128, 16], F32)
        sq = P.sb("sq", [128, 256], F32)
        qkn = P.sb("qkn", [128, 256], BF16)
        PT = [[P.sb("PT%d%d" % (c, b), [128, 512], BF16) for b in range(2)] for c in range(2)]
        osb = P.sb("osb", [128, 128], F32)
        o2 = P.sb("o2", [128, 128], F32)
        yo = [P.sb("yo%d" % i, [128, 128], F32) for i in range(2)]
        psZ = banks[0]
        psS = [[banks[1], banks[2]], [banks[3], banks[4]]]
        psO = [banks[5], banks[6]]
        it = 0
        for j in range(4 if STAGE >= 1 else 0):
            P.dma("pool", wsb[:], win_d[:, j * 384:(j + 1) * 384].rearrange("(c p) n -> p c n", p=128),
                  writes=["wsb"])
            for i in range(NT):
                xt = xts[it % 2]; xk = "xt%d" % (it % 2)
                P.dma("sp", xt[:], over["xtile"](i) if "xtile" in over else x_d[i * 128:(i + 1) * 128, :], writes=[xk])
                norm_mod_T(P, xt[:], xk, GM, SH, gk, hb, hT, psT, idb, junk, small, "", act_copy=True)
                if STAGE < 2:
                    continue
                for c in range(8):
                    P.mm(psZ[:, 0:384], hT[:, c, :], wsb[:, c, :], c == 0, c == 7, reads=["hT", "wsb"], writes=[psZ.name])
                if STAGE < 3:
                    continue
                if SUB == 5:
                    continue
                P.act(sq[:], psZ[:, 0:256], AF.Square, reads=[psZ.name], writes=["sq"])
                P.op("dve", lambda: nc.vector.tensor_reduce(out=small[:, 4:8], in_=sq[:].rearrange("p (g d) -> p g d", g=4), axis=AX.X, op=ALU.add),
                     reads=["sq"], writes=["ss4"])
                rstd_from_ss(P, small[:, 4:8], small[:, 8:12], 64, "ss4", "rstd4")
                P.tt(sq[:].rearrange("p (g d) -> p g d", g=4), psZ[:, 0:256].rearrange("p (g d) -> p g d", g=4),
                     small[:, 8:12].unsqueeze(2).broadcast_to([128, 4, 64]), ALU.mult, reads=[psZ.name, "rstd4"], writes=["sq"])
                P.tt(qkn[:], sq[:], Gqk[:], ALU.mult, reads=["sq", "Gqk"], writes=["qkn"])
                if SUB == 2:
                    continue
                P.copy("act", Vaug[:, i, 0:128], psZ[:, 256:384], reads=[psZ.name], writes=["V%d" % i])
                if SUB == 3:
                    continue
                for g in range(4):
                    P.tr(psT[0:64, g * 128:(g + 1) * 128], qkn[:, g * 64:(g + 1) * 64], idb[:], reads=["qkn", "idb"], writes=[psT.name])
                if SUB == 4:
                    continue
                P.copy("dve", QT[0:64, :, :], psT[0:64, 0:256].rearrange("p (c t) -> p c t", c=2), reads=[psT.name], writes=["QT"])
                if SUB == 6:
                    continue
                for c in range(2):
                    P.copy(KTENG, KT[0:64, c, i * 128:(i + 1) * 128], psT[0:64, 256 + c * 128:256 + (c + 1) * 128],
                           reads=[psT.name], writes=["K%d" % i])
                if SUB == 7:
                    continue
                if SUB != 1:
                  P.stt(QT[64:68, :, :], qad[64:68, j:j + 1, :].broadcast_to([4, 2, 128]), float(i),
                        qab[64:68, j:j + 1, :].broadcast_to([4, 2, 128]), ALU.mult, ALU.add, reads=["qab", "qad"], writes=["QT"])
                if STAGE < 4:
                    continue
                ngrp = i // 4 + 1
                for g in range(ngrp):
                    kts = list(range(4 * g, min(4 * g + 4, i + 1)))
                    n = len(kts)
                    for c in range(2):
                        bank = psS[c][g % 2]
                        for jj, kt in enumerate(kts):
                            P.mm(bank[:, jj * 128:(jj + 1) * 128], KT[0:68, c, kt * 128:(kt + 1) * 128], QT[0:68, c, :],
                                 True, kt != i, reads=["K%d" % kt, "KTaug", "QT"], writes=[bank.name])
                            if kt == i:
                                P.mm(bank[:, jj * 128:(jj + 1) * 128], idb[:], negb[:], False, True,
                                     reads=["idb", "negb"], writes=[bank.name])
                        pt = PT[c][g % 2]; pk = "PT%d%d" % (c, g % 2)
                        P.act(pt[:, 0:n * 128], bank[:, 0:n * 128], AF.Exp, reads=[bank.name], writes=[pk])
                        for jj, kt in enumerate(kts):
                            P.mm(psO[c][:, 0:129], pt[:, jj * 128:(jj + 1) * 128], Vaug[:, kt, :], kt == 0, kt == i,
                                 reads=[pk, "V%d" % kt, "Vones"], writes=[psO[c].name])
                if STAGE < 5:
                    continue
                P.op("dve", lambda: nc.vector.reciprocal(out=small[:, 12:13], in_=psO[0][:, 128:129]), reads=[psO[0].name], writes=["rd0"])
                P.op("dve", lambda: nc.vector.reciprocal(out=small[:, 13:14], in_=psO[1][:, 128:129]), reads=[psO[1].name], writes=["rd1"])
                P.ts(o2[:], psO[1][:, 0:128], small[:, 13:14], neglam, ALU.mult, ALU.mult, reads=[psO[1].name, "rd1", "neglam"], writes=["o2"])
                P.stt(osb[:], psO[0][:, 0:128], small[:, 12:13], o2[:], ALU.mult, ALU.add, reads=[psO[0].name, "rd0", "o2"], writes=["osb"])
                P.act(o2[:], osb[:], AF.Square, reads=["osb"], writes=["o2", "sso"], accum_out=small[:, 14:15])
                rstd_from_ss(P, small[:, 14:15], small[:, 15:16], 128, "sso", "rstdo")
                yt = yo[it % 2]; yk = "yo%d" % (it % 2)
                P.stt(yt[:], osb[:], small[:, 15:16], ONG[:], ALU.mult, ALU.mult, reads=["osb", "rstdo", "ONG"], writes=[yk])
                ydst = over["ytile"](i)[:, j * 128:(j + 1) * 128] if "ytile" in over else y_d[i * 128:(i + 1) * 128, j * 128:(j + 1) * 128]
                P.dma("sp", ydst, yt[:], reads=[yk], writes=["yout"])
                it += 1
        P.finish(["yout"])
        for k, v in P.lastw.items():
            pass
        for jx in range(len(P.dsem) if standalone else 0):
            if P.dcnt[jx] > 0:
                P._wait("sp", ("D%d" % jx, P.dsem[jx], 16 * P.dcnt[jx]))
        print("mix1 ninst", P.ninst, "nwait", P.nwait)
    return nc


def mix1_consts(S):
    ident = np.eye(128, dtype=np.float32)
    k_idx = np.arange(128)[:, None]; q_idx = np.arange(128)[None, :]
    negm = np.where(k_idx > q_idx, NEG, 0.0).astype(np.float32)
    ka = np.zeros((4, S), np.float32)
    t = np.arange(S)
    ka[0] = 1.0; ka[1] = t % 128; ka[2] = t // 128; ka[3] = 1.0
    return ident, negm, ka


def alibi_q_rows(slopes):
    nh = len(slopes)
    qab = np.zeros((4, nh, 128), np.float32); qad = np.zeros((4, nh, 128), np.float32)
    for h, s in enumerate(slopes):
        qab[0, h] = -s * np.arange(128); qab[1, h] = s; qab[2, h] = 128.0 * s
        qad[3, h] = -128.0 * s
    return qab.reshape(4, nh * 128), qad.reshape(4, nh * 128)


def slopes8():
    return [2.0 ** (-8.0 * (h + 1) / 8) for h in range(8)]


def prep_mix1(inp, layer, x_b, c_b, hh, S):
    o = layer // 2
    w = inp["od_w_in"][o]
    cols = []
    for j in range(4):
        h = 4 * hh + j
        cols.append(w[:, h * 128:(h + 1) * 128])
        cols.append(w[:, 1024 + h * 128:1024 + (h + 1) * 128])
        cols.append(w[:, 2048 + h * 128:2048 + (h + 1) * 128])
    win = np.ascontiguousarray(np.concatenate(cols, axis=1))
    ident, negm, ka = mix1_consts(S)
    sl = slopes8()[4 * hh:4 * hh + 4]
    qab, qad = alibi_q_rows(sl)
    qn = inp["od_qn_g"][o]; kn = inp["od_kn_g"][o]
    gqk = np.concatenate([qn, qn, kn, kn])[None, :].astype(np.float32)
    import math
    lam_init = 0.8 - 0.6 * math.exp(-0.3 * layer)
    return {
        "x": None if x_b is None else np.ascontiguousarray(x_b), "c": np.ascontiguousarray(c_b.reshape(8, 128).T),
        "adaw": np.ascontiguousarray(inp["ada_w"][layer][:, 0:2048]), "adab": np.ascontiguousarray(inp["ada_b"][layer][None, 0:2048]),
        "ng": np.ascontiguousarray(inp["norm_mix_g"][layer][None, :]), "win": win, "gqk": gqk,
        "lam": np.ascontiguousarray(inp["od_lam"][o].reshape(1, 256)), "ong": np.ascontiguousarray(inp["od_onorm_g"][o][None, :]),
        "ident": ident, "negm": negm, "qab": qab, "qad": qad, "ka": ka,
        "laminit": np.array([[lam_init, 1.0 - lam_init]], np.float32),
    }


def build_ffn(NTOK, nc=None, P=None, pfx="", over=None):
    over = over or {}
    standalone = nc is None
    NST = NTOK // 512
    if standalone:
        nc = bass.Bass("TRN2", target_bir_lowering=False)
    dt = lambda n, s: over[n] if n in over else nc.dram_tensor(pfx + n, s, F32, kind="ExternalInput").ap()
    yg = over.get("yg"); SEQ = 2 * NTOK
    x_d = None if "xtile" in over else dt("x", [NTOK, D])
    c_d = dt("c", [128, 8])
    y_d = dt("y", [NTOK, D]) if yg is None else None
    hsel_d = dt("hsel", [128, 2]) if yg is not None else None
    adaw_d = dt("adaw", [D, 4096]); adab_d = dt("adab", [1, 4096]); ng_d = dt("ng", [1, D])
    wout_d = dt("wout", [D, D]); wq_d = dt("wq", [D, 2048]); keysT_d = dt("keysT", [128, 256])
    uT_d = dt("uT", [D, 16384]); v_d = dt("v", [16384, D]); ident_d = dt("ident", [128, 128])
    o_d = None if "otile" in over else nc.dram_tensor("o", [NTOK, D], F32, kind="ExternalOutput").ap()
    with ExitStack() as st:
        if P is None:
            P = Prog(nc, st)
        P.st = st
        idf, idb = load_consts(P, ident_d)
        banks = [P.ps("bk%d" % i, [128, 512], F32) for i in range(7)]
        psT = P.ps("psT", [128, 1024], BF16)
        mod = compute_mod(P, c_d, adaw_d, adab_d, 4096, banks[0], "m")
        GM = P.sb("GM", [128, D], F32)
        P.dma("sp", GM[:], ng_d[0:1, :].partition_broadcast(128), writes=["GM"])
        P.stt(GM[:], mod[:, 2048:3072], 1.0, GM[:], ALU.add, ALU.mult, reads=["mmod", "GM"], writes=["GM"])
        G1 = mod[:, 0:1024]; SH = mod[:, 1024:2048]; G2 = mod[:, 3072:4096]
        gk = ["GM", "mmod"]
        keysb = P.sb("keysb", [128, 256], BF16)
        P.dma("pool", keysb[:], keysT_d[:, :], writes=["keysb"])
        W = [P.sb("W%d" % i, [128, 8, 512], BF16) for i in range(3)]
        Vc = [P.sb("Vc%d" % i, [128, 4, 1024], BF16) for i in range(2)]
        wn = [0]; vn = [0]

        def loadW(src):
            i = wn[0] % 3; wn[0] += 1
            P.dma("pool", W[i][:], src.rearrange("(c p) n -> p c n", p=128), writes=["W%d" % i])
            return W[i], "W%d" % i

        xt = P.sb("xt", [128, D], F32)
        if yg is not None:
            ysf = P.sb("ysf", [128, 4, 512], F32)
            hsel = P.sb("hsel_s", [128, 2], F32)
            P.dma("sp", hsel[:], hsel_d[:, :], writes=["hsel"])
        yb = P.sb("yb", [128, D], BF16)
        yT = P.sb("yT", [128, 8, 128], BF16)
        junk = P.sb("junk", [128, D], F32)
        hb = P.sb("hb", [128, D], BF16)
        small = P.sb("small", [128, 8], F32)
        x1 = [P.sb("x1_%d" % t, [128, D], F32) for t in range(4)]
        h2T = [P.sb("h2T_%d" % t, [128, 8, 128], BF16) for t in range(4)]
        qTb = [P.sb("qTb_%d" % t, [128, 16, 128], BF16) for t in range(4)]
        sT = [P.sb("sT_%d" % t, [128, 16, 128], BF16) for t in range(4)]
        thr = [P.sb("thr_%d" % t, [128, 8], F32) for t in range(4)]
        nb = [P.sb("nb_%d" % t, [128, 8], F32) for t in range(4)]
        acc = [P.sb("acc_%d" % t, [128, D], F32) for t in range(4)]
        sbf = P.sb("sbf", [128, 16, 128], BF16)
        t16 = P.sb("t16", [128, 16, 16], F32)
        tmp128 = P.sb("tmp128", [128, 128], BF16)
        cand = P.sb("cand", [128, 8, 256], F32)
        cand2 = P.sb("cand2", [128, 256], F32)
        c16 = P.sb("c16", [128, 8, 16], F32)
        e16 = P.sb("e16", [128, 8, 16], F32)
        zs = P.sb("zs", [128, 8], F32)
        eb = [P.sb("eb%d" % i, [128, 512], BF16) for i in range(2)]
        wb = [P.sb("wbm%d" % i, [128, 512], BF16) for i in range(2)]
        gh = [P.sb("gh%d" % i, [128, 512], F32) for i in range(2)]
        Ab = [P.sb("Ab%d" % i, [128, 512], BF16) for i in range(2)]
        AT = [P.sb("AT%d" % i, [128, 4, 128], BF16) for i in range(2)]
        ot = P.sb("ot", [128, D], F32)
        psZ = [banks[0], banks[1]]; psG = banks[2]; psH = banks[3]; psO = [banks[4], banks[5]]; psX = banks[6]
        sel1 = lambda c: idb[:, 4 * c:4 * c + 4].unsqueeze(2).broadcast_to([128, 4, 128])
        sel2 = idb[:, :].unsqueeze(1).broadcast_to([128, 4, 128])
        cnt = [0]
        for stile in range(NST):
            t0 = stile * 512
            wo = [loadW(wout_d[:, hf * 512:(hf + 1) * 512]) for hf in range(2)]
            for t in range(4):
                r0 = t0 + t * 128
                if yg is None:
                    P.dma("pool", yb[:], y_d[r0:r0 + 128, :], writes=["yb"])
                else:
                    for r in range(2):
                        for q in range(2):
                            P.dma("sp", ysf[:, r * 2 + q, :], yg(r, q, r0), writes=["ysf"])
                    for r in range(2):
                        P.ts(junk[:, r * 512:(r + 1) * 512], ysf[:, r * 2, :], hsel[:, 0:1], None, ALU.mult, None,
                             reads=["ysf", "hsel"], writes=["junk"])
                        P.stt(yb[:, r * 512:(r + 1) * 512], ysf[:, r * 2 + 1, :], hsel[:, 1:2], junk[:, r * 512:(r + 1) * 512],
                              ALU.mult, ALU.add, reads=["ysf", "hsel", "junk"], writes=["yb"])
                P.dma("sp", xt[:], over["xtile"](r0) if "xtile" in over else x_d[r0:r0 + 128, :], writes=["xt"])
                for c in range(8):
                    P.tr(psT[:, c * 128:(c + 1) * 128], yb[:, c * 128:(c + 1) * 128], idb[:], reads=["yb", "idb"], writes=[psT.name])
                P.copy("act", yT[:].rearrange("p c t -> p (c t)"), psT[:, :], reads=[psT.name], writes=["yT"])
                for hf in range(2):
                    bk = psO[hf]
                    for c in range(8):
                        P.mm(bk[:, :], yT[:, c, :], wo[hf][0][:, c, :], c == 0, c == 7, reads=["yT", wo[hf][1]], writes=[bk.name])
                    P.tt(junk[:, hf * 512:(hf + 1) * 512], bk[:, :], G1[:, hf * 512:(hf + 1) * 512], ALU.mult,
                         reads=[bk.name, "mmod"], writes=["junk"])
                P.tt(x1[t][:], junk[:], xt[:], ALU.add, reads=["junk", "xt"], writes=["x1_%d" % t])
                norm_mod_T(P, x1[t][:], "x1_%d" % t, GM, SH, gk, hb, h2T[t], psT, idb, junk, small, "", act_copy=True)
                P.lastw["h2T_%d" % t] = P.lastw["hT"]
            for g in range(4):
                wq, wqk = loadW(wq_d[:, g * 512:(g + 1) * 512])
                for t in range(4):
                    bk = psZ[(g * 4 + t) % 2]
                    for hp in range(4):
                        for c in range(8):
                            P.mm(bk[:, hp * 128:(hp + 1) * 128], wq[:, c, hp * 128:(hp + 1) * 128], h2T[t][:, c, :], c == 0, c == 7,
                                 reads=[wqk, "h2T_%d" % t], writes=[bk.name])
                    P.copy("act", qTb[t][:, 4 * g:4 * g + 4, :].rearrange("p a t -> p (a t)"), bk[:, :], reads=[bk.name], writes=["qTb_%d" % t])
            for t in range(4):
                for g in range(4):
                    bk = psZ[g % 2]
                    for a in range(4):
                        hp = 4 * g + a
                        P.mm(bk[:, a * 128:(a + 1) * 128], qTb[t][:, hp, :], keysb[:, (hp % 2) * 128:(hp % 2 + 1) * 128], True, True,
                             reads=["qTb_%d" % t, "keysb"], writes=[bk.name])
                    P.copy("act", sbf[:, 4 * g:4 * g + 4, :].rearrange("p a n -> p (a n)"), bk[:, :], reads=[bk.name], writes=["sbf"])
                for hp in range(16):
                    P.op("dve", lambda: nc.vector.max(out=t16[:, hp, 0:8], in_=sbf[:, hp, :]), reads=["sbf"], writes=["t16"])
                    P.op("dve", lambda: nc.vector.match_replace(out=tmp128[:], in_to_replace=t16[:, hp, 0:8], in_values=sbf[:, hp, :], imm_value=-1e30),
                         reads=["sbf", "t16"], writes=["tmp128"])
                    P.op("dve", lambda: nc.vector.max(out=t16[:, hp, 8:16], in_=tmp128[:]), reads=["tmp128"], writes=["t16"])
                tv = t16[:].rearrange("p (h two) k -> p h two k", two=2)
                for h in range(8):
                    P.tt(cand[:, h, :].rearrange("p (a b) -> p a b", a=16), tv[:, h, 0, :].unsqueeze(2).broadcast_to([128, 16, 16]),
                         tv[:, h, 1, :].unsqueeze(1).broadcast_to([128, 16, 16]), ALU.add, reads=["t16"], writes=["cand"])
                for h in range(8):
                    P.op("dve", lambda: nc.vector.max(out=c16[:, h, 0:8], in_=cand[:, h, :]), reads=["cand"], writes=["c16"])
                    P.op("dve", lambda: nc.vector.match_replace(out=cand2[:], in_to_replace=c16[:, h, 0:8], in_values=cand[:, h, :], imm_value=-1e30),
                         reads=["cand", "c16"], writes=["cand2"])
                    P.op("dve", lambda: nc.vector.max(out=c16[:, h, 8:16], in_=cand2[:]), reads=["cand2"], writes=["c16"])
                P.copy("dve", thr[t][:], c16[:, :, 15], reads=["c16"], writes=["thr_%d" % t])
                P.tt(e16[:], c16[:], c16[:, :, 0:1].broadcast_to([128, 8, 16]), ALU.subtract, reads=["c16"], writes=["e16"])
                P.act(e16[:], e16[:], AF.Exp, reads=["e16"], writes=["e16"])
                P.op("dve", lambda: nc.vector.tensor_reduce(out=zs[:], in_=e16[:], axis=AX.X, op=ALU.add), reads=["e16"], writes=["zs"])
                P.act(zs[:], zs[:], AF.Ln, reads=["zs"], writes=["zs"])
                P.tt(zs[:], zs[:], c16[:, :, 0], ALU.add, reads=["zs", "c16"], writes=["zs"])
                P.ts(nb[t][:], zs[:], -1.0, None, ALU.mult, None, reads=["zs"], writes=["nb_%d" % t])
                for half in range(2):
                    for a in range(8):
                        P.tr(psT[:, a * 128:(a + 1) * 128], sbf[:, half * 8 + a, :], idb[:], reads=["sbf", "idb"], writes=[psT.name])
                    P.copy("act", sT[t][:, half * 8:half * 8 + 8, :].rearrange("p a t -> p (a t)"), psT[:, :], reads=[psT.name], writes=["sT_%d" % t])
            for ch in range(32):
                uw, uk = loadW(uT_d[:, ch * 512:(ch + 1) * 512])
                vi = vn[0] % 2; vn[0] += 1
                P.dma("pool", Vc[vi][:], v_d[ch * 512:(ch + 1) * 512, :].rearrange("(a p) n -> p a n", p=128), writes=["Vc%d" % vi])
                for t in range(4):
                    k = cnt[0] % 2
                    for h in range(8):
                        kk = (cnt[0] * 8 + h) % 2
                        bz = psZ[kk]
                        P.mm(bz[:, :], sT[t][:, 2 * h, :], sel1(ch), True, False, reads=["sT_%d" % t, "idb"], writes=[bz.name])
                        P.mm(bz[:, :], sT[t][:, 2 * h + 1, :], sel2, False, True, reads=["sT_%d" % t, "idb"], writes=[bz.name])
                        P.act(eb[kk][:], bz[:, :], AF.Exp, reads=[bz.name, "nb_%d" % t], writes=["eb%d" % kk], bias=nb[t][:, h:h + 1], scale=1.0)
                        P.stt(wb[kk][:], bz[:, :], thr[t][:, h:h + 1], eb[kk][:], ALU.is_ge, ALU.mult,
                              reads=[bz.name, "thr_%d" % t, "eb%d" % kk], writes=["wbm%d" % kk])
                        P.mm(psG[:, :], idb[:], wb[kk][:], h == 0, h == 7, reads=["idb", "wbm%d" % kk], writes=[psG.name])
                    for c in range(8):
                        P.mm(psH[:, :], h2T[t][:, c, :], uw[:, c, :], c == 0, c == 7, reads=["h2T_%d" % t, uk], writes=[psH.name])
                    P.act(gh[k][:], psH[:, :], AF.Gelu, reads=[psH.name], writes=["gh%d" % k])
                    P.tt(Ab[k][:], psG[:, :], gh[k][:], ALU.mult, reads=[psG.name, "gh%d" % k], writes=["Ab%d" % k])
                    for es in range(4):
                        P.tr(psT[:, es * 128:(es + 1) * 128], Ab[k][:, es * 128:(es + 1) * 128], idb[:], reads=["Ab%d" % k, "idb"], writes=[psT.name])
                    P.copy("act", AT[k][:].rearrange("p a t -> p (a t)"), psT[:, 0:512], reads=[psT.name], writes=["AT%d" % k])
                    for hf in range(2):
                        for es in range(4):
                            P.mm(psO[hf][:, :], AT[k][:, es, :], Vc[vi][:, es, hf * 512:(hf + 1) * 512], es == 0, es == 3,
                                 reads=["AT%d" % k, "Vc%d" % vi], writes=[psO[hf].name])
                        if ch == 0:
                            P.copy("dve", acc[t][:, hf * 512:(hf + 1) * 512], psO[hf][:, :], reads=[psO[hf].name], writes=["acc_%d" % t])
                        else:
                            P.tt(acc[t][:, hf * 512:(hf + 1) * 512], acc[t][:, hf * 512:(hf + 1) * 512], psO[hf][:, :], ALU.add,
                                 reads=[psO[hf].name, "acc_%d" % t], writes=["acc_%d" % t])
                    cnt[0] += 1
            for t in range(4):
                r0 = t0 + t * 128
                P.tt(ot[:], acc[t][:], G2, ALU.mult, reads=["acc_%d" % t, "mmod"], writes=["ot"], e="pool")
                P.tt(ot[:], ot[:], x1[t][:], ALU.add, reads=["ot", "x1_%d" % t], writes=["ot"], e="pool")
                P.dma("sp", over["otile"](r0) if "otile" in over else o_d[r0:r0 + 128, :], ot[:], reads=["ot"], writes=["oout"])
        for jx in range(len(P.dsem) if standalone else 0):
            if P.dcnt[jx] > 0:
                P._wait("sp", ("D%d" % jx, P.dsem[jx], 16 * P.dcnt[jx]))
        print("ffn ninst", P.ninst, "nwait", P.nwait)
    return nc


def prep_ffn(inp, layer, x_rows, y_rows, c_b, wout):
    return {
        "x": None if x_rows is None else np.ascontiguousarray(x_rows), "y": None if y_rows is None else np.ascontiguousarray(y_rows), "c": np.ascontiguousarray(c_b.reshape(8, 128).T),
        "adaw": np.ascontiguousarray(inp["ada_w"][layer][:, 2048:6144]), "adab": np.ascontiguousarray(inp["ada_b"][layer][None, 2048:6144]),
        "ng": np.ascontiguousarray(inp["norm_ffn_g"][layer][None, :]), "wout": np.ascontiguousarray(wout),
        "wq": np.ascontiguousarray(inp["peer_wq"][layer]),
        "keysT": np.ascontiguousarray(np.concatenate([inp["peer_keys"][layer][0].T, inp["peer_keys"][layer][1].T], axis=1)),
        "uT": np.ascontiguousarray(inp["peer_u"][layer].T), "v": np.ascontiguousarray(inp["peer_v"][layer]),
        "ident": np.eye(128, dtype=np.float32),
    }


def build_mix0(S, nc=None, P=None, pfx="", over=None):
    over = over or {}
    standalone = nc is None
    NT = S // 128
    if standalone:
        nc = bass.Bass("TRN2", target_bir_lowering=False)
    dt = lambda n, s: over[n] if n in over else nc.dram_tensor(pfx + n, s, F32, kind="ExternalInput").ap()
    x_d = dt("x", [S, D]); c_d = dt("c", [128, 8]); adaw_d = dt("adaw", [D, 2048]); adab_d = dt("adab", [1, 2048])
    ng_d = dt("ng", [1, D]); win_d = dt("win", [D, 1796]); convw_d = dt("convw", [128, 16]); gb_d = dt("gb", [1, 4])
    mg_d = dt("mg", [1, 256]); gqk_d = dt("gqk", [1, 512]); ident_d = dt("ident", [128, 128]); negm_d = dt("negm", [128, 128])
    posm_d = dt("posm", [128, 128]); trile_d = dt("trile", [128, 128]); sgt_d = dt("sgt", [128, 128])
    qab_d = dt("qab", [4, 4 * 128]); qad_d = dt("qad", [4, 4 * 128]); ka_d = dt("ka", [4, S]); kblk_d = dt("kblk", [32, S])
    fut_d = dt("fut", [33, 32]); own_d = dt("own", [33, 32])
    y_d = None if "ytile" in over else nc.dram_tensor("y", [S, 512], F32, kind="ExternalOutput").ap()
    with ExitStack() as st:
        if P is None:
            P = Prog(nc, st)
        P.st = st
        idf, idb = load_consts(P, ident_d)
        banks = [P.ps("bk%d" % i, [128, 512], F32) for i in range(7)]
        psT = P.ps("psT", [128, 1024], BF16)
        mod = compute_mod(P, c_d, adaw_d, adab_d, 2048, banks[0], "m")
        GM = P.sb("GM", [128, D], F32)
        P.dma("sp", GM[:], ng_d[0:1, :].partition_broadcast(128), writes=["GM"])
        P.stt(GM[:], mod[:, 1024:2048], 1.0, GM[:], ALU.add, ALU.mult, reads=["mmod", "GM"], writes=["GM"])
        SH = mod[:, 0:1024]
        gk = ["GM", "mmod"]
        cf = lambda name, src: (lambda t: (P.dma("sp", t[:], src, writes=[name]), t)[1])(P.sb(name, [128, 128], F32))
        negb = P.sb("negb", [128, 128], BF16)
        P.dma("pool", negb[:], negm_d[:, :], writes=["negb"])
        posm = cf("posm_s", posm_d[:, :]); trile = cf("trile_s", trile_d[:, :]); sgt = cf("sgt_s", sgt_d[:, :])
        onesf = P.sb("onesf", [128, 128], F32)
        P.memset("pool", onesf[:], 1.0, writes=["onesf"])
        convw = P.sb("convw_s", [128, 4, 4], F32)
        P.dma("sp", convw[:].rearrange("p a j -> p (a j)"), convw_d[:, :], writes=["convw"])
        GB = P.sb("GB", [128, 4], F32)
        P.dma("sp", GB[:], gb_d[0:1, :].partition_broadcast(128), writes=["GB"])
        MG = P.sb("MG", [128, 256], F32)
        P.dma("sp", MG[:], mg_d[0:1, :].partition_broadcast(128), writes=["MG"])
        Gqk = P.sb("Gqk", [128, 512], F32)
        P.dma("sp", Gqk[:], gqk_d[0:1, :].partition_broadcast(128), writes=["Gqk"])
        P.ts(Gqk[:, 0:256], Gqk[:, 0:256], 0.125, None, ALU.mult, None, reads=["Gqk"], writes=["Gqk"])
        wsb = P.sb("wsb", [128, 8, 1796], BF16)
        for c in range(8):
            P.dma("pool", wsb[:, c, :], win_d[c * 128:(c + 1) * 128, :], writes=["wsb"])
        KT = P.sb("KT", [100, 4, S], BF16)
        QT = P.sb("QT", [100, 4, 128], BF16)
        QTf = P.sb("QTf", [64, 4, 128], F32)
        qab = P.sb("qab_s", [100, 4, 128], F32)
        qad = P.sb("qad_s", [100, 4, 128], F32)
        P.dma("sp", qab[96:100, :, :], qab_d.rearrange("r (h t) -> r h t", h=4), writes=["qab"])
        P.dma("sp", qad[96:100, :, :], qad_d.rearrange("r (h t) -> r h t", h=4), writes=["qad"])
        for a in range(4):
            P.dma("pool", KT[64:96, a, :], kblk_d[:, :], writes=["KTaug"])
            P.dma("pool", KT[96:100, a, :], ka_d[:, :], writes=["KTaug"])
        Vm = P.sb("Vm", [128, NT, 4, 65], BF16)
        P.memset("pool", Vm[:, :, :, 64:65], 1.0, writes=["Vones"])
        kmT = P.sb("kmT", [64, 4, 32], F32)
        P.memset("pool", kmT[:], 0.0, writes=["kmT"])
        Bw = P.sb("Bw", [128, 4, 96], F32)
        P.memset("pool", Bw[:], 0.0, writes=["Bw"])
        FUT = P.sb("FUT", [128, 32], F32); OWN = P.sb("OWN", [128, 32], F32)
        xts = [P.sb("xt%d" % i, [128, D], F32) for i in range(2)]
        junk = P.sb("junk", [128, D], F32)
        hb = P.sb("hb", [128, D], BF16)
        hT = P.sb("hT", [128, 8, 128], BF16)
        small = P.sb("small", [128, 40], F32)
        cbuf = P.sb("cbuf", [128, 4, 131], F32)
        P.memset("pool", cbuf[:], 0.0, writes=["cbuf"])
        cacc = P.sb("cacc", [128, 4, 128], F32)
        ctmp = P.sb("ctmp", [128, 4, 128], F32)
        qkT = P.sb("qkT", [128, 4, 128], BF16)
        gi = P.sb("gi", [128, 8], F32)
        Lm = P.sb("Lm", [128, 128], F32)
        DT = P.sb("DT", [128, 128], F32)
        SmT = P.sb("SmT", [128, 128], BF16)
        vaug = P.sb("vaug", [128, 2, 129], BF16)
        P.memset("pool", vaug[:, :, 128:129], 1.0, writes=["vaug1"])
        kk = P.sb("kk", [128, 128], BF16)
        intra = P.sb("intra", [128, 129], F32)
        num = P.sb("num", [128, 129], F32)
        hm = P.sb("hm", [128, 128], F32)
        sig = P.sb("sig", [128, 128], F32)
        Cf = [P.sb("Cf%d" % m, [128, 129], F32) for m in range(2)]
        Cb = [P.sb("Cb%d" % m, [128, 129], BF16) for m in range(2)]
        for m in range(2):
            P.memset("pool", Cf[m][:], 0.0, writes=["Cf%d" % m])
            P.memset("pool", Cb[m][:], 0.0, writes=["Cb%d" % m])
        sq = P.sb("sq", [128, 512], F32)
        qkn = P.sb("qkn", [128, 512], F32)
        gm = P.sb("gm", [128, 32], F32)
        top8 = P.sb("top8", [128, 8], F32)
        PT = [P.sb("PT%d" % b, [128, 512], BF16) for b in range(2)]
        ymix = [P.sb("ymix%d" % b, [128, 512], F32) for b in range(2)]
        bkA, bkB, bkC, bkD, bkE, bkF, bkG = banks
        for i in range(NT):
            blk = i // 2
            xt = xts[i % 2]; xk = "xt%d" % (i % 2)
            ym = ymix[i % 2]; yk = "ymix%d" % (i % 2)
            P.dma("sp", xt[:], x_d[i * 128:(i + 1) * 128, :], writes=[xk])
            if i % 2 == 0:
                P.dma("sp", FUT[:], fut_d[blk:blk + 1, :].partition_broadcast(128), writes=["FUT"])
                P.dma("sp", OWN[:], own_d[blk:blk + 1, :].partition_broadcast(128), writes=["OWN"])
            norm_mod_T(P, xt[:], xk, GM, SH, gk, hb, hT, psT, idb, junk, small, "", act_copy=True)
            for a in range(4):
                for c in range(8):
                    P.mm(bkD[:, a * 128:(a + 1) * 128], wsb[:, c, a * 128:(a + 1) * 128], hT[:, c, :], c == 0, c == 7,
                         reads=["wsb", "hT"], writes=[bkD.name])
            for bk, c0, n in ((bkA, 512, 512), (bkB, 1024, 512), (bkC, 1536, 260)):
                for c in range(8):
                    P.mm(bk[:, 0:n], hT[:, c, :], wsb[:, c, c0:c0 + n], c == 0, c == 7, reads=["wsb", "hT"], writes=[bk.name])
            P.copy("pool", cbuf[:, :, 0:3], cbuf[:, :, 128:131], reads=["cbuf"], writes=["cbuf"])
            P.copy("act", cbuf[:, :, 3:131], bkD[:, :].rearrange("p (a t) -> p a t", a=4), reads=[bkD.name], writes=["cbuf"])
            for j in range(4):
                dst = cacc if j == 0 else ctmp
                P.tt(dst[:], cbuf[:, :, j:j + 128], convw[:, :, j:j + 1].broadcast_to([128, 4, 128]), ALU.mult,
                     reads=["cbuf", "convw"], writes=["cacc" if j == 0 else "ctmp"])
                if j > 0:
                    P.tt(cacc[:], cacc[:], ctmp[:], ALU.add, reads=["cacc", "ctmp"], writes=["cacc"])
            P.act(ctmp[:], cacc[:], AF.Silu, reads=["cacc"], writes=["ctmp"])
            P.copy("dve", qkT[:, 0:2, :], ctmp[:, 0:2, :], reads=["ctmp"], writes=["qkT"])
            P.ts(qkT[:, 2:4, :], ctmp[:, 2:4, :], 128.0 ** -0.5, None, ALU.mult, None, reads=["ctmp"], writes=["qkT"])
            P.tt(gi[:, 0:4], bkC[:, 256:260], GB[:], ALU.add, reads=[bkC.name, "GB"], writes=["gi"])
            P.act(gi[:, 4:6], gi[:, 2:4], AF.Exp, reads=["gi"], writes=["gi"], scale=-1.0)
            P.ts(gi[:, 4:6], gi[:, 4:6], 1.0, None, ALU.add, None, reads=["gi"], writes=["gi"])
            P.act(gi[:, 6:8], gi[:, 4:6], AF.Ln, reads=["gi"], writes=["gi"])
            nfl = gi[:, 6:8]
            P.mm(bkD[:, 0:2], trile[:], nfl, True, True, reads=["trile_s", "gi", "cbuf"], writes=[bkD.name])
            P.mm(bkD[:, 2:4], onesf[:], nfl, True, True, reads=["onesf", "gi"], writes=[bkD.name])
            P.act(small[:, 4:8], bkD[:, 0:4], AF.Exp, reads=[bkD.name], writes=["wd"], scale=-1.0)
            P.copy("act", vaug[:, :, 0:128], bkA[:, 0:256].rearrange("p (m d) -> p m d", m=2), reads=[bkA.name], writes=["vaug"])
            for m in range(2):
                P.ts(Lm[:], sgt[:], nfl[:, m:m + 1], None, ALU.mult, None, reads=["sgt_s", "gi"], writes=["Lm"])
                P.mm(bkE[:, 0:128], Lm[:], trile[:], True, False, reads=["Lm", "trile_s"], writes=[bkE.name])
                P.mm(bkE[:, 0:128], idf[:], posm[:], False, True, reads=["idf", "posm_s"], writes=[bkE.name])
                P.act(DT[:], bkE[:, 0:128], AF.Exp, reads=[bkE.name, "gi"], writes=["DT"], bias=gi[:, m:m + 1], scale=-1.0)
                P.mm(bkE[:, 128:256], qkT[:, 2 + m, :], qkT[:, m, :], True, True, reads=["qkT"], writes=[bkE.name])
                P.tt(SmT[:], bkE[:, 128:256], DT[:], ALU.mult, reads=[bkE.name, "DT"], writes=["SmT"])
                P.tr(psT[:, 0:128], qkT[:, 2 + m, :], idb[:], reads=["qkT", "idb"], writes=[psT.name])
                P.ts(kk[:], psT[:, 0:128], DT[:, 127:128], None, ALU.mult, None, reads=[psT.name, "DT"], writes=["kk"])
                P.mm(bkF[:, 0:129], qkT[:, m, :], Cb[m][:], True, True, reads=["qkT", "Cb%d" % m], writes=[bkF.name])
                P.mm(bkF[:, 136:265], SmT[:], vaug[:, m, :], True, True, reads=["SmT", "vaug", "vaug1"], writes=[bkF.name])
                P.mm(bkF[:, 272:401], kk[:], vaug[:, m, :], True, True, reads=["kk", "vaug", "vaug1"], writes=[bkF.name])
                P.copy("act", intra[:], bkF[:, 136:265], reads=[bkF.name], writes=["intra"])
                P.stt(num[:], bkF[:, 0:129], small[:, 4 + m:5 + m], intra[:], ALU.mult, ALU.add, reads=[bkF.name, "wd", "intra"], writes=["num"])
                P.stt(Cf[m][:], Cf[m][:], small[:, 6 + m:7 + m], bkF[:, 272:401], ALU.mult, ALU.add,
                      reads=["Cf%d" % m, "wd", bkF.name], writes=["Cf%d" % m])
                P.copy("pool", Cb[m][:], Cf[m][:], reads=["Cf%d" % m], writes=["Cb%d" % m])
                P.ts(small[:, 8:9], num[:, 128:129], 1.0, None, ALU.max, None, reads=["num"], writes=["den"])
                P.ts(small[:, 11:12], num[:, 128:129], -1.0, 1.0, ALU.mult, ALU.max, reads=["num"], writes=["den2"])
                P.tt(small[:, 8:9], small[:, 8:9], small[:, 11:12], ALU.max, reads=["den", "den2"], writes=["den"])
                P.op("dve", lambda: nc.vector.reciprocal(out=small[:, 8:9], in_=small[:, 8:9]), reads=["den"], writes=["den"])
                P.ts(hm[:], num[:, 0:128], small[:, 8:9], None, ALU.mult, None, reads=["num", "den"], writes=["hm"])
                P.act(sig[:], hm[:], AF.Square, reads=["hm"], writes=["sig", "ssm"], accum_out=small[:, 9:10])
                rstd_from_ss(P, small[:, 9:10], small[:, 10:11], 128, "ssm", "rstdm")
                P.act(sig[:], bkA[:, 256 + m * 128:256 + (m + 1) * 128], AF.Sigmoid, reads=[bkA.name], writes=["sig"])
                P.stt(hm[:], hm[:], small[:, 10:11], MG[:, m * 128:(m + 1) * 128], ALU.mult, ALU.mult, reads=["hm", "rstdm", "MG"], writes=["hm"])
                P.tt(ym[:, m * 128:(m + 1) * 128], hm[:], sig[:], ALU.mult, reads=["hm", "sig"], writes=[yk])
            P.act(sq[:], bkB[:, :], AF.Square, reads=[bkB.name], writes=["sq"])
            P.op("dve", lambda: nc.vector.tensor_reduce(out=small[:, 16:24], in_=sq[:].rearrange("p (g d) -> p g d", g=8), axis=AX.X, op=ALU.add),
                 reads=["sq"], writes=["ss8"])
            rstd_from_ss(P, small[:, 16:24], small[:, 24:32], 64, "ss8", "rstd8")
            P.tt(sq[:].rearrange("p (g d) -> p g d", g=8), bkB[:, :].rearrange("p (g d) -> p g d", g=8),
                 small[:, 24:32].unsqueeze(2).broadcast_to([128, 8, 64]), ALU.mult, reads=[bkB.name, "rstd8"], writes=["sq"])
            P.tt(qkn[:], sq[:], Gqk[:], ALU.mult, reads=["sq", "Gqk"], writes=["qkn"])
            P.copy("act", Vm[:, i, :, 0:64], bkC[:, 0:256].rearrange("p (a d) -> p a d", a=4), reads=[bkC.name], writes=["V%d" % i])
            for g in range(4):
                P.tr(bkB[0:64, g * 128:(g + 1) * 128], qkn[:, g * 64:(g + 1) * 64], idf[:], reads=["qkn", "idf"], writes=[bkB.name])
                P.tr(bkC[0:64, g * 128:(g + 1) * 128], qkn[:, 256 + g * 64:256 + (g + 1) * 64], idf[:], reads=["qkn", "idf"], writes=[bkC.name])
            P.copy("dve", QT[0:64, :, :], bkB[0:64, :].rearrange("p (a t) -> p a t", a=4), reads=[bkB.name], writes=["QT"])
            P.copy("dve", QTf[:], bkB[0:64, :].rearrange("p (a t) -> p a t", a=4), reads=[bkB.name], writes=["QTf"])
            P.copy("dve", KT[0:64, :, i * 128:(i + 1) * 128], bkC[0:64, :].rearrange("p (a t) -> p a t", a=4), reads=[bkC.name], writes=["K%d" % i])
            P.stt(QT[96:100, :, :], qad[96:100, :, :], float(i), qab[96:100, :, :], ALU.mult, ALU.add, reads=["qab", "qad"], writes=["QT"])
            for a in range(4):
                P.mm(bkG[:, a * 32:(a + 1) * 32], QTf[:, a, :], kmT[:, a, :], True, True, reads=["QTf", "kmT"], writes=[bkG.name])
            for a in range(4):
                P.tt(gm[:], bkG[:, a * 32:(a + 1) * 32], FUT[:], ALU.add, reads=[bkG.name, "FUT"], writes=["gm"])
                P.op("dve", lambda: nc.vector.max(out=top8[:], in_=gm[:]), reads=["gm"], writes=["top8"])
                P.ts(gm[:], gm[:], top8[:, 2:3], 30000.0, ALU.is_ge, ALU.mult, reads=["gm", "top8"], writes=["gm"])
                P.stt(Bw[:, a, 64:96], gm[:], -30000.0, OWN[:], ALU.add, ALU.max, reads=["gm", "OWN"], writes=["Bw"])
            for a in range(4):
                P.tr(bkB[0:96, a * 128:(a + 1) * 128], Bw[:, a, :], idf[:], reads=["Bw", "idf", "QT", "QTf"], writes=[bkB.name])
            P.copy("dve", QT[64:96, :, :], bkB[64:96, :].rearrange("p (a t) -> p a t", a=4), reads=[bkB.name], writes=["QT"])
            for a in range(4):
                P.mm(bkG[0:64, 128 + a:129 + a], qkn[:, 256 + a * 64:256 + (a + 1) * 64], onesf[:, 0:1], True, True,
                     reads=["qkn", "onesf"], writes=[bkG.name])
            if blk < 32:
                P.stt(kmT[:, :, blk], bkG[0:64, 128:132], 1.0 / 256.0, kmT[:, :, blk], ALU.mult, ALU.add, reads=[bkG.name, "kmT"], writes=["kmT"])
            ngrp = i // 4 + 1
            for a in range(4):
                for g in range(ngrp):
                    kts = list(range(4 * g, min(4 * g + 4, i + 1)))
                    n = len(kts)
                    bank = (bkE, bkF)[g % 2]
                    for jj, kt in enumerate(kts):
                        P.mm(bank[:, jj * 128:(jj + 1) * 128], KT[0:100, a, kt * 128:(kt + 1) * 128], QT[0:100, a, :],
                             True, kt != i, reads=["K%d" % kt, "KTaug", "QT"], writes=[bank.name])
                        if kt == i:
                            P.mm(bank[:, jj * 128:(jj + 1) * 128], idb[:], negb[:], False, True, reads=["idb", "negb"], writes=[bank.name])
                    pt = PT[g % 2]; pk = "PT%d" % (g % 2)
                    P.act(pt[:, 0:n * 128], bank[:, 0:n * 128], AF.Exp, reads=[bank.name], writes=[pk])
                    for jj, kt in enumerate(kts):
                        P.mm(bkA[:, 0:65], pt[:, jj * 128:(jj + 1) * 128], Vm[:, kt, a, :], kt == 0, kt == i,
                             reads=[pk, "V%d" % kt, "Vones"], writes=[bkA.name])
                P.op("dve", lambda: nc.vector.reciprocal(out=small[:, 12:13], in_=bkA[:, 64:65]), reads=[bkA.name], writes=["rdb"])
                P.ts(ym[:, 256 + a * 64:256 + (a + 1) * 64], bkA[:, 0:64], small[:, 12:13], None, ALU.mult, None,
                     reads=[bkA.name, "rdb"], writes=[yk])
            P.dma("sp", over["ytile"](i) if "ytile" in over else y_d[i * 128:(i + 1) * 128, :], ym[:], reads=[yk], writes=["yout"])
        for jx in range(len(P.dsem) if standalone else 0):
            if P.dcnt[jx] > 0:
                P._wait("sp", ("D%d" % jx, P.dsem[jx], 16 * P.dcnt[jx]))
        print("mix0 ninst", P.ninst, "nwait", P.nwait)
    return nc


def prep_mix0(inp, layer, x_b, c_b, hh, S):
    e = layer // 2
    w = inp["ev_w_in"][e]
    mh = [2 * hh, 2 * hh + 1]; bh = [4 * hh + a for a in range(4)]
    cols = []
    for base in (0, 512):
        for m in mh:
            cols.append(w[:, base + m * 128:base + (m + 1) * 128])
    for base in (1024, 1536):
        for m in mh:
            cols.append(w[:, base + m * 128:base + (m + 1) * 128])
    for base in (2056, 2568):
        for a in bh:
            cols.append(w[:, base + a * 64:base + (a + 1) * 64])
    for a in bh:
        cols.append(w[:, 3080 + a * 64:3080 + (a + 1) * 64])
    cols.append(w[:, 2048 + mh[0]:2048 + mh[0] + 2])
    cols.append(w[:, 2052 + mh[0]:2052 + mh[0] + 2])
    win = np.ascontiguousarray(np.concatenate(cols, axis=1))
    cw = inp["ev_conv_w"][e]
    convw = np.zeros((128, 4, 4), np.float32)
    for ai, (base, m) in enumerate([(0, mh[0]), (0, mh[1]), (512, mh[0]), (512, mh[1])]):
        convw[:, ai, :] = cw[:, base + m * 128:base + (m + 1) * 128].T
    gb = np.concatenate([inp["ev_igate_b"][e][mh[0]:mh[0] + 2], inp["ev_fgate_b"][e][mh[0]:mh[0] + 2]])[None, :]
    mg = inp["ev_mnorm_g"][e][mh[0]:mh[0] + 2].reshape(1, 256)
    qn = inp["ev_qn_g"][e]; kn = inp["ev_kn_g"][e]
    gqk = np.concatenate([qn] * 4 + [kn] * 4)[None, :]
    ident, negm, ka = mix1_consts(S)
    sl = slopes8()[4 * hh:4 * hh + 4]
    qab, qad = alibi_q_rows(sl)
    li = np.arange(128)
    trile = (li[:, None] <= li[None, :]).astype(np.float32)
    sgt = (li[:, None] > li[None, :]).astype(np.float32)
    t = np.arange(S)
    kblk = (np.arange(32)[:, None] == (t // 256)[None, :]).astype(np.float32)
    nb = np.arange(32)
    fut = np.stack([np.where(nb >= b, NEG, 0.0) for b in range(33)]).astype(np.float32)
    own = np.stack([np.where(nb == b, 0.0, NEG) for b in range(33)]).astype(np.float32)
    f = np.ascontiguousarray
    return {
        "x": f(x_b), "c": f(c_b.reshape(8, 128).T), "adaw": f(inp["ada_w"][layer][:, 0:2048]), "adab": f(inp["ada_b"][layer][None, 0:2048]),
        "ng": f(inp["norm_mix_g"][layer][None, :]), "win": win, "convw": f(convw.reshape(128, 16)), "gb": f(gb.astype(np.float32)),
        "mg": f(mg), "gqk": f(gqk.astype(np.float32)), "ident": ident, "negm": negm, "posm": f(-negm), "trile": trile, "sgt": sgt,
        "qab": qab, "qad": qad, "ka": ka, "kblk": kblk, "fut": fut, "own": own,
    }


PAIRS = [[0, 1], [2, 3], [4, 5], [6, 7]]


def build_fused(S):
    NTOK = S // 2
    NCH = max(1, (S * 512 * 4) // (2 << 20))
    RY = S // NCH
    RX = NTOK // NCH
    nc = bass.Bass("TRN2", target_bir_lowering=False)
    dr = lambda n, shp: nc.dram_tensor(n, shp, F32).ap()
    y0c = [dr("y0c%d" % j, [RY, 512]) for j in range(NCH)]; yg0c = [dr("yg0c%d" % j, [2 * RY, 512]) for j in range(NCH)]
    y1c = [dr("y1c%d" % j, [RY, 512]) for j in range(NCH)]; yg1c = [dr("yg1c%d" % j, [2 * RY, 512]) for j in range(NCH)]
    x1c = [dr("x1c%d" % j, [RX, D]) for j in range(NCH)]; x1gc = [dr("x1gc%d" % j, [2 * RX, D]) for j in range(NCH)]

    def ytile(ch):
        return lambda i: ch[(i * 128) // RY][(i * 128) % RY:(i * 128) % RY + 128, :]

    def ygtile(ch):
        def f(r, q, r0):
            t = q * NTOK + r0
            return ch[t // RY][r * RY + t % RY:r * RY + t % RY + 128, :]
        return f

    def x1tile(r0):
        return x1c[r0 // RX][r0 % RX:r0 % RX + 128, :]

    def x1gtile(i):
        t = i * 128
        rank, loc = t // NTOK, t % NTOK
        return x1gc[loc // RX][rank * RX + loc % RX:rank * RX + loc % RX + 128, :]

    with ExitStack() as sems:
        Ps = [Prog(nc, None, 16, p, sems) for p in ("a_", "b_", "c_", "d_")]
        ccs = [sems.enter_context(nc.semaphore("cc%d" % i)) for i in range(3)]

        def exchange(Pprev, Pnext, srcs, dsts, cs):
            toks = Pprev.all_tokens()
            Pnext.wait_all(toks, engines=["pool"])
            for a_, d_ in zip(srcs, dsts):
                nc.gpsimd.collective_compute("AllGather", ALU.bypass, replica_groups=PAIRS, ins=[a_.opt()], outs=[d_.opt()]).then_inc(cs)
            Pnext.wait_all(toks + [("CC", cs, len(srcs))], engines=["pe", "act", "dve", "sp", "pool"])

        build_mix0(S, nc, Ps[0], "a_", over={"ytile": ytile(y0c)})
        exchange(Ps[0], Ps[1], y0c, yg0c, ccs[0])
        build_ffn(NTOK, nc, Ps[1], "b_", over={"yg": ygtile(yg0c), "otile": x1tile})
        exchange(Ps[1], Ps[2], x1c, x1gc, ccs[1])
        build_mix1(S, nc, Ps[2], "c_", over={"xtile": x1gtile, "ytile": ytile(y1c)})
        exchange(Ps[2], Ps[3], y1c, yg1c, ccs[2])
        build_ffn(NTOK, nc, Ps[3], "d_", over={"xtile": x1tile, "yg": ygtile(yg1c)})
        Ps[3].wait_all(Ps[3].all_tokens(), engines=["sp"])
        print("fused ninst", sum(p.ninst for p in Ps), "nwait", sum(p.nwait for p in Ps))
    return nc


_CACHE = {}


def _get(name, fn, *a):
    key = (name,) + a
    if key not in _CACHE:
        _CACHE[key] = fn(*a)
    return _CACHE[key]


def kernel(**inputs):
    inp = {k: np.asarray(v) for k, v in inputs.items()}
    x = inp["x"].astype(np.float32, copy=False)
    c = inp["c"]
    B, S, _ = x.shape
    NTOK = S // 2
    nc = _get("fused", build_fused, S)
    wo0 = inp["ev_w_out"][0]
    wo0p = np.concatenate([wo0[0:256], wo0[512:768], wo0[256:512], wo0[768:1024]], axis=0)
    maps = []
    for k in range(8):
        b, hh = k // 2, k % 2
        hsel = np.zeros((128, 2), np.float32); hsel[:, hh] = 1.0
        m = {}
        for pfx, d in (("a_", prep_mix0(inp, 0, x[b], c[b], hh, S)),
                       ("b_", prep_ffn(inp, 0, x[b, hh * NTOK:(hh + 1) * NTOK], None, c[b], wo0p)),
                       ("c_", prep_mix1(inp, 1, None, c[b], hh, S)),
                       ("d_", prep_ffn(inp, 1, None, None, c[b], inp["od_w_out"][0]))):
            for kk, v in d.items():
                if v is not None:
                    m[pfx + kk] = v
        m["b_hsel"] = hsel; m["d_hsel"] = hsel
        maps.append(m)
    res = run_bass_kernel_spmd(nc, maps, core_ids=list(range(8)))
    out = np.concatenate([res.results[k]["o"] for k in range(8)], axis=0).reshape(B, S, D)
    return out.astype(np.float32)
```

```python
import numpy as np
from contextlib import ExitStack
import concourse.bass as bass
import concourse.mybir as mybir
from concourse.bass_utils import run_bass_kernel_spmd

F32 = mybir.dt.float32
BF16 = mybir.dt.bfloat16
AF = mybir.ActivationFunctionType
ALU = mybir.AluOpType
AX = mybir.AxisListType

D = 1024
EPS = 1e-6
NEG = -30000.0
import os
STAGE = int(os.environ.get('KSTAGE', '9'))
SUB = int(os.environ.get('KSUB', '0'))
KTENG = os.environ.get('KTENG', 'dve')


class Prog:
    def __init__(self, nc, stack, n_dma_sems=24, pfx="", sem_stack=None):
        self.nc = nc
        self.st = stack
        self.pfx = pfx
        sem_stack = sem_stack if sem_stack is not None else stack
        self.eng = {"pe": nc.tensor, "act": nc.scalar, "dve": nc.vector, "pool": nc.gpsimd, "sp": nc.sync}
        self.sem = {k: sem_stack.enter_context(nc.semaphore(pfx + "s_" + k)) for k in self.eng}
        self.cnt = {k: 0 for k in self.eng}
        self.dsem = [sem_stack.enter_context(nc.semaphore(pfx + "d%d" % i)) for i in range(n_dma_sems)]
        self.dcnt = [0] * n_dma_sems
        self.dnext = 0
        self.waited = {k: {} for k in self.eng}
        self.lastw = {}
        self.readers = {}
        self.ninst = 0
        self.nwait = 0

    def sb(self, name, shape, dt):
        return self.st.enter_context(self.nc.sbuf_tensor(self.pfx + name, shape, dt))

    def ps(self, name, shape, dt):
        return self.st.enter_context(self.nc.psum_tensor(self.pfx + name, shape, dt))

    def all_tokens(self):
        toks = [("E" + e, self.sem[e], self.cnt[e]) for e in self.eng if self.cnt[e] > 0]
        toks += [("D%d" % j, self.dsem[j], 16 * self.dcnt[j]) for j in range(len(self.dsem)) if self.dcnt[j] > 0]
        return toks

    def wait_all(self, toks, engines=None):
        for e in (engines or self.eng):
            for sid, sm, v in toks:
                self.eng[e].wait_ge(sm, v)
                self.nwait += 1

    def _wait(self, e, tok):
        if tok is None:
            return
        sid, s, v = tok
        if self.waited[e].get(sid, 0) >= v:
            return
        self.eng[e].wait_ge(s, v)
        self.nwait += 1
        self.waited[e][sid] = v

    def _deps(self, e, reads, writes):
        for k in reads:
            self._wait(e, self.lastw.get(k))
        for k in writes:
            self._wait(e, self.lastw.get(k))
            for t in self.readers.get(k, ()):
                self._wait(e, t)

    def _commit(self, tok, reads, writes):
        for k in writes:
            self.lastw[k] = tok
            self.readers[k] = []
        for k in reads:
            lst = self.readers.setdefault(k, [])
            lst.append(tok)
            if len(lst) > 16:
                best = {}
                for t in lst:
                    if t[0] not in best or best[t[0]][2] < t[2]:
                        best[t[0]] = t
                self.readers[k] = list(best.values())

    def op(self, e, ins_fn, reads=(), writes=()):
        self._deps(e, reads, writes)
        ins = ins_fn()
        self.cnt[e] += 1
        ins.then_inc(self.sem[e], 1)
        tok = ("E" + e, self.sem[e], self.cnt[e])
        self.waited[e]["E" + e] = self.cnt[e] - 1
        self._commit(tok, reads, writes)
        self.ninst += 1
        return tok

    def dma(self, q, out, in_, reads=(), writes=(), **kw):
        j = self.dnext
        self.dnext = (self.dnext + 1) % len(self.dsem)
        if self.dcnt[j] > 0:
            self._wait(q, ("D%d" % j, self.dsem[j], 16 * self.dcnt[j]))
        self._deps(q, reads, writes)
        ins = self.eng[q].dma_start(out=out, in_=in_, **kw)
        self.dcnt[j] += 1
        ins.then_inc(self.dsem[j], 16)
        tok = ("D%d" % j, self.dsem[j], 16 * self.dcnt[j])
        self.waited[q]["D%d" % j] = 16 * (self.dcnt[j] - 1)
        self._commit(tok, reads, writes)
        self.ninst += 1
        return tok

    def finish(self, keys, e="sp"):
        for k in keys:
            self._wait(e, self.lastw.get(k))

    def mm(self, out, lhsT, rhs, start, stop, reads, writes):
        nc = self.nc
        return self.op("pe", lambda: nc.tensor.matmul(out, lhsT=lhsT, rhs=rhs, start=start, stop=stop),
                       reads=reads, writes=writes)

    def tr(self, out, in_, ident, reads, writes):
        nc = self.nc
        return self.op("pe", lambda: nc.tensor.transpose(out=out, in_=in_, identity=ident), reads=reads, writes=writes)

    def act(self, out, in_, func, reads, writes, **kw):
        nc = self.nc
        return self.op("act", lambda: nc.scalar.activation(out=out, in_=in_, func=func, **kw), reads=reads, writes=writes)

    def tt(self, out, in0, in1, op, reads, writes, e="dve"):
        eng = self.eng[e]
        return self.op(e, lambda: eng.tensor_tensor(out=out, in0=in0, in1=in1, op=op), reads=reads, writes=writes)

    def ts(self, out, in0, s1, s2, op0, op1, reads, writes, e="dve", **kw):
        eng = self.eng[e]
        if op1 is None:
            return self.op(e, lambda: eng.tensor_scalar(out=out, in0=in0, scalar1=s1, scalar2=None, op0=op0, **kw),
                           reads=reads, writes=writes)
        return self.op(e, lambda: eng.tensor_scalar(out=out, in0=in0, scalar1=s1, scalar2=s2, op0=op0, op1=op1, **kw),
                       reads=reads, writes=writes)

    def stt(self, out, in0, scalar, in1, op0, op1, reads, writes):
        nc = self.nc
        return self.op("dve", lambda: nc.vector.scalar_tensor_tensor(out=out, in0=in0, scalar=scalar, in1=in1, op0=op0, op1=op1),
                       reads=reads, writes=writes)

    def copy(self, e, out, in_, reads, writes):
        nc = self.nc
        if e == "act":
            return self.op("act", lambda: nc.scalar.copy(out=out, in_=in_), reads=reads, writes=writes)
        eng = self.eng[e]
        return self.op(e, lambda: eng.tensor_copy(out=out, in_=in_), reads=reads, writes=writes)

    def memset(self, e, ap, val, writes):
        eng = self.eng[e]
        return self.op(e, lambda: eng.memset(ap, val), writes=writes)


def load_consts(P, ident_d):
    idf = P.sb("idf", [128, 128], F32)
    idb = P.sb("idb", [128, 128], BF16)
    P.dma("sp", idf[:], ident_d[:, :], writes=["idf"])
    P.dma("pool", idb[:], ident_d[:, :], writes=["idb"])
    return idf, idb


def compute_mod(P, c_d, adaw_d, adab_d, ncols, ps_bank, name):
    nc = P.nc
    CW = 256
    ccol = P.sb(name + "_ccol", [128, 8], F32)
    cact = P.sb(name + "_cact", [128, 8], F32)
    crep = P.sb(name + "_crep", [128, 8, 128], F32)
    mod = P.sb(name + "_mod", [128, ncols], F32)
    brep = P.sb(name + "_brep", [128, CW], F32)
    wch = P.sb(name + "_wch", [128, 8, CW], F32)
    P.dma("sp", ccol[:], c_d[:, :], writes=[name + "ccol"])
    P.act(cact[:], ccol[:], AF.Silu, reads=[name + "ccol"], writes=[name + "cact"])
    P.copy("dve", crep[:], cact[:].unsqueeze(2).broadcast_to([128, 8, 128]), reads=[name + "cact"], writes=[name + "crep"])
    for j in range(ncols // CW):
        P.dma("sp", wch[:], adaw_d[:, j * CW:(j + 1) * CW].rearrange("(c p) n -> p c n", p=128), writes=[name + "wch"])
        P.dma("sp", brep[:], adab_d[0:1, j * CW:(j + 1) * CW].partition_broadcast(128), writes=[name + "brep"])
        for c in range(8):
            P.mm(ps_bank[:, 0:CW], crep[:, c, :], wch[:, c, :], c == 0, c == 7,
                 reads=[name + "crep", name + "wch"], writes=[ps_bank.name])
        P.tt(mod[:, j * CW:(j + 1) * CW], ps_bank[:, 0:CW], brep[:], ALU.add,
             reads=[ps_bank.name, name + "brep"], writes=[name + "mod"])
    return mod


def rstd_from_ss(P, ss, rstd, n, key_in, key_out, width=1):
    nc = P.nc
    P.ts(rstd, ss, 1.0 / n, EPS, ALU.mult, ALU.add, reads=[key_in], writes=[key_out])
    P.act(rstd, rstd, AF.Sqrt, reads=[key_out], writes=[key_out])
    P.op("dve", lambda: nc.vector.reciprocal(out=rstd, in_=rstd), reads=[key_out], writes=[key_out])


def norm_mod_T(P, xt, xkey, GM, SH, gkeys, hb, hT, psT, idb, junk, small, tag, act_copy=True):
    nc = P.nc
    ss = small[:, 0:1]
    rstd = small[:, 1:2]
    P.act(junk[:], xt, AF.Square, reads=[xkey], writes=["junk" + tag, "ss" + tag], accum_out=ss)
    rstd_from_ss(P, ss, rstd, D, "ss" + tag, "rstd" + tag)
    P.stt(junk[:], xt, rstd, GM[:], ALU.mult, ALU.mult, reads=[xkey, "rstd" + tag] + gkeys, writes=["junk" + tag])
    P.tt(hb[:], junk[:], SH[:], ALU.add, reads=["junk" + tag] + gkeys, writes=["hb" + tag])
    for c in range(8):
        P.tr(psT[:, c * 128:(c + 1) * 128], hb[:, c * 128:(c + 1) * 128], idb[:], reads=["hb" + tag, "idb"], writes=[psT.name])
    P.copy("act" if act_copy else "dve", hT[:].rearrange("p c t -> p (c t)"), psT[:, :], reads=[psT.name], writes=["hT" + tag])


def build_mix1(S, nc=None, P=None, pfx="", over=None):
    over = over or {}
    standalone = nc is None
    NT = S // 128
    if standalone:
        nc = bass.Bass("TRN2", target_bir_lowering=False)
    dt = lambda n, s: over[n] if n in over else nc.dram_tensor(pfx + n, s, F32, kind="ExternalInput").ap()
    x_d = None if "xtile" in over else dt("x", [S, D])
    c_d = dt("c", [128, 8]); adaw_d = dt("adaw", [D, 2048]); adab_d = dt("adab", [1, 2048])
    ng_d = dt("ng", [1, D]); win_d = dt("win", [D, 4 * 384]); gqk_d = dt("gqk", [1, 256]); lam_d = dt("lam", [1, 256])
    ong_d = dt("ong", [1, 128]); ident_d = dt("ident", [128, 128]); negm_d = dt("negm", [128, 128])
    qab_d = dt("qab", [4, 4 * 128]); qad_d = dt("qad", [4, 4 * 128]); ka_d = dt("ka", [4, S])
    laminit_d = dt("laminit", [1, 2])
    y_d = None if "ytile" in over else nc.dram_tensor("y", [S, 512], F32, kind="ExternalOutput").ap()
    with ExitStack() as st:
        if P is None:
            P = Prog(nc, st)
        P.st = st
        idf, idb = load_consts(P, ident_d)
        banks = [P.ps("bk%d" % i, [128, 512], F32) for i in range(7)]
        psT = P.ps("psT", [128, 1024], BF16)
        mod = compute_mod(P, c_d, adaw_d, adab_d, 2048, banks[0], "m")
        GM = P.sb("GM", [128, D], F32)
        P.dma("sp", GM[:], ng_d[0:1, :].partition_broadcast(128), writes=["GM"])
        P.stt(GM[:], mod[:, 1024:2048], 1.0, GM[:], ALU.add, ALU.mult, reads=["mmod", "GM"], writes=["GM"])
        SH = mod[:, 0:1024]
        gk = ["GM", "mmod"]
        negb = P.sb("negb", [128, 128], BF16)
        P.dma("pool", negb[:], negm_d[:, :], writes=["negb"])
        Gqk = P.sb("Gqk", [128, 256], F32)
        P.dma("sp", Gqk[:], gqk_d[0:1, :].partition_broadcast(128), writes=["Gqk"])
        P.ts(Gqk[:, 0:128], Gqk[:, 0:128], 0.125, None, ALU.mult, None, reads=["Gqk"], writes=["Gqk"])
        ONG = P.sb("ONG", [128, 128], F32)
        P.dma("sp", ONG[:], ong_d[0:1, :].partition_broadcast(128), writes=["ONG"])
        lamt = P.sb("lamt_s", [128, 256], F32)
        lami = P.sb("lami", [128, 2], F32)
        lsm = P.sb("lsm", [128, 4], F32)
        P.dma("sp", lamt[:], lam_d[0:1, :].partition_broadcast(128), writes=["lamt"])
        P.dma("sp", lami[:], laminit_d[0:1, :].partition_broadcast(128), writes=["lami"])
        lv = lamt[:].rearrange("p (a d) -> p a d", a=4)
        P.tt(lamt[:, 0:64], lv[:, 0, :], lv[:, 1, :], ALU.mult, reads=["lamt"], writes=["lamt"])
        P.tt(lamt[:, 128:192], lv[:, 2, :], lv[:, 3, :], ALU.mult, reads=["lamt"], writes=["lamt"])
        P.op("dve", lambda: nc.vector.tensor_reduce(out=lsm[:, 0:1], in_=lamt[:, 0:64], axis=AX.X, op=ALU.add), reads=["lamt"], writes=["lsm"])
        P.op("dve", lambda: nc.vector.tensor_reduce(out=lsm[:, 1:2], in_=lamt[:, 128:192], axis=AX.X, op=ALU.add), reads=["lamt"], writes=["lsm"])
        P.act(lsm[:, 0:2], lsm[:, 0:2], AF.Exp, reads=["lsm"], writes=["lsm"])
        P.tt(lsm[:, 2:3], lsm[:, 1:2], lsm[:, 0:1], ALU.subtract, reads=["lsm"], writes=["lsm"])
        P.tt(lsm[:, 3:4], lsm[:, 2:3], lami[:, 0:1], ALU.subtract, reads=["lsm", "lami"], writes=["neglam"])
        neglam = lsm[:, 3:4]
        P.ts(ONG[:], ONG[:], lami[:, 1:2], None, ALU.mult, None, reads=["ONG", "lami"], writes=["ONG"])

        KT = P.sb("KT", [68, 2, S], BF16)
        QT = P.sb("QT", [68, 2, 128], BF16)
        qab = P.sb("qab_s", [68, 4, 128], F32)
        qad = P.sb("qad_s", [68, 4, 128], F32)
        P.dma("sp", qab[64:68, :, :], qab_d.rearrange("r (h t) -> r h t", h=4), writes=["qab"])
        P.dma("sp", qad[64:68, :, :], qad_d.rearrange("r (h t) -> r h t", h=4), writes=["qad"])
        for c in range(2):
            P.dma("pool", KT[64:68, c, :], ka_d[:, :], writes=["KTaug"])
        Vaug = P.sb("Vaug", [128, NT, 129], BF16)
        P.memset("pool", Vaug[:, :, 128:129], 1.0, writes=["Vones"])
        wsb = P.sb("wsb", [128, 8, 384], BF16)
        xts = [P.sb("xt%d" % i, [128, D], F32) for i in range(2)]
        junk = P.sb("junk", [128, D], F32)
        hb = P.sb("hb", [128, D], BF16)
        hT = P.sb("hT", [128, 8, 128], BF16)
        small = P.sb("small", [128, 16], F32)
        sq = P.sb("sq", [128, 256], F32)
        qkn = P.sb("qkn", [128, 256], BF16)
        PT = [[P.sb("PT%d%d" % (c, b), [128, 512], BF16) for b in range(2)] for c in range(2)]
        osb = P.sb("osb", [128, 128], F32)
        o2 = P.sb("o2", [128, 128], F32)
        yo = [P.sb("yo%d" % i, [128, 128], F32) for i in range(2)]
        psZ = banks[0]
        psS = [[banks[1], banks[2]], [banks[3], banks[4]]]
        psO = [banks[5], banks[6]]
        it = 0
        for j in range(4 if STAGE >= 1 else 0):
            P.dma("pool", wsb[:], win_d[:, j * 384:(j + 1) * 384].rearrange("(c p) n -> p c n", p=128),
                  writes=["wsb"])
            for i in range(NT):
                xt = xts[it % 2]; xk = "xt%d" % (it % 2)
                P.dma("sp", xt[:], over["xtile"](i) if "xtile" in over else x_d[i * 128:(i + 1) * 128, :], writes=[xk])
                norm_mod_T(P, xt[:], xk, GM, SH, gk, hb, hT, psT, idb, junk, small, "", act_copy=True)
                if STAGE < 2:
                    continue
                for c in range(8):
                    P.mm(psZ[:, 0:384], hT[:, c, :], wsb[:, c, :], c == 0, c == 7, reads=["hT", "wsb"], writes=[psZ.name])
                if STAGE < 3:
                    continue
                if SUB == 5:
                    continue
                P.act(sq[:], psZ[:, 0:256], AF.Square, reads=[psZ.name], writes=["sq"])
                P.op("dve", lambda: nc.vector.tensor_reduce(out=small[:, 4:8], in_=sq[:].rearrange("p (g d) -> p g d", g=4), axis=AX.X, op=ALU.add),
                     reads=["sq"], writes=["ss4"])
                rstd_from_ss(P, small[:, 4:8], small[:, 8:12], 64, "ss4", "rstd4")
                P.tt(sq[:].rearrange("p (g d) -> p g d", g=4), psZ[:, 0:256].rearrange("p (g d) -> p g d", g=4),
                     small[:, 8:12].unsqueeze(2).broadcast_to([128, 4, 64]), ALU.mult, reads=[psZ.name, "rstd4"], writes=["sq"])
                P.tt(qkn[:], sq[:], Gqk[:], ALU.mult, reads=["sq", "Gqk"], writes=["qkn"])
                if SUB == 2:
                    continue
                P.copy("act", Vaug[:, i, 0:128], psZ[:, 256:384], reads=[psZ.name], writes=["V%d" % i])
                if SUB == 3:
                    continue
                for g in range(4):
                    P.tr(psT[0:64, g * 128:(g + 1) * 128], qkn[:, g * 64:(g + 1) * 64], idb[:], reads=["qkn", "idb"], writes=[psT.name])
                if SUB == 4:
                    continue
                P.copy("dve", QT[0:64, :, :], psT[0:64, 0:256].rearrange("p (c t) -> p c t", c=2), reads=[psT.name], writes=["QT"])
                if SUB == 6:
                    continue
                for c in range(2):
                    P.copy(KTENG, KT[0:64, c, i * 128:(i + 1) * 128], psT[0:64, 256 + c * 128:256 + (c + 1) * 128],
                           reads=[psT.name], writes=["K%d" % i])
                if SUB == 7:
                    continue
                if SUB != 1:
                  P.stt(QT[64:68, :, :], qad[64:68, j:j + 1, :].broadcast_to([4, 2, 128]), float(i),
                        qab[64:68, j:j + 1, :].broadcast_to([4, 2, 128]), ALU.mult, ALU.add, reads=["qab", "qad"], writes=["QT"])
                if STAGE < 4:
                    continue
                ngrp = i // 4 + 1
                for g in range(ngrp):
                    kts = list(range(4 * g, min(4 * g + 4, i + 1)))
                    n = len(kts)
                    for c in range(2):
                        bank = psS[c][g % 2]
                        for jj, kt in enumerate(kts):
                            P.mm(bank[:, jj * 128:(jj + 1) * 128], KT[0:68, c, kt * 128:(kt + 1) * 128], QT[0:68, c, :],
                                 True, kt != i, reads=["K%d" % kt, "KTaug", "QT"], writes=[bank.name])
                            if kt == i:
                                P.mm(bank[:, jj * 128:(jj + 1) * 128], idb[:], negb[:], False, True,
                                     reads=["idb", "negb"], writes=[bank.name])
                        pt = PT[c][g % 2]; pk = "PT%d%d" % (c, g % 2)
                        P.act(pt[:, 0:n * 128], bank[:, 0:n * 128], AF.Exp, reads=[bank.name], writes=[pk])
                        for jj, kt in enumerate(kts):
                            P.mm(psO[c][:, 0:129], pt[:, jj * 128:(jj + 1) * 128], Vaug[:, kt, :], kt == 0, kt == i,
                                 reads=[pk, "V%d" % kt, "Vones"], writes=[psO[c].name])
                if STAGE < 5:
                    continue
                P.op("dve", lambda: nc.vector.reciprocal(out=small[:, 12:13], in_=psO[0][:, 128:129]), reads=[psO[0].name], writes=["rd0"])
                P.op("dve", lambda: nc.vector.reciprocal(out=small[:, 13:14], in_=psO[1][:, 128:129]), reads=[psO[1].name], writes=["rd1"])
                P.ts(o2[:], psO[1][:, 0:128], small[:, 13:14], neglam, ALU.mult, ALU.mult, reads=[psO[1].name, "rd1", "neglam"], writes=["o2"])
                P.stt(osb[:], psO[0][:, 0:128], small[:, 12:13], o2[:], ALU.mult, ALU.add, reads=[psO[0].name, "rd0", "o2"], writes=["osb"])
                P.act(o2[:], osb[:], AF.Square, reads=["osb"], writes=["o2", "sso"], accum_out=small[:, 14:15])
                rstd_from_ss(P, small[:, 14:15], small[:, 15:16], 128, "sso", "rstdo")
                yt = yo[it % 2]; yk = "yo%d" % (it % 2)
                P.stt(yt[:], osb[:], small[:, 15:16], ONG[:], ALU.mult, ALU.mult, reads=["osb", "rstdo", "ONG"], writes=[yk])
                ydst = over["ytile"](i)[:, j * 128:(j + 1) * 128] if "ytile" in over else y_d[i * 128:(i + 1) * 128, j * 128:(j + 1) * 128]
                P.dma("sp", ydst, yt[:], reads=[yk], writes=["yout"])
                it += 1
        P.finish(["yout"])
        for k, v in P.lastw.items():
            pass
        for jx in range(len(P.dsem) if standalone else 0):
            if P.dcnt[jx] > 0:
                P._wait("sp", ("D%d" % jx, P.dsem[jx], 16 * P.dcnt[jx]))
        print("mix1 ninst", P.ninst, "nwait", P.nwait)
    return nc


def mix1_consts(S):
    ident = np.eye(128, dtype=np.float32)
    k_idx = np.arange(128)[:, None]; q_idx = np.arange(128)[None, :]
    negm = np.where(k_idx > q_idx, NEG, 0.0).astype(np.float32)
    ka = np.zeros((4, S), np.float32)
    t = np.arange(S)
    ka[0] = 1.0; ka[1] = t % 128; ka[2] = t // 128; ka[3] = 1.0
    return ident, negm, ka


def alibi_q_rows(slopes):
    nh = len(slopes)
    qab = np.zeros((4, nh, 128), np.float32); qad = np.zeros((4, nh, 128), np.float32)
    for h, s in enumerate(slopes):
        qab[0, h] = -s * np.arange(128); qab[1, h] = s; qab[2, h] = 128.0 * s
        qad[3, h] = -128.0 * s
    return qab.reshape(4, nh * 128), qad.reshape(4, nh * 128)


def slopes8():
    return [2.0 ** (-8.0 * (h + 1) / 8) for h in range(8)]


def prep_mix1(inp, layer, x_b, c_b, hh, S):
    o = layer // 2
    w = inp["od_w_in"][o]
    cols = []
    for j in range(4):
        h = 4 * hh + j
        cols.append(w[:, h * 128:(h + 1) * 128])
        cols.append(w[:, 1024 + h * 128:1024 + (h + 1) * 128])
        cols.append(w[:, 2048 + h * 128:2048 + (h + 1) * 128])
    win = np.ascontiguousarray(np.concatenate(cols, axis=1))
    ident, negm, ka = mix1_consts(S)
    sl = slopes8()[4 * hh:4 * hh + 4]
    qab, qad = alibi_q_rows(sl)
    qn = inp["od_qn_g"][o]; kn = inp["od_kn_g"][o]
    gqk = np.concatenate([qn, qn, kn, kn])[None, :].astype(np.float32)
    import math
    lam_init = 0.8 - 0.6 * math.exp(-0.3 * layer)
    return {
        "x": None if x_b is None else np.ascontiguousarray(x_b), "c": np.ascontiguousarray(c_b.reshape(8, 128).T),
        "adaw": np.ascontiguousarray(inp["ada_w"][layer][:, 0:2048]), "adab": np.ascontiguousarray(inp["ada_b"][layer][None, 0:2048]),
        "ng": np.ascontiguousarray(inp["norm_mix_g"][layer][None, :]), "win": win, "gqk": gqk,
        "lam": np.ascontiguousarray(inp["od_lam"][o].reshape(1, 256)), "ong": np.ascontiguousarray(inp["od_onorm_g"][o][None, :]),
        "ident": ident, "negm": negm, "qab": qab, "qad": qad, "ka": ka,
        "laminit": np.array([[lam_init, 1.0 - lam_init]], np.float32),
    }


def build_ffn(NTOK, nc=None, P=None, pfx="", over=None):
    over = over or {}
    standalone = nc is None
    NST = NTOK // 512
    if standalone:
        nc = bass.Bass("TRN2", target_bir_lowering=False)
    dt = lambda n, s: over[n] if n in over else nc.dram_tensor(pfx + n, s, F32, kind="ExternalInput").ap()
    yg = over.get("yg"); SEQ = 2 * NTOK
    x_d = None if "xtile" in over else dt("x", [NTOK, D])
    c_d = dt("c", [128, 8])
    y_d = dt("y", [NTOK, D]) if yg is None else None
    hsel_d = dt("hsel", [128, 2]) if yg is not None else None
    adaw_d = dt("adaw", [D, 4096]); adab_d = dt("adab", [1, 4096]); ng_d = dt("ng", [1, D])
    wout_d = dt("wout", [D, D]); wq_d = dt("wq", [D, 2048]); keysT_d = dt("keysT", [128, 256])
    uT_d = dt("uT", [D, 16384]); v_d = dt("v", [16384, D]); ident_d = dt("ident", [128, 128])
    o_d = None if "otile" in over else nc.dram_tensor("o", [NTOK, D], F32, kind="ExternalOutput").ap()
    with ExitStack() as st:
        if P is None:
            P = Prog(nc, st)
        P.st = st
        idf, idb = load_consts(P, ident_d)
        banks = [P.ps("bk%d" % i, [128, 512], F32) for i in range(7)]
        psT = P.ps("psT", [128, 1024], BF16)
        mod = compute_mod(P, c_d, adaw_d, adab_d, 4096, banks[0], "m")
        GM = P.sb("GM", [128, D], F32)
        P.dma("sp", GM[:], ng_d[0:1, :].partition_broadcast(128), writes=["GM"])
        P.stt(GM[:], mod[:, 2048:3072], 1.0, GM[:], ALU.add, ALU.mult, reads=["mmod", "GM"], writes=["GM"])
        G1 = mod[:, 0:1024]; SH = mod[:, 1024:2048]; G2 = mod[:, 3072:4096]
        gk = ["GM", "mmod"]
        keysb = P.sb("keysb", [128, 256], BF16)
        P.dma("pool", keysb[:], keysT_d[:, :], writes=["keysb"])
        W = [P.sb("W%d" % i, [128, 8, 512], BF16) for i in range(3)]
        Vc = [P.sb("Vc%d" % i, [128, 4, 1024], BF16) for i in range(2)]
        wn = [0]; vn = [0]

        def loadW(src):
            i = wn[0] % 3; wn[0] += 1
            P.dma("pool", W[i][:], src.rearrange("(c p) n -> p c n", p=128), writes=["W%d" % i])
            return W[i], "W%d" % i

        xt = P.sb("xt", [128, D], F32)
        if yg is not None:
            ysf = P.sb("ysf", [128, 4, 512], F32)
            hsel = P.sb("hsel_s", [128, 2], F32)
            P.dma("sp", hsel[:], hsel_d[:, :], writes=["hsel"])
        yb = P.sb("yb", [128, D], BF16)
        yT = P.sb("yT", [128, 8, 128], BF16)
        junk = P.sb("junk", [128, D], F32)
        hb = P.sb("hb", [128, D], BF16)
        small = P.sb("small", [128, 8], F32)
        x1 = [P.sb("x1_%d" % t, [128, D], F32) for t in range(4)]
        h2T = [P.sb("h2T_%d" % t, [128, 8, 128], BF16) for t in range(4)]
        qTb = [P.sb("qTb_%d" % t, [128, 16, 128], BF16) for t in range(4)]
        sT = [P.sb("sT_%d" % t, [128, 16, 128], BF16) for t in range(4)]
        thr = [P.sb("thr_%d" % t, [128, 8], F32) for t in range(4)]
        nb = [P.sb("nb_%d" % t, [128, 8], F32) for t in range(4)]
        acc = [P.sb("acc_%d" % t, [128, D], F32) for t in range(4)]
        sbf = P.sb("sbf", [128, 16, 128], BF16)
        t16 = P.sb("t16", [128, 16, 16], F32)
        tmp128 = P.sb("tmp128", [128, 128], BF16)
        cand = P.sb("cand", [128, 8, 256], F32)
        cand2 = P.sb("cand2", [128, 256], F32)
        c16 = P.sb("c16", [128, 8, 16], F32)
        e16 = P.sb("e16", [128, 8, 16], F32)
        zs = P.sb("zs", [128, 8], F32)
        ef = [P.sb("ef%d" % i, [128, 512], F32) for i in range(3)]
        wb = [P.sb("wbm%d" % i, [128, 512], BF16) for i in range(8)]
        ethr = [P.sb("ethr_%d" % t, [128, 8], F32) for t in range(4)]
        gh = [P.sb("gh%d" % i, [128, 512], F32) for i in range(2)]
        Ab = [P.sb("Ab%d" % i, [128, 512], BF16) for i in range(2)]
        AT = [P.sb("AT%d" % i, [128, 4, 128], BF16) for i in range(2)]
        ot = junk
        psZ = [banks[0], banks[1], banks[6]]; psG = banks[2]; psH = banks[3]; psO = [banks[4], banks[5]]; zc = [0]
        sel1 = lambda c: idb[:, 4 * c:4 * c + 4].unsqueeze(2).broadcast_to([128, 4, 128])
        sel2 = idb[:, :].unsqueeze(1).broadcast_to([128, 4, 128])
        cnt = [0]
        for stile in range(NST):
            t0 = stile * 512
            wo = [loadW(wout_d[:, hf * 512:(hf + 1) * 512]) for hf in range(2)]
            for t in range(4):
                r0 = t0 + t * 128
                if yg is None:
                    P.dma("pool", yb[:], y_d[r0:r0 + 128, :], writes=["yb"])
                else:
                    for r in range(2):
                        for q in range(2):
                            P.dma("sp", ysf[:, r * 2 + q, :], yg(r, q, r0), writes=["ysf"])
                    for r in range(2):
                        P.ts(junk[:, r * 512:(r + 1) * 512], ysf[:, r * 2, :], hsel[:, 0:1], None, ALU.mult, None,
                             reads=["ysf", "hsel"], writes=["junk"])
                        P.stt(yb[:, r * 512:(r + 1) * 512], ysf[:, r * 2 + 1, :], hsel[:, 1:2], junk[:, r * 512:(r + 1) * 512],
                              ALU.mult, ALU.add, reads=["ysf", "hsel", "junk"], writes=["yb"])
                P.dma("sp", xt[:], over["xtile"](r0) if "xtile" in over else x_d[r0:r0 + 128, :], writes=["xt"])
                for c in range(8):
                    P.tr(psT[:, c * 128:(c + 1) * 128], yb[:, c * 128:(c + 1) * 128], idb[:], reads=["yb", "idb"], writes=[psT.name])
                P.copy("act", yT[:].rearrange("p c t -> p (c t)"), psT[:, :], reads=[psT.name], writes=["yT"])
                for hf in range(2):
                    bk = psO[hf]
                    for c in range(8):
                        P.mm(bk[:, :], yT[:, c, :], wo[hf][0][:, c, :], c == 0, c == 7, reads=["yT", wo[hf][1]], writes=[bk.name])
                    P.tt(junk[:, hf * 512:(hf + 1) * 512], bk[:, :], G1[:, hf * 512:(hf + 1) * 512], ALU.mult,
                         reads=[bk.name, "mmod"], writes=["junk"])
                P.tt(x1[t][:], junk[:], xt[:], ALU.add, reads=["junk", "xt"], writes=["x1_%d" % t])
                norm_mod_T(P, x1[t][:], "x1_%d" % t, GM, SH, gk, hb, h2T[t], psT, idb, junk, small, "", act_copy=True)
                P.lastw["h2T_%d" % t] = P.lastw["hT"]
            for g in range(4):
                wq, wqk = loadW(wq_d[:, g * 512:(g + 1) * 512])
                for t in range(4):
                    bk = psZ[(g * 4 + t) % 2]
                    for hp in range(4):
                        for c in range(8):
                            P.mm(bk[:, hp * 128:(hp + 1) * 128], wq[:, c, hp * 128:(hp + 1) * 128], h2T[t][:, c, :], c == 0, c == 7,
                                 reads=[wqk, "h2T_%d" % t], writes=[bk.name])
                    P.copy("act", qTb[t][:, 4 * g:4 * g + 4, :].rearrange("p a t -> p (a t)"), bk[:, :], reads=[bk.name], writes=["qTb_%d" % t])
            for t in range(4):
                for g in range(4):
                    bk = psZ[g % 2]
                    for a in range(4):
                        hp = 4 * g + a
                        P.mm(bk[:, a * 128:(a + 1) * 128], qTb[t][:, hp, :], keysb[:, (hp % 2) * 128:(hp % 2 + 1) * 128], True, True,
                             reads=["qTb_%d" % t, "keysb"], writes=[bk.name])
                    P.copy("act", sbf[:, 4 * g:4 * g + 4, :].rearrange("p a n -> p (a n)"), bk[:, :], reads=[bk.name], writes=["sbf"])
                for hp in range(16):
                    P.op("dve", lambda: nc.vector.max(out=t16[:, hp, 0:8], in_=sbf[:, hp, :]), reads=["sbf"], writes=["t16"])
                    P.op("dve", lambda: nc.vector.match_replace(out=tmp128[:], in_to_replace=t16[:, hp, 0:8], in_values=sbf[:, hp, :], imm_value=-1e30),
                         reads=["sbf", "t16"], writes=["tmp128"])
                    P.op("dve", lambda: nc.vector.max(out=t16[:, hp, 8:16], in_=tmp128[:]), reads=["tmp128"], writes=["t16"])
                tv = t16[:].rearrange("p (h two) k -> p h two k", two=2)
                for h in range(8):
                    P.tt(cand[:, h, :].rearrange("p (a b) -> p a b", a=16), tv[:, h, 0, :].unsqueeze(2).broadcast_to([128, 16, 16]),
                         tv[:, h, 1, :].unsqueeze(1).broadcast_to([128, 16, 16]), ALU.add, reads=["t16"], writes=["cand"])
                for h in range(8):
                    P.op("dve", lambda: nc.vector.max(out=c16[:, h, 0:8], in_=cand[:, h, :]), reads=["cand"], writes=["c16"])
                    P.op("dve", lambda: nc.vector.match_replace(out=cand2[:], in_to_replace=c16[:, h, 0:8], in_values=cand[:, h, :], imm_value=-1e30),
                         reads=["cand", "c16"], writes=["cand2"])
                    P.op("dve", lambda: nc.vector.max(out=c16[:, h, 8:16], in_=cand2[:]), reads=["cand2"], writes=["c16"])
                P.copy("dve", thr[t][:], c16[:, :, 15], reads=["c16"], writes=["thr_%d" % t])
                P.tt(e16[:], c16[:], c16[:, :, 0:1].broadcast_to([128, 8, 16]), ALU.subtract, reads=["c16"], writes=["e16"])
                P.act(e16[:], e16[:], AF.Exp, reads=["e16"], writes=["e16"])
                P.op("dve", lambda: nc.vector.tensor_reduce(out=zs[:], in_=e16[:], axis=AX.X, op=ALU.add), reads=["e16"], writes=["zs"])
                P.act(zs[:], zs[:], AF.Ln, reads=["zs"], writes=["zs"])
                P.tt(zs[:], zs[:], c16[:, :, 0], ALU.add, reads=["zs", "c16"], writes=["zs"])
                P.ts(nb[t][:], zs[:], -1.0, None, ALU.mult, None, reads=["zs"], writes=["nb_%d" % t])
                P.tt(ethr[t][:], thr[t][:], nb[t][:], ALU.add, reads=["thr_%d" % t, "nb_%d" % t], writes=["ethr_%d" % t])
                P.act(ethr[t][:], ethr[t][:], AF.Exp, reads=["ethr_%d" % t], writes=["ethr_%d" % t])
                for half in range(2):
                    for a in range(8):
                        P.tr(psT[:, a * 128:(a + 1) * 128], sbf[:, half * 8 + a, :], idb[:], reads=["sbf", "idb"], writes=[psT.name])
                    P.copy("act", sT[t][:, half * 8:half * 8 + 8, :].rearrange("p a t -> p (a t)"), psT[:, :], reads=[psT.name], writes=["sT_%d" % t])
            def tail(pu):
                pk, pt_, pvi, pch = pu
                for es in range(4):
                    P.tr(psT[:, es * 128:(es + 1) * 128], Ab[pk][:, es * 128:(es + 1) * 128], idb[:], reads=["Ab%d" % pk, "idb"], writes=[psT.name])
                P.copy("act", AT[pk][:].rearrange("p a t -> p (a t)"), psT[:, 0:512], reads=[psT.name], writes=["AT%d" % pk])

            def tail2(pu):
                pk, pt_, pvi, pch = pu
                for hf in range(2):
                    for es in range(4):
                        P.mm(psO[hf][:, :], AT[pk][:, es, :], Vc[pvi][:, es, hf * 512:(hf + 1) * 512], es == 0, es == 3,
                             reads=["AT%d" % pk, "Vc%d" % pvi], writes=[psO[hf].name])
                    if pch == 0:
                        P.copy("dve", acc[pt_][:, hf * 512:(hf + 1) * 512], psO[hf][:, :], reads=[psO[hf].name], writes=["acc_%d" % pt_])
                    else:
                        P.tt(acc[pt_][:, hf * 512:(hf + 1) * 512], acc[pt_][:, hf * 512:(hf + 1) * 512], psO[hf][:, :], ALU.add,
                             reads=[psO[hf].name, "acc_%d" % pt_], writes=["acc_%d" % pt_])

            prev = None
            for ch in range(32):
                uw, uk = loadW(uT_d[:, ch * 512:(ch + 1) * 512])
                vi = vn[0] % 2; vn[0] += 1
                P.dma("pool", Vc[vi][:], v_d[ch * 512:(ch + 1) * 512, :].rearrange("(a p) n -> p a n", p=128), writes=["Vc%d" % vi])
                for t in range(4):
                    k = cnt[0] % 2
                    for h in range(8):
                        bz = psZ[zc[0] % 3]; ei = zc[0] % 3; zc[0] += 1
                        P.mm(bz[:, :], sT[t][:, 2 * h, :], sel1(ch), True, False, reads=["sT_%d" % t, "idb"], writes=[bz.name])
                        P.mm(bz[:, :], sT[t][:, 2 * h + 1, :], sel2, False, True, reads=["sT_%d" % t, "idb"], writes=[bz.name])
                        P.act(ef[ei][:], bz[:, :], AF.Exp, reads=[bz.name, "nb_%d" % t], writes=["ef%d" % ei], bias=nb[t][:, h:h + 1], scale=1.0)
                        P.stt(wb[h][:], ef[ei][:], ethr[t][:, h:h + 1], ef[ei][:], ALU.is_ge, ALU.mult,
                              reads=["ef%d" % ei, "ethr_%d" % t], writes=["wbm%d" % h])
                    if prev is not None:
                        tail(prev)
                    for c in range(8):
                        P.mm(psH[:, :], h2T[t][:, c, :], uw[:, c, :], c == 0, c == 7, reads=["h2T_%d" % t, uk], writes=[psH.name])
                    P.act(gh[k][:], psH[:, :], AF.Gelu, reads=[psH.name], writes=["gh%d" % k])
                    for h in range(8):
                        P.mm(psG[:, :], idb[:], wb[h][:], h == 0, h == 7, reads=["idb", "wbm%d" % h], writes=[psG.name])
                    P.tt(Ab[k][:], psG[:, :], gh[k][:], ALU.mult, reads=[psG.name, "gh%d" % k], writes=["Ab%d" % k])
                    if prev is not None:
                        tail2(prev)
                    prev = (k, t, vi, ch)
                    cnt[0] += 1
            tail(prev); tail2(prev)
            for t in range(4):
                r0 = t0 + t * 128
                P.tt(ot[:], acc[t][:], G2, ALU.mult, reads=["acc_%d" % t, "mmod"], writes=["junk"], e="pool")
                P.tt(ot[:], ot[:], x1[t][:], ALU.add, reads=["junk", "x1_%d" % t], writes=["junk"], e="pool")
                P.dma("sp", over["otile"](r0) if "otile" in over else o_d[r0:r0 + 128, :], ot[:], reads=["junk"], writes=["oout"])
        for jx in range(len(P.dsem) if standalone else 0):
            if P.dcnt[jx] > 0:
                P._wait("sp", ("D%d" % jx, P.dsem[jx], 16 * P.dcnt[jx]))
        print("ffn ninst", P.ninst, "nwait", P.nwait)
    return nc


def prep_ffn(inp, layer, x_rows, y_rows, c_b, wout):
    return {
        "x": None if x_rows is None else np.ascontiguousarray(x_rows), "y": None if y_rows is None else np.ascontiguousarray(y_rows), "c": np.ascontiguousarray(c_b.reshape(8, 128).T),
        "adaw": np.ascontiguousarray(inp["ada_w"][layer][:, 2048:6144]), "adab": np.ascontiguousarray(inp["ada_b"][layer][None, 2048:6144]),
        "ng": np.ascontiguousarray(inp["norm_ffn_g"][layer][None, :]), "wout": np.ascontiguousarray(wout),
        "wq": np.ascontiguousarray(inp["peer_wq"][layer]),
        "keysT": np.ascontiguousarray(np.concatenate([inp["peer_keys"][layer][0].T, inp["peer_keys"][layer][1].T], axis=1)),
        "uT": np.ascontiguousarray(inp["peer_u"][layer].T), "v": np.ascontiguousarray(inp["peer_v"][layer]),
        "ident": np.eye(128, dtype=np.float32),
    }


def build_mix0(S, nc=None, P=None, pfx="", over=None):
    over = over or {}
    standalone = nc is None
    NT = S // 128
    if standalone:
        nc = bass.Bass("TRN2", target_bir_lowering=False)
    dt = lambda n, s: over[n] if n in over else nc.dram_tensor(pfx + n, s, F32, kind="ExternalInput").ap()
    x_d = dt("x", [S, D]); c_d = dt("c", [128, 8]); adaw_d = dt("adaw", [D, 2048]); adab_d = dt("adab", [1, 2048])
    ng_d = dt("ng", [1, D]); win_d = dt("win", [D, 1796]); convw_d = dt("convw", [128, 16]); gb_d = dt("gb", [1, 4])
    mg_d = dt("mg", [1, 256]); gqk_d = dt("gqk", [1, 512]); ident_d = dt("ident", [128, 128]); negm_d = dt("negm", [128, 128])
    posm_d = dt("posm", [128, 128]); trile_d = dt("trile", [128, 128]); sgt_d = dt("sgt", [128, 128])
    qab_d = dt("qab", [4, 4 * 128]); qad_d = dt("qad", [4, 4 * 128]); ka_d = dt("ka", [4, S]); kblk_d = dt("kblk", [32, S])
    fut_d = dt("fut", [33, 32]); own_d = dt("own", [33, 32])
    y_d = None if "ytile" in over else nc.dram_tensor("y", [S, 512], F32, kind="ExternalOutput").ap()
    with ExitStack() as st:
        if P is None:
            P = Prog(nc, st)
        P.st = st
        idf, idb = load_consts(P, ident_d)
        banks = [P.ps("bk%d" % i, [128, 512], F32) for i in range(7)]
        psT = P.ps("psT", [128, 1024], BF16)
        mod = compute_mod(P, c_d, adaw_d, adab_d, 2048, banks[0], "m")
        GM = P.sb("GM", [128, D], F32)
        P.dma("sp", GM[:], ng_d[0:1, :].partition_broadcast(128), writes=["GM"])
        P.stt(GM[:], mod[:, 1024:2048], 1.0, GM[:], ALU.add, ALU.mult, reads=["mmod", "GM"], writes=["GM"])
        SH = mod[:, 0:1024]
        gk = ["GM", "mmod"]
        cf = lambda name, src: (lambda t: (P.dma("sp", t[:], src, writes=[name]), t)[1])(P.sb(name, [128, 128], F32))
        negb = P.sb("negb", [128, 128], BF16)
        P.dma("pool", negb[:], negm_d[:, :], writes=["negb"])
        posm = cf("posm_s", posm_d[:, :]); trile = cf("trile_s", trile_d[:, :]); sgt = cf("sgt_s", sgt_d[:, :])
        onesf = P.sb("onesf", [128, 128], F32)
        P.memset("pool", onesf[:], 1.0, writes=["onesf"])
        convw = P.sb("convw_s", [128, 4, 4], F32)
        P.dma("sp", convw[:].rearrange("p a j -> p (a j)"), convw_d[:, :], writes=["convw"])
        GB = P.sb("GB", [128, 4], F32)
        P.dma("sp", GB[:], gb_d[0:1, :].partition_broadcast(128), writes=["GB"])
        MG = P.sb("MG", [128, 256], F32)
        P.dma("sp", MG[:], mg_d[0:1, :].partition_broadcast(128), writes=["MG"])
        Gqk = P.sb("Gqk", [128, 512], F32)
        P.dma("sp", Gqk[:], gqk_d[0:1, :].partition_broadcast(128), writes=["Gqk"])
        P.ts(Gqk[:, 0:256], Gqk[:, 0:256], 0.125, None, ALU.mult, None, reads=["Gqk"], writes=["Gqk"])
        wsb = P.sb("wsb", [128, 8, 1796], BF16)
        for c in range(8):
            P.dma("pool", wsb[:, c, :], win_d[c * 128:(c + 1) * 128, :], writes=["wsb"])
        KT = P.sb("KT", [100, 4, S], BF16)
        QT = P.sb("QT", [100, 4, 128], BF16)
        QTf = P.sb("QTf", [64, 4, 128], F32)
        qab = P.sb("qab_s", [100, 4, 128], F32)
        qad = P.sb("qad_s", [100, 4, 128], F32)
        P.dma("sp", qab[96:100, :, :], qab_d.rearrange("r (h t) -> r h t", h=4), writes=["qab"])
        P.dma("sp", qad[96:100, :, :], qad_d.rearrange("r (h t) -> r h t", h=4), writes=["qad"])
        for a in range(4):
            P.dma("pool", KT[64:96, a, :], kblk_d[:, :], writes=["KTaug"])
            P.dma("pool", KT[96:100, a, :], ka_d[:, :], writes=["KTaug"])
        Vm = P.sb("Vm", [128, NT, 4, 65], BF16)
        P.memset("pool", Vm[:, :, :, 64:65], 1.0, writes=["Vones"])
        kmT = P.sb("kmT", [64, 4, 32], F32)
        P.memset("pool", kmT[:], 0.0, writes=["kmT"])
        Bw = P.sb("Bw", [128, 4, 96], F32)
        P.memset("pool", Bw[:], 0.0, writes=["Bw"])
        FUT = P.sb("FUT", [128, 32], F32); OWN = P.sb("OWN", [128, 32], F32)
        xts = [P.sb("xt%d" % i, [128, D], F32) for i in range(2)]
        junk = P.sb("junk", [128, D], F32)
        hb = P.sb("hb", [128, D], BF16)
        hT = P.sb("hT", [128, 8, 128], BF16)
        small = P.sb("small", [128, 40], F32)
        cbuf = P.sb("cbuf", [128, 4, 131], F32)
        P.memset("pool", cbuf[:], 0.0, writes=["cbuf"])
        cacc = P.sb("cacc", [128, 4, 128], F32)
        ctmp = P.sb("ctmp", [128, 4, 128], F32)
        qkT = P.sb("qkT", [128, 4, 128], BF16)
        gi = P.sb("gi", [128, 8], F32)
        Lm = P.sb("Lm", [128, 128], F32)
        DT = P.sb("DT", [128, 128], F32)
        SmT = P.sb("SmT", [128, 128], BF16)
        vaug = P.sb("vaug", [128, 2, 129], BF16)
        P.memset("pool", vaug[:, :, 128:129], 1.0, writes=["vaug1"])
        kk = P.sb("kk", [128, 128], BF16)
        intra = P.sb("intra", [128, 129], F32)
        num = P.sb("num", [128, 129], F32)
        hm = P.sb("hm", [128, 128], F32)
        sig = P.sb("sig", [128, 128], F32)
        Cf = [P.sb("Cf%d" % m, [128, 129], F32) for m in range(2)]
        Cb = [P.sb("Cb%d" % m, [128, 129], BF16) for m in range(2)]
        for m in range(2):
            P.memset("pool", Cf[m][:], 0.0, writes=["Cf%d" % m])
            P.memset("pool", Cb[m][:], 0.0, writes=["Cb%d" % m])
        sq = P.sb("sq", [128, 512], F32)
        qkn = P.sb("qkn", [128, 512], F32)
        gm = P.sb("gm", [128, 32], F32)
        top8 = P.sb("top8", [128, 8], F32)
        PT = [P.sb("PT%d" % b, [128, 512], BF16) for b in range(2)]
        ymix = [P.sb("ymix%d" % b, [128, 512], F32) for b in range(2)]
        bkA, bkB, bkC, bkD, bkE, bkF, bkG = banks
        for i in range(NT):
            blk = i // 2
            xt = xts[i % 2]; xk = "xt%d" % (i % 2)
            ym = ymix[i % 2]; yk = "ymix%d" % (i % 2)
            P.dma("sp", xt[:], x_d[i * 128:(i + 1) * 128, :], writes=[xk])
            if i % 2 == 0:
                P.dma("sp", FUT[:], fut_d[blk:blk + 1, :].partition_broadcast(128), writes=["FUT"])
                P.dma("sp", OWN[:], own_d[blk:blk + 1, :].partition_broadcast(128), writes=["OWN"])
            norm_mod_T(P, xt[:], xk, GM, SH, gk, hb, hT, psT, idb, junk, small, "", act_copy=True)
            for a in range(4):
                for c in range(8):
                    P.mm(bkD[:, a * 128:(a + 1) * 128], wsb[:, c, a * 128:(a + 1) * 128], hT[:, c, :], c == 0, c == 7,
                         reads=["wsb", "hT"], writes=[bkD.name])
            for bk, c0, n in ((bkA, 512, 512), (bkB, 1024, 512), (bkC, 1536, 260)):
                for c in range(8):
                    P.mm(bk[:, 0:n], hT[:, c, :], wsb[:, c, c0:c0 + n], c == 0, c == 7, reads=["wsb", "hT"], writes=[bk.name])
            P.copy("pool", cbuf[:, :, 0:3], cbuf[:, :, 128:131], reads=["cbuf"], writes=["cbuf"])
            P.copy("act", cbuf[:, :, 3:131], bkD[:, :].rearrange("p (a t) -> p a t", a=4), reads=[bkD.name], writes=["cbuf"])
            for j in range(4):
                dst = cacc if j == 0 else ctmp
                P.tt(dst[:], cbuf[:, :, j:j + 128], convw[:, :, j:j + 1].broadcast_to([128, 4, 128]), ALU.mult,
                     reads=["cbuf", "convw"], writes=["cacc" if j == 0 else "ctmp"])
                if j > 0:
                    P.tt(cacc[:], cacc[:], ctmp[:], ALU.add, reads=["cacc", "ctmp"], writes=["cacc"])
            P.act(ctmp[:], cacc[:], AF.Silu, reads=["cacc"], writes=["ctmp"])
            P.copy("dve", qkT[:, 0:2, :], ctmp[:, 0:2, :], reads=["ctmp"], writes=["qkT"])
            P.ts(qkT[:, 2:4, :], ctmp[:, 2:4, :], 128.0 ** -0.5, None, ALU.mult, None, reads=["ctmp"], writes=["qkT"])
            P.tt(gi[:, 0:4], bkC[:, 256:260], GB[:], ALU.add, reads=[bkC.name, "GB"], writes=["gi"])
            P.act(gi[:, 4:6], gi[:, 2:4], AF.Exp, reads=["gi"], writes=["gi"], scale=-1.0)
            P.ts(gi[:, 4:6], gi[:, 4:6], 1.0, None, ALU.add, None, reads=["gi"], writes=["gi"])
            P.act(gi[:, 6:8], gi[:, 4:6], AF.Ln, reads=["gi"], writes=["gi"])
            nfl = gi[:, 6:8]
            P.mm(bkD[:, 0:2], trile[:], nfl, True, True, reads=["trile_s", "gi", "cbuf"], writes=[bkD.name])
            P.mm(bkD[:, 2:4], onesf[:], nfl, True, True, reads=["onesf", "gi"], writes=[bkD.name])
            P.act(small[:, 4:8], bkD[:, 0:4], AF.Exp, reads=[bkD.name], writes=["wd"], scale=-1.0)
            P.copy("act", vaug[:, :, 0:128], bkA[:, 0:256].rearrange("p (m d) -> p m d", m=2), reads=[bkA.name], writes=["vaug"])
            for m in range(2):
                P.ts(Lm[:], sgt[:], nfl[:, m:m + 1], None, ALU.mult, None, reads=["sgt_s", "gi"], writes=["Lm"])
                P.mm(bkE[:, 0:128], Lm[:], trile[:], True, False, reads=["Lm", "trile_s"], writes=[bkE.name])
                P.mm(bkE[:, 0:128], idf[:], posm[:], False, True, reads=["idf", "posm_s"], writes=[bkE.name])
                P.act(DT[:], bkE[:, 0:128], AF.Exp, reads=[bkE.name, "gi"], writes=["DT"], bias=gi[:, m:m + 1], scale=-1.0)
                P.mm(bkE[:, 128:256], qkT[:, 2 + m, :], qkT[:, m, :], True, True, reads=["qkT"], writes=[bkE.name])
                P.tt(SmT[:], bkE[:, 128:256], DT[:], ALU.mult, reads=[bkE.name, "DT"], writes=["SmT"])
                P.tr(psT[:, 0:128], qkT[:, 2 + m, :], idb[:], reads=["qkT", "idb"], writes=[psT.name])
                P.ts(kk[:], psT[:, 0:128], DT[:, 127:128], None, ALU.mult, None, reads=[psT.name, "DT"], writes=["kk"])
                P.mm(bkF[:, 0:129], qkT[:, m, :], Cb[m][:], True, True, reads=["qkT", "Cb%d" % m], writes=[bkF.name])
                P.mm(bkF[:, 136:265], SmT[:], vaug[:, m, :], True, True, reads=["SmT", "vaug", "vaug1"], writes=[bkF.name])
                P.mm(bkF[:, 272:401], kk[:], vaug[:, m, :], True, True, reads=["kk", "vaug", "vaug1"], writes=[bkF.name])
                P.copy("act", intra[:], bkF[:, 136:265], reads=[bkF.name], writes=["intra"])
                P.stt(num[:], bkF[:, 0:129], small[:, 4 + m:5 + m], intra[:], ALU.mult, ALU.add, reads=[bkF.name, "wd", "intra"], writes=["num"])
                P.stt(Cf[m][:], Cf[m][:], small[:, 6 + m:7 + m], bkF[:, 272:401], ALU.mult, ALU.add,
                      reads=["Cf%d" % m, "wd", bkF.name], writes=["Cf%d" % m])
                P.copy("pool", Cb[m][:], Cf[m][:], reads=["Cf%d" % m], writes=["Cb%d" % m])
                P.ts(small[:, 8:9], num[:, 128:129], 1.0, None, ALU.max, None, reads=["num"], writes=["den"])
                P.ts(small[:, 11:12], num[:, 128:129], -1.0, 1.0, ALU.mult, ALU.max, reads=["num"], writes=["den2"])
                P.tt(small[:, 8:9], small[:, 8:9], small[:, 11:12], ALU.max, reads=["den", "den2"], writes=["den"])
                P.op("dve", lambda: nc.vector.reciprocal(out=small[:, 8:9], in_=small[:, 8:9]), reads=["den"], writes=["den"])
                P.ts(hm[:], num[:, 0:128], small[:, 8:9], None, ALU.mult, None, reads=["num", "den"], writes=["hm"])
                P.act(sig[:], hm[:], AF.Square, reads=["hm"], writes=["sig", "ssm"], accum_out=small[:, 9:10])
                rstd_from_ss(P, small[:, 9:10], small[:, 10:11], 128, "ssm", "rstdm")
                P.act(sig[:], bkA[:, 256 + m * 128:256 + (m + 1) * 128], AF.Sigmoid, reads=[bkA.name], writes=["sig"])
                P.stt(hm[:], hm[:], small[:, 10:11], MG[:, m * 128:(m + 1) * 128], ALU.mult, ALU.mult, reads=["hm", "rstdm", "MG"], writes=["hm"])
                P.tt(ym[:, m * 128:(m + 1) * 128], hm[:], sig[:], ALU.mult, reads=["hm", "sig"], writes=[yk])
            P.act(sq[:], bkB[:, :], AF.Square, reads=[bkB.name], writes=["sq"])
            P.op("dve", lambda: nc.vector.tensor_reduce(out=small[:, 16:24], in_=sq[:].rearrange("p (g d) -> p g d", g=8), axis=AX.X, op=ALU.add),
                 reads=["sq"], writes=["ss8"])
            rstd_from_ss(P, small[:, 16:24], small[:, 24:32], 64, "ss8", "rstd8")
            P.tt(sq[:].rearrange("p (g d) -> p g d", g=8), bkB[:, :].rearrange("p (g d) -> p g d", g=8),
                 small[:, 24:32].unsqueeze(2).broadcast_to([128, 8, 64]), ALU.mult, reads=[bkB.name, "rstd8"], writes=["sq"])
            P.tt(qkn[:], sq[:], Gqk[:], ALU.mult, reads=["sq", "Gqk"], writes=["qkn"])
            P.copy("act", Vm[:, i, :, 0:64], bkC[:, 0:256].rearrange("p (a d) -> p a d", a=4), reads=[bkC.name], writes=["V%d" % i])
            for g in range(4):
                P.tr(bkB[0:64, g * 128:(g + 1) * 128], qkn[:, g * 64:(g + 1) * 64], idf[:], reads=["qkn", "idf"], writes=[bkB.name])
                P.tr(bkC[0:64, g * 128:(g + 1) * 128], qkn[:, 256 + g * 64:256 + (g + 1) * 64], idf[:], reads=["qkn", "idf"], writes=[bkC.name])
            P.copy("dve", QT[0:64, :, :], bkB[0:64, :].rearrange("p (a t) -> p a t", a=4), reads=[bkB.name], writes=["QT"])
            P.copy("dve", QTf[:], bkB[0:64, :].rearrange("p (a t) -> p a t", a=4), reads=[bkB.name], writes=["QTf"])
            P.copy("dve", KT[0:64, :, i * 128:(i + 1) * 128], bkC[0:64, :].rearrange("p (a t) -> p a t", a=4), reads=[bkC.name], writes=["K%d" % i])
            P.stt(QT[96:100, :, :], qad[96:100, :, :], float(i), qab[96:100, :, :], ALU.mult, ALU.add, reads=["qab", "qad"], writes=["QT"])
            for a in range(4):
                P.mm(bkG[:, a * 32:(a + 1) * 32], QTf[:, a, :], kmT[:, a, :], True, True, reads=["QTf", "kmT"], writes=[bkG.name])
            for a in range(4):
                P.tt(gm[:], bkG[:, a * 32:(a + 1) * 32], FUT[:], ALU.add, reads=[bkG.name, "FUT"], writes=["gm"])
                P.op("dve", lambda: nc.vector.max(out=top8[:], in_=gm[:]), reads=["gm"], writes=["top8"])
                P.ts(gm[:], gm[:], top8[:, 2:3], 30000.0, ALU.is_ge, ALU.mult, reads=["gm", "top8"], writes=["gm"])
                P.stt(Bw[:, a, 64:96], gm[:], -30000.0, OWN[:], ALU.add, ALU.max, reads=["gm", "OWN"], writes=["Bw"])
            for a in range(4):
                P.tr(bkB[0:96, a * 128:(a + 1) * 128], Bw[:, a, :], idf[:], reads=["Bw", "idf", "QT", "QTf"], writes=[bkB.name])
            P.copy("dve", QT[64:96, :, :], bkB[64:96, :].rearrange("p (a t) -> p a t", a=4), reads=[bkB.name], writes=["QT"])
            for a in range(4):
                P.mm(bkG[0:64, 128 + a:129 + a], qkn[:, 256 + a * 64:256 + (a + 1) * 64], onesf[:, 0:1], True, True,
                     reads=["qkn", "onesf"], writes=[bkG.name])
            if blk < 32:
                P.stt(kmT[:, :, blk], bkG[0:64, 128:132], 1.0 / 256.0, kmT[:, :, blk], ALU.mult, ALU.add, reads=[bkG.name, "kmT"], writes=["kmT"])
            ngrp = i // 4 + 1
            for a in range(4):
                for g in range(ngrp):
                    kts = list(range(4 * g, min(4 * g + 4, i + 1)))
                    n = len(kts)
                    bank = (bkE, bkF)[g % 2]
                    for jj, kt in enumerate(kts):
                        P.mm(bank[:, jj * 128:(jj + 1) * 128], KT[0:100, a, kt * 128:(kt + 1) * 128], QT[0:100, a, :],
                             True, kt != i, reads=["K%d" % kt, "KTaug", "QT"], writes=[bank.name])
                        if kt == i:
                            P.mm(bank[:, jj * 128:(jj + 1) * 128], idb[:], negb[:], False, True, reads=["idb", "negb"], writes=[bank.name])
                    pt = PT[g % 2]; pk = "PT%d" % (g % 2)
                    P.act(pt[:, 0:n * 128], bank[:, 0:n * 128], AF.Exp, reads=[bank.name], writes=[pk])
                    for jj, kt in enumerate(kts):
                        P.mm(bkA[:, 0:65], pt[:, jj * 128:(jj + 1) * 128], Vm[:, kt, a, :], kt == 0, kt == i,
                             reads=[pk, "V%d" % kt, "Vones"], writes=[bkA.name])
                P.op("dve", lambda: nc.vector.reciprocal(out=small[:, 12:13], in_=bkA[:, 64:65]), reads=[bkA.name], writes=["rdb"])
                P.ts(ym[:, 256 + a * 64:256 + (a + 1) * 64], bkA[:, 0:64], small[:, 12:13], None, ALU.mult, None,
                     reads=[bkA.name, "rdb"], writes=[yk])
            P.dma("sp", over["ytile"](i) if "ytile" in over else y_d[i * 128:(i + 1) * 128, :], ym[:], reads=[yk], writes=["yout"])
        for jx in range(len(P.dsem) if standalone else 0):
            if P.dcnt[jx] > 0:
                P._wait("sp", ("D%d" % jx, P.dsem[jx], 16 * P.dcnt[jx]))
        print("mix0 ninst", P.ninst, "nwait", P.nwait)
    return nc


def prep_mix0(inp, layer, x_b, c_b, hh, S):
    e = layer // 2
    w = inp["ev_w_in"][e]
    mh = [2 * hh, 2 * hh + 1]; bh = [4 * hh + a for a in range(4)]
    cols = []
    for base in (0, 512):
        for m in mh:
            cols.append(w[:, base + m * 128:base + (m + 1) * 128])
    for base in (1024, 1536):
        for m in mh:
            cols.append(w[:, base + m * 128:base + (m + 1) * 128])
    for base in (2056, 2568):
        for a in bh:
            cols.append(w[:, base + a * 64:base + (a + 1) * 64])
    for a in bh:
        cols.append(w[:, 3080 + a * 64:3080 + (a + 1) * 64])
    cols.append(w[:, 2048 + mh[0]:2048 + mh[0] + 2])
    cols.append(w[:, 2052 + mh[0]:2052 + mh[0] + 2])
    win = np.ascontiguousarray(np.concatenate(cols, axis=1))
    cw = inp["ev_conv_w"][e]
    convw = np.zeros((128, 4, 4), np.float32)
    for ai, (base, m) in enumerate([(0, mh[0]), (0, mh[1]), (512, mh[0]), (512, mh[1])]):
        convw[:, ai, :] = cw[:, base + m * 128:base + (m + 1) * 128].T
    gb = np.concatenate([inp["ev_igate_b"][e][mh[0]:mh[0] + 2], inp["ev_fgate_b"][e][mh[0]:mh[0] + 2]])[None, :]
    mg = inp["ev_mnorm_g"][e][mh[0]:mh[0] + 2].reshape(1, 256)
    qn = inp["ev_qn_g"][e]; kn = inp["ev_kn_g"][e]
    gqk = np.concatenate([qn] * 4 + [kn] * 4)[None, :]
    ident, negm, ka = mix1_consts(S)
    sl = slopes8()[4 * hh:4 * hh + 4]
    qab, qad = alibi_q_rows(sl)
    li = np.arange(128)
    trile = (li[:, None] <= li[None, :]).astype(np.float32)
    sgt = (li[:, None] > li[None, :]).astype(np.float32)
    t = np.arange(S)
    kblk = (np.arange(32)[:, None] == (t // 256)[None, :]).astype(np.float32)
    nb = np.arange(32)
    fut = np.stack([np.where(nb >= b, NEG, 0.0) for b in range(33)]).astype(np.float32)
    own = np.stack([np.where(nb == b, 0.0, NEG) for b in range(33)]).astype(np.float32)
    f = np.ascontiguousarray
    return {
        "x": f(x_b), "c": f(c_b.reshape(8, 128).T), "adaw": f(inp["ada_w"][layer][:, 0:2048]), "adab": f(inp["ada_b"][layer][None, 0:2048]),
        "ng": f(inp["norm_mix_g"][layer][None, :]), "win": win, "convw": f(convw.reshape(128, 16)), "gb": f(gb.astype(np.float32)),
        "mg": f(mg), "gqk": f(gqk.astype(np.float32)), "ident": ident, "negm": negm, "posm": f(-negm), "trile": trile, "sgt": sgt,
        "qab": qab, "qad": qad, "ka": ka, "kblk": kblk, "fut": fut, "own": own,
    }


PAIRS = [[0, 1], [2, 3], [4, 5], [6, 7]]


def build_fused(S):
    NTOK = S // 2
    NCH = max(1, (S * 512 * 4) // (2 << 20))
    RY = S // NCH
    RX = NTOK // NCH
    nc = bass.Bass("TRN2", target_bir_lowering=False)
    dr = lambda n, shp: nc.dram_tensor(n, shp, F32).ap()
    y0c = [dr("y0c%d" % j, [RY, 512]) for j in range(NCH)]; yg0c = [dr("yg0c%d" % j, [2 * RY, 512]) for j in range(NCH)]
    y1c = [dr("y1c%d" % j, [RY, 512]) for j in range(NCH)]; yg1c = [dr("yg1c%d" % j, [2 * RY, 512]) for j in range(NCH)]
    x1c = [dr("x1c%d" % j, [RX, D]) for j in range(NCH)]; x1gc = [dr("x1gc%d" % j, [2 * RX, D]) for j in range(NCH)]

    def ytile(ch):
        return lambda i: ch[(i * 128) // RY][(i * 128) % RY:(i * 128) % RY + 128, :]

    def ygtile(ch):
        def f(r, q, r0):
            t = q * NTOK + r0
            return ch[t // RY][r * RY + t % RY:r * RY + t % RY + 128, :]
        return f

    def x1tile(r0):
        return x1c[r0 // RX][r0 % RX:r0 % RX + 128, :]

    def x1gtile(i):
        t = i * 128
        rank, loc = t // NTOK, t % NTOK
        return x1gc[loc // RX][rank * RX + loc % RX:rank * RX + loc % RX + 128, :]

    with ExitStack() as sems:
        Ps = [Prog(nc, None, 16, p, sems) for p in ("a_", "b_", "c_", "d_")]
        ccs = [sems.enter_context(nc.semaphore("cc%d" % i)) for i in range(3)]

        def exchange(Pprev, Pnext, srcs, dsts, cs):
            toks = Pprev.all_tokens()
            Pnext.wait_all(toks, engines=["pool"])
            for a_, d_ in zip(srcs, dsts):
                nc.gpsimd.collective_compute("AllGather", ALU.bypass, replica_groups=PAIRS, ins=[a_.opt()], outs=[d_.opt()]).then_inc(cs)
            Pnext.wait_all(toks + [("CC", cs, len(srcs))], engines=["pe", "act", "dve", "sp", "pool"])

        build_mix0(S, nc, Ps[0], "a_", over={"ytile": ytile(y0c)})
        exchange(Ps[0], Ps[1], y0c, yg0c, ccs[0])
        build_ffn(NTOK, nc, Ps[1], "b_", over={"yg": ygtile(yg0c), "otile": x1tile})
        exchange(Ps[1], Ps[2], x1c, x1gc, ccs[1])
        build_mix1(S, nc, Ps[2], "c_", over={"xtile": x1gtile, "ytile": ytile(y1c)})
        exchange(Ps[2], Ps[3], y1c, yg1c, ccs[2])
        build_ffn(NTOK, nc, Ps[3], "d_", over={"xtile": x1tile, "yg": ygtile(yg1c)})
        Ps[3].wait_all(Ps[3].all_tokens(), engines=["sp"])
        print("fused ninst", sum(p.ninst for p in Ps), "nwait", sum(p.nwait for p in Ps))
    return nc


_CACHE = {}


def _get(name, fn, *a):
    key = (name,) + a
    if key not in _CACHE:
        _CACHE[key] = fn(*a)
    return _CACHE[key]


def kernel(**inputs):
    inp = {k: np.asarray(v) for k, v in inputs.items()}
    x = inp["x"].astype(np.float32, copy=False)
    c = inp["c"]
    B, S, _ = x.shape
    NTOK = S // 2
    nc = _get("fused", build_fused, S)
    wo0 = inp["ev_w_out"][0]
    wo0p = np.concatenate([wo0[0:256], wo0[512:768], wo0[256:512], wo0[768:1024]], axis=0)
    maps = []
    for k in range(8):
        b, hh = k // 2, k % 2
        hsel = np.zeros((128, 2), np.float32); hsel[:, hh] = 1.0
        m = {}
        for pfx, d in (("a_", prep_mix0(inp, 0, x[b], c[b], hh, S)),
                       ("b_", prep_ffn(inp, 0, x[b, hh * NTOK:(hh + 1) * NTOK], None, c[b], wo0p)),
                       ("c_", prep_mix1(inp, 1, None, c[b], hh, S)),
                       ("d_", prep_ffn(inp, 1, None, None, c[b], inp["od_w_out"][0]))):
            for kk, v in d.items():
                if v is not None:
                    m[pfx + kk] = v
        m["b_hsel"] = hsel; m["d_hsel"] = hsel
        maps.append(m)
    res = run_bass_kernel_spmd(nc, maps, core_ids=list(range(8)))
    out = np.concatenate([res.results[k]["o"] for k in range(8)], axis=0).reshape(B, S, D)
    return out.astype(np.float32)
```

```python
import numpy as np
from contextlib import ExitStack
import concourse.bass as bass
import concourse.mybir as mybir
from concourse.bass_utils import run_bass_kernel_spmd

F32 = mybir.dt.float32
BF16 = mybir.dt.bfloat16
AF = mybir.ActivationFunctionType
ALU = mybir.AluOpType
AX = mybir.AxisListType

D = 1024
EPS = 1e-6
NEG = -30000.0
import os
STAGE = int(os.environ.get('KSTAGE', '9'))
SUB = int(os.environ.get('KSUB', '0'))
KTENG = os.environ.get('KTENG', 'dve')


class Prog:
    def __init__(self, nc, stack, n_dma_sems=24, pfx="", sem_stack=None):
        self.nc = nc
        self.st = stack
        self.pfx = pfx
        sem_stack = sem_stack if sem_stack is not None else stack
        self.eng = {"pe": nc.tensor, "act": nc.scalar, "dve": nc.vector, "pool": nc.gpsimd, "sp": nc.sync}
        self.sem = {k: sem_stack.enter_context(nc.semaphore(pfx + "s_" + k)) for k in self.eng}
        self.cnt = {k: 0 for k in self.eng}
        self.dsem = [sem_stack.enter_context(nc.semaphore(pfx + "d%d" % i)) for i in range(n_dma_sems)]
        self.dcnt = [0] * n_dma_sems
        self.dnext = 0
        self.waited = {k: {} for k in self.eng}
        self.lastw = {}
        self.readers = {}
        self.ninst = 0
        self.nwait = 0

    def sb(self, name, shape, dt):
        return self.st.enter_context(self.nc.sbuf_tensor(self.pfx + name, shape, dt))

    def ps(self, name, shape, dt):
        return self.st.enter_context(self.nc.psum_tensor(self.pfx + name, shape, dt))

    def all_tokens(self):
        toks = [("E" + e, self.sem[e], self.cnt[e]) for e in self.eng if self.cnt[e] > 0]
        toks += [("D%d" % j, self.dsem[j], 16 * self.dcnt[j]) for j in range(len(self.dsem)) if self.dcnt[j] > 0]
        return toks

    def wait_all(self, toks, engines=None):
        for e in (engines or self.eng):
            for sid, sm, v in toks:
                self.eng[e].wait_ge(sm, v)
                self.nwait += 1

    def _wait(self, e, tok):
        if tok is None:
            return
        sid, s, v = tok
        if self.waited[e].get(sid, 0) >= v:
            return
        self.eng[e].wait_ge(s, v)
        self.nwait += 1
        self.waited[e][sid] = v

    def _deps(self, e, reads, writes):
        for k in reads:
            self._wait(e, self.lastw.get(k))
        for k in writes:
            self._wait(e, self.lastw.get(k))
            for t in self.readers.get(k, ()):
                self._wait(e, t)

    def _commit(self, tok, reads, writes):
        for k in writes:
            self.lastw[k] = tok
            self.readers[k] = []
        for k in reads:
            lst = self.readers.setdefault(k, [])
            lst.append(tok)
            if len(lst) > 16:
                best = {}
                for t in lst:
                    if t[0] not in best or best[t[0]][2] < t[2]:
                        best[t[0]] = t
                self.readers[k] = list(best.values())

    def op(self, e, ins_fn, reads=(), writes=()):
        self._deps(e, reads, writes)
        ins = ins_fn()
        self.cnt[e] += 1
        ins.then_inc(self.sem[e], 1)
        tok = ("E" + e, self.sem[e], self.cnt[e])
        self.waited[e]["E" + e] = self.cnt[e] - 1
        self._commit(tok, reads, writes)
        self.ninst += 1
        return tok

    def dma(self, q, out, in_, reads=(), writes=(), **kw):
        j = self.dnext
        self.dnext = (self.dnext + 1) % len(self.dsem)
        if self.dcnt[j] > 0:
            self._wait(q, ("D%d" % j, self.dsem[j], 16 * self.dcnt[j]))
        self._deps(q, reads, writes)
        ins = self.eng[q].dma_start(out=out, in_=in_, **kw)
        self.dcnt[j] += 1
        ins.then_inc(self.dsem[j], 16)
        tok = ("D%d" % j, self.dsem[j], 16 * self.dcnt[j])
        self.waited[q]["D%d" % j] = 16 * (self.dcnt[j] - 1)
        self._commit(tok, reads, writes)
        self.ninst += 1
        return tok

    def finish(self, keys, e="sp"):
        for k in keys:
            self._wait(e, self.lastw.get(k))

    def mm(self, out, lhsT, rhs, start, stop, reads, writes):
        nc = self.nc
        return self.op("pe", lambda: nc.tensor.matmul(out, lhsT=lhsT, rhs=rhs, start=start, stop=stop),
                       reads=reads, writes=writes)

    def tr(self, out, in_, ident, reads, writes):
        nc = self.nc
        return self.op("pe", lambda: nc.tensor.transpose(out=out, in_=in_, identity=ident), reads=reads, writes=writes)

    def act(self, out, in_, func, reads, writes, **kw):
        nc = self.nc
        return self.op("act", lambda: nc.scalar.activation(out=out, in_=in_, func=func, **kw), reads=reads, writes=writes)

    def tt(self, out, in0, in1, op, reads, writes, e="dve"):
        eng = self.eng[e]
        return self.op(e, lambda: eng.tensor_tensor(out=out, in0=in0, in1=in1, op=op), reads=reads, writes=writes)

    def ts(self, out, in0, s1, s2, op0, op1, reads, writes, e="dve", **kw):
        eng = self.eng[e]
        if op1 is None:
            return self.op(e, lambda: eng.tensor_scalar(out=out, in0=in0, scalar1=s1, scalar2=None, op0=op0, **kw),
                           reads=reads, writes=writes)
        return self.op(e, lambda: eng.tensor_scalar(out=out, in0=in0, scalar1=s1, scalar2=s2, op0=op0, op1=op1, **kw),
                       reads=reads, writes=writes)

    def stt(self, out, in0, scalar, in1, op0, op1, reads, writes):
        nc = self.nc
        return self.op("dve", lambda: nc.vector.scalar_tensor_tensor(out=out, in0=in0, scalar=scalar, in1=in1, op0=op0, op1=op1),
                       reads=reads, writes=writes)

    def copy(self, e, out, in_, reads, writes):
        nc = self.nc
        if e == "act":
            return self.op("act", lambda: nc.scalar.copy(out=out, in_=in_), reads=reads, writes=writes)
        eng = self.eng[e]
        return self.op(e, lambda: eng.tensor_copy(out=out, in_=in_), reads=reads, writes=writes)

    def memset(self, e, ap, val, writes):
        eng = self.eng[e]
        return self.op(e, lambda: eng.memset(ap, val), writes=writes)


def load_consts(P, ident_d):
    idf = P.sb("idf", [128, 128], F32)
    idb = P.sb("idb", [128, 128], BF16)
    P.dma("sp", idf[:], ident_d[:, :], writes=["idf"])
    P.dma("pool", idb[:], ident_d[:, :], writes=["idb"])
    return idf, idb


def compute_mod(P, c_d, adaw_d, adab_d, ncols, ps_bank, name):
    nc = P.nc
    CW = 256
    ccol = P.sb(name + "_ccol", [128, 8], F32)
    cact = P.sb(name + "_cact", [128, 8], F32)
    crep = P.sb(name + "_crep", [128, 8, 128], F32)
    mod = P.sb(name + "_mod", [128, ncols], F32)
    brep = P.sb(name + "_brep", [128, CW], F32)
    wch = P.sb(name + "_wch", [128, 8, CW], F32)
    P.dma("sp", ccol[:], c_d[:, :], writes=[name + "ccol"])
    P.act(cact[:], ccol[:], AF.Silu, reads=[name + "ccol"], writes=[name + "cact"])
    P.copy("dve", crep[:], cact[:].unsqueeze(2).broadcast_to([128, 8, 128]), reads=[name + "cact"], writes=[name + "crep"])
    for j in range(ncols // CW):
        P.dma("sp", wch[:], adaw_d[:, j * CW:(j + 1) * CW].rearrange("(c p) n -> p c n", p=128), writes=[name + "wch"])
        P.dma("sp", brep[:], adab_d[0:1, j * CW:(j + 1) * CW].partition_broadcast(128), writes=[name + "brep"])
        for c in range(8):
            P.mm(ps_bank[:, 0:CW], crep[:, c, :], wch[:, c, :], c == 0, c == 7,
                 reads=[name + "crep", name + "wch"], writes=[ps_bank.name])
        P.tt(mod[:, j * CW:(j + 1) * CW], ps_bank[:, 0:CW], brep[:], ALU.add,
             reads=[ps_bank.name, name + "brep"], writes=[name + "mod"])
    return mod


def rstd_from_ss(P, ss, rstd, n, key_in, key_out, width=1):
    nc = P.nc
    P.ts(rstd, ss, 1.0 / n, EPS, ALU.mult, ALU.add, reads=[key_in], writes=[key_out])
    P.act(rstd, rstd, AF.Sqrt, reads=[key_out], writes=[key_out])
    P.op("dve", lambda: nc.vector.reciprocal(out=rstd, in_=rstd), reads=[key_out], writes=[key_out])


def norm_mod_T(P, xt, xkey, GM, SH, gkeys, hb, hT, psT, idb, junk, small, tag, act_copy=True):
    nc = P.nc
    ss = small[:, 0:1]
    rstd = small[:, 1:2]
    P.act(junk[:], xt, AF.Square, reads=[xkey], writes=["junk" + tag, "ss" + tag], accum_out=ss)
    rstd_from_ss(P, ss, rstd, D, "ss" + tag, "rstd" + tag)
    P.stt(junk[:], xt, rstd, GM[:], ALU.mult, ALU.mult, reads=[xkey, "rstd" + tag] + gkeys, writes=["junk" + tag])
    P.tt(hb[:], junk[:], SH[:], ALU.add, reads=["junk" + tag] + gkeys, writes=["hb" + tag])
    for c in range(8):
        P.tr(psT[:, c * 128:(c + 1) * 128], hb[:, c * 128:(c + 1) * 128], idb[:], reads=["hb" + tag, "idb"], writes=[psT.name])
    P.copy("act" if act_copy else "dve", hT[:].rearrange("p c t -> p (c t)"), psT[:, :], reads=[psT.name], writes=["hT" + tag])


def build_mix1(S, nc=None, P=None, pfx="", over=None):
    over = over or {}
    standalone = nc is None
    NT = S // 128
    if standalone:
        nc = bass.Bass("TRN2", target_bir_lowering=False)
    dt = lambda n, s: over[n] if n in over else nc.dram_tensor(pfx + n, s, F32, kind="ExternalInput").ap()
    x_d = None if "xtile" in over else dt("x", [S, D])
    c_d = dt("c", [128, 8]); adaw_d = dt("adaw", [D, 2048]); adab_d = dt("adab", [1, 2048])
    ng_d = dt("ng", [1, D]); win_d = dt("win", [D, 4 * 384]); gqk_d = dt("gqk", [1, 256]); lam_d = dt("lam", [1, 256])
    ong_d = dt("ong", [1, 128]); ident_d = dt("ident", [128, 128]); negm_d = dt("negm", [128, 128])
    qab_d = dt("qab", [4, 4 * 128]); qad_d = dt("qad", [4, 4 * 128]); ka_d = dt("ka", [4, S])
    laminit_d = dt("laminit", [1, 2])
    y_d = None if "ytile" in over else nc.dram_tensor("y", [S, 512], F32, kind="ExternalOutput").ap()
    with ExitStack() as st:
        if P is None:
            P = Prog(nc, st)
        P.st = st
        idf, idb = load_consts(P, ident_d)
        banks = [P.ps("bk%d" % i, [128, 512], F32) for i in range(7)]
        psT = P.ps("psT", [128, 1024], BF16)
        mod = compute_mod(P, c_d, adaw_d, adab_d, 2048, banks[0], "m")
        GM = P.sb("GM", [128, D], F32)
        P.dma("sp", GM[:], ng_d[0:1, :].partition_broadcast(128), writes=["GM"])
        P.stt(GM[:], mod[:, 1024:2048], 1.0, GM[:], ALU.add, ALU.mult, reads=["mmod", "GM"], writes=["GM"])
        SH = mod[:, 0:1024]
        gk = ["GM", "mmod"]
        negb = P.sb("negb", [128, 128], BF16)
        P.dma("pool", negb[:], negm_d[:, :], writes=["negb"])
        Gqk = P.sb("Gqk", [128, 256], F32)
        P.dma("sp", Gqk[:], gqk_d[0:1, :].partition_broadcast(128), writes=["Gqk"])
        P.ts(Gqk[:, 0:128], Gqk[:, 0:128], 0.125, None, ALU.mult, None, reads=["Gqk"], writes=["Gqk"])
        ONG = P.sb("ONG", [128, 128], F32)
        P.dma("sp", ONG[:], ong_d[0:1, :].partition_broadcast(128), writes=["ONG"])
        lamt = P.sb("lamt_s", [128, 256], F32)
        lami = P.sb("lami", [128, 2], F32)
        lsm = P.sb("lsm", [128, 4], F32)
        P.dma("sp", lamt[:], lam_d[0:1, :].partition_broadcast(128), writes=["lamt"])
        P.dma("sp", lami[:], laminit_d[0:1, :].partition_broadcast(128), writes=["lami"])
        lv = lamt[:].rearrange("p (a d) -> p a d", a=4)
        P.tt(lamt[:, 0:64], lv[:, 0, :], lv[:, 1, :], ALU.mult, reads=["lamt"], writes=["lamt"])
        P.tt(lamt[:, 128:192], lv[:, 2, :], lv[:, 3, :], ALU.mult, reads=["lamt"], writes=["lamt"])
        P.op("dve", lambda: nc.vector.tensor_reduce(out=lsm[:, 0:1], in_=lamt[:, 0:64], axis=AX.X, op=ALU.add), reads=["lamt"], writes=["lsm"])
        P.op("dve", lambda: nc.vector.tensor_reduce(out=lsm[:, 1:2], in_=lamt[:, 128:192], axis=AX.X, op=ALU.add), reads=["lamt"], writes=["lsm"])
        P.act(lsm[:, 0:2], lsm[:, 0:2], AF.Exp, reads=["lsm"], writes=["lsm"])
        P.tt(lsm[:, 2:3], lsm[:, 1:2], lsm[:, 0:1], ALU.subtract, reads=["lsm"], writes=["lsm"])
        P.tt(lsm[:, 3:4], lsm[:, 2:3], lami[:, 0:1], ALU.subtract, reads=["lsm", "lami"], writes=["neglam"])
        neglam = lsm[:, 3:4]
        P.ts(ONG[:], ONG[:], lami[:, 1:2], None, ALU.mult, None, reads=["ONG", "lami"], writes=["ONG"])

        KT = P.sb("KT", [68, 2, S], BF16)
        QT = P.sb("QT", [68, 2, 128], BF16)
        qab = P.sb("qab_s", [68, 4, 128], F32)
        qad = P.sb("qad_s", [68, 4, 128], F32)
        P.dma("sp", qab[64:68, :, :], qab_d.rearrange("r (h t) -> r h t", h=4), writes=["qab"])
        P.dma("sp", qad[64:68, :, :], qad_d.rearrange("r (h t) -> r h t", h=4), writes=["qad"])
        for c in range(2):
            P.dma("pool", KT[64:68, c, :], ka_d[:, :], writes=["KTaug"])
        Vaug = P.sb("Vaug", [128, NT, 129], BF16)
        P.memset("pool", Vaug[:, :, 128:129], 1.0, writes=["Vones"])
        wsb = P.sb("wsb", [128, 8, 384], BF16)
        xts = [P.sb("xt%d" % i, [128, D], F32) for i in range(2)]
        junk = P.sb("junk", [128, D], F32)
        hb = P.sb("hb", [128, D], BF16)
        hT = P.sb("hT", [128, 8, 128], BF16)
        small = P.sb("small", [128, 16], F32)
        sq = P.sb("sq", [128, 256], F32)
        qkn = P.sb("qkn", [128, 256], BF16)
        PT = [[P.sb("PT%d%d" % (c, b), [128, 512], BF16) for b in range(2)] for c in range(2)]
        osb = P.sb("osb", [128, 128], F32)
        o2 = P.sb("o2", [128, 128], F32)
        yo = [P.sb("yo%d" % i, [128, 128], F32) for i in range(2)]
        psZ = banks[0]
        psS = [[banks[1], banks[2]], [banks[3], banks[4]]]
        psO = [banks[5], banks[6]]
        it = 0
        for j in range(4 if STAGE >= 1 else 0):
            P.dma("pool", wsb[:], win_d[:, j * 384:(j + 1) * 384].rearrange("(c p) n -> p c n", p=128),
                  writes=["wsb"])
            for i in range(NT):
                xt = xts[it % 2]; xk = "xt%d" % (it % 2)
                P.dma("sp", xt[:], over["xtile"](i) if "xtile" in over else x_d[i * 128:(i + 1) * 128, :], writes=[xk])
                norm_mod_T(P, xt[:], xk, GM, SH, gk, hb, hT, psT, idb, junk, small, "", act_copy=True)
                if STAGE < 2:
                    continue
                for c in range(8):
                    P.mm(psZ[:, 0:384], hT[:, c, :], wsb[:, c, :], c == 0, c == 7, reads=["hT", "wsb"], writes=[psZ.name])
                if STAGE < 3:
                    continue
                if SUB == 5:
                    continue
                P.act(sq[:], psZ[:, 0:256], AF.Square, reads=[psZ.name], writes=["sq"])
                P.op("dve", lambda: nc.vector.tensor_reduce(out=small[:, 4:8], in_=sq[:].rearrange("p (g d) -> p g d", g=4), axis=AX.X, op=ALU.add),
                     reads=["sq"], writes=["ss4"])
                rstd_from_ss(P, small[:, 4:8], small[:, 8:12], 64, "ss4", "rstd4")
                P.tt(sq[:].rearrange("p (g d) -> p g d", g=4), psZ[:, 0:256].rearrange("p (g d) -> p g d", g=4),
                     small[:, 8:12].unsqueeze(2).broadcast_to([128, 4, 64]), ALU.mult, reads=[psZ.name, "rstd4"], writes=["sq"])
                P.tt(qkn[:], sq[:], Gqk[:], ALU.mult, reads=["sq", "Gqk"], writes=["qkn"])
                if SUB == 2:
                    continue
                P.copy("act", Vaug[:, i, 0:128], psZ[:, 256:384], reads=[psZ.name], writes=["V%d" % i])
                if SUB == 3:
                    continue
                for g in range(4):
                    P.tr(psT[0:64, g * 128:(g + 1) * 128], qkn[:, g * 64:(g + 1) * 64], idb[:], reads=["qkn", "idb"], writes=[psT.name])
                if SUB == 4:
                    continue
                P.copy("dve", QT[0:64, :, :], psT[0:64, 0:256].rearrange("p (c t) -> p c t", c=2), reads=[psT.name], writes=["QT"])
                if SUB == 6:
                    continue
                for c in range(2):
                    P.copy(KTENG, KT[0:64, c, i * 128:(i + 1) * 128], psT[0:64, 256 + c * 128:256 + (c + 1) * 128],
                           reads=[psT.name], writes=["K%d" % i])
                if SUB == 7:
                    continue
                if SUB != 1:
                  P.stt(QT[64:68, :, :], qad[64:68, j:j + 1, :].broadcast_to([4, 2, 128]), float(i),
                        qab[64:68, j:j + 1, :].broadcast_to([4, 2, 128]), ALU.mult, ALU.add, reads=["qab", "qad"], writes=["QT"])
                if STAGE < 4:
                    continue
                ngrp = i // 4 + 1
                items = [(g, c) for g in range(ngrp) for c in range(2)]

                def emit_S(g, c):
                    kts = list(range(4 * g, min(4 * g + 4, i + 1)))
                    bank = psS[c][g % 2]
                    for jj, kt in enumerate(kts):
                        P.mm(bank[:, jj * 128:(jj + 1) * 128], KT[0:68, c, kt * 128:(kt + 1) * 128], QT[0:68, c, :],
                             True, kt != i, reads=["K%d" % kt, "KTaug", "QT"], writes=[bank.name])
                        if kt == i:
                            P.mm(bank[:, jj * 128:(jj + 1) * 128], idb[:], negb[:], False, True,
                                 reads=["idb", "negb"], writes=[bank.name])
                    pt = PT[c][g % 2]; pk = "PT%d%d" % (c, g % 2)
                    P.act(pt[:, 0:len(kts) * 128], bank[:, 0:len(kts) * 128], AF.Exp, reads=[bank.name], writes=[pk])

                def emit_PV(g, c):
                    kts = list(range(4 * g, min(4 * g + 4, i + 1)))
                    pt = PT[c][g % 2]; pk = "PT%d%d" % (c, g % 2)
                    for jj, kt in enumerate(kts):
                        P.mm(psO[c][:, 0:129], pt[:, jj * 128:(jj + 1) * 128], Vaug[:, kt, :], kt == 0, kt == i,
                             reads=[pk, "V%d" % kt, "Vones"], writes=[psO[c].name])

                emit_S(*items[0])
                for ix in range(len(items)):
                    if ix + 1 < len(items):
                        emit_S(*items[ix + 1])
                    emit_PV(*items[ix])
                if STAGE < 5:
                    continue
                P.op("dve", lambda: nc.vector.reciprocal(out=small[:, 12:13], in_=psO[0][:, 128:129]), reads=[psO[0].name], writes=["rd0"])
                P.op("dve", lambda: nc.vector.reciprocal(out=small[:, 13:14], in_=psO[1][:, 128:129]), reads=[psO[1].name], writes=["rd1"])
                P.ts(o2[:], psO[1][:, 0:128], small[:, 13:14], neglam, ALU.mult, ALU.mult, reads=[psO[1].name, "rd1", "neglam"], writes=["o2"])
                P.stt(osb[:], psO[0][:, 0:128], small[:, 12:13], o2[:], ALU.mult, ALU.add, reads=[psO[0].name, "rd0", "o2"], writes=["osb"])
                P.act(o2[:], osb[:], AF.Square, reads=["osb"], writes=["o2", "sso"], accum_out=small[:, 14:15])
                rstd_from_ss(P, small[:, 14:15], small[:, 15:16], 128, "sso", "rstdo")
                yt = yo[it % 2]; yk = "yo%d" % (it % 2)
                P.stt(yt[:], osb[:], small[:, 15:16], ONG[:], ALU.mult, ALU.mult, reads=["osb", "rstdo", "ONG"], writes=[yk])
                ydst = over["ytile"](i)[:, j * 128:(j + 1) * 128] if "ytile" in over else y_d[i * 128:(i + 1) * 128, j * 128:(j + 1) * 128]
                P.dma("sp", ydst, yt[:], reads=[yk], writes=["yout"])
                it += 1
        P.finish(["yout"])
        for k, v in P.lastw.items():
            pass
        for jx in range(len(P.dsem) if standalone else 0):
            if P.dcnt[jx] > 0:
                P._wait("sp", ("D%d" % jx, P.dsem[jx], 16 * P.dcnt[jx]))
        print("mix1 ninst", P.ninst, "nwait", P.nwait)
    return nc


def mix1_consts(S):
    ident = np.eye(128, dtype=np.float32)
    k_idx = np.arange(128)[:, None]; q_idx = np.arange(128)[None, :]
    negm = np.where(k_idx > q_idx, NEG, 0.0).astype(np.float32)
    ka = np.zeros((4, S), np.float32)
    t = np.arange(S)
    ka[0] = 1.0; ka[1] = t % 128; ka[2] = t // 128; ka[3] = 1.0
    return ident, negm, ka


def alibi_q_rows(slopes):
    nh = len(slopes)
    qab = np.zeros((4, nh, 128), np.float32); qad = np.zeros((4, nh, 128), np.float32)
    for h, s in enumerate(slopes):
        qab[0, h] = -s * np.arange(128); qab[1, h] = s; qab[2, h] = 128.0 * s
        qad[3, h] = -128.0 * s
    return qab.reshape(4, nh * 128), qad.reshape(4, nh * 128)


def slopes8():
    return [2.0 ** (-8.0 * (h + 1) / 8) for h in range(8)]


def prep_mix1(inp, layer, x_b, c_b, hh, S):
    o = layer // 2
    w = inp["od_w_in"][o]
    cols = []
    for j in range(4):
        h = 4 * hh + j
        cols.append(w[:, h * 128:(h + 1) * 128])
        cols.append(w[:, 1024 + h * 128:1024 + (h + 1) * 128])
        cols.append(w[:, 2048 + h * 128:2048 + (h + 1) * 128])
    win = np.ascontiguousarray(np.concatenate(cols, axis=1))
    ident, negm, ka = mix1_consts(S)
    sl = slopes8()[4 * hh:4 * hh + 4]
    qab, qad = alibi_q_rows(sl)
    qn = inp["od_qn_g"][o]; kn = inp["od_kn_g"][o]
    gqk = np.concatenate([qn, qn, kn, kn])[None, :].astype(np.float32)
    import math
    lam_init = 0.8 - 0.6 * math.exp(-0.3 * layer)
    return {
        "x": None if x_b is None else np.ascontiguousarray(x_b), "c": np.ascontiguousarray(c_b.reshape(8, 128).T),
        "adaw": np.ascontiguousarray(inp["ada_w"][layer][:, 0:2048]), "adab": np.ascontiguousarray(inp["ada_b"][layer][None, 0:2048]),
        "ng": np.ascontiguousarray(inp["norm_mix_g"][layer][None, :]), "win": win, "gqk": gqk,
        "lam": np.ascontiguousarray(inp["od_lam"][o].reshape(1, 256)), "ong": np.ascontiguousarray(inp["od_onorm_g"][o][None, :]),
        "ident": ident, "negm": negm, "qab": qab, "qad": qad, "ka": ka,
        "laminit": np.array([[lam_init, 1.0 - lam_init]], np.float32),
    }


def build_ffn(NTOK, nc=None, P=None, pfx="", over=None):
    over = over or {}
    standalone = nc is None
    NST = NTOK // 512
    if standalone:
        nc = bass.Bass("TRN2", target_bir_lowering=False)
    dt = lambda n, s: over[n] if n in over else nc.dram_tensor(pfx + n, s, F32, kind="ExternalInput").ap()
    yg = over.get("yg"); SEQ = 2 * NTOK
    x_d = None if "xtile" in over else dt("x", [NTOK, D])
    c_d = dt("c", [128, 8])
    y_d = dt("y", [NTOK, D]) if yg is None else None
    hsel_d = dt("hsel", [128, 2]) if yg is not None else None
    adaw_d = dt("adaw", [D, 4096]); adab_d = dt("adab", [1, 4096]); ng_d = dt("ng", [1, D])
    wout_d = dt("wout", [D, D]); wq_d = dt("wq", [D, 2048]); keysT_d = dt("keysT", [128, 256])
    uT_d = dt("uT", [D, 16384]); v_d = dt("v", [16384, D]); ident_d = dt("ident", [128, 128])
    o_d = None if "otile" in over else nc.dram_tensor("o", [NTOK, D], F32, kind="ExternalOutput").ap()
    with ExitStack() as st:
        if P is None:
            P = Prog(nc, st)
        P.st = st
        idf, idb = load_consts(P, ident_d)
        banks = [P.ps("bk%d" % i, [128, 512], F32) for i in range(7)]
        psT = P.ps("psT", [128, 1024], BF16)
        mod = compute_mod(P, c_d, adaw_d, adab_d, 4096, banks[0], "m")
        GM = P.sb("GM", [128, D], F32)
        P.dma("sp", GM[:], ng_d[0:1, :].partition_broadcast(128), writes=["GM"])
        P.stt(GM[:], mod[:, 2048:3072], 1.0, GM[:], ALU.add, ALU.mult, reads=["mmod", "GM"], writes=["GM"])
        G1 = mod[:, 0:1024]; SH = mod[:, 1024:2048]; G2 = mod[:, 3072:4096]
        gk = ["GM", "mmod"]
        keysb = P.sb("keysb", [128, 256], BF16)
        P.dma("pool", keysb[:], keysT_d[:, :], writes=["keysb"])
        W = [P.sb("W%d" % i, [128, 8, 512], BF16) for i in range(2)]
        Vc = [P.sb("Vc%d" % i, [128, 4, 1024], BF16) for i in range(2)]
        wn = [0]; vn = [0]

        def loadW(src):
            i = wn[0] % 2; wn[0] += 1
            P.dma("pool", W[i][:], src.rearrange("(c p) n -> p c n", p=128), writes=["W%d" % i])
            return W[i], "W%d" % i

        xt = P.sb("xt", [128, D], F32)
        if yg is not None:
            ysf = P.sb("ysf", [128, 4, 512], F32)
            hsel = P.sb("hsel_s", [128, 2], F32)
            P.dma("sp", hsel[:], hsel_d[:, :], writes=["hsel"])
        yb = P.sb("yb", [128, D], BF16)
        yT = P.sb("yT", [128, 8, 128], BF16)
        junk = P.sb("junk", [128, D], F32)
        hb = P.sb("hb", [128, D], BF16)
        small = P.sb("small", [128, 8], F32)
        x1 = [P.sb("x1_%d" % t, [128, D], F32) for t in range(4)]
        h2T = [P.sb("h2T_%d" % t, [128, 8, 128], BF16) for t in range(4)]
        qTb = [P.sb("qTb_%d" % t, [128, 16, 128], BF16) for t in range(4)]
        sT = [P.sb("sT_%d" % t, [128, 16, 128], BF16) for t in range(4)]
        thr = [P.sb("thr_%d" % t, [128, 8], F32) for t in range(4)]
        nb = [P.sb("nb_%d" % t, [128, 8], F32) for t in range(4)]
        acc = [P.sb("acc_%d" % t, [128, D], F32) for t in range(4)]
        sbf = P.sb("sbf", [128, 16, 128], BF16)
        t16 = P.sb("t16", [128, 16, 16], F32)
        tmp128 = P.sb("tmp128", [128, 128], BF16)
        cand = P.sb("cand", [128, 8, 256], F32)
        cand2 = P.sb("cand2", [128, 256], F32)
        c16 = P.sb("c16", [128, 8, 16], F32)
        e16 = P.sb("e16", [128, 8, 16], F32)
        zs = P.sb("zs", [128, 8], F32)
        ef = [P.sb("ef%d" % i, [128, 512], F32) for i in range(3)]
        wb = [None] + [P.sb("wbm%d" % i, [128, 512], BF16) for i in range(1, 8)]
        Gs = [P.sb("Gs%d" % i, [128, 512], F32) for i in range(2)]
        ethr = [P.sb("ethr_%d" % t, [128, 8], F32) for t in range(4)]
        gh = [P.sb("gh%d" % i, [128, 512], F32) for i in range(2)]
        Ab = [P.sb("Ab%d" % i, [128, 512], BF16) for i in range(2)]
        AT = [P.sb("AT%d" % i, [128, 4, 128], BF16) for i in range(2)]
        ot = junk
        psZ = [banks[0], banks[1], banks[6], banks[2]]; psH = banks[3]; psO = [banks[4], banks[5]]; zc = [0]
        sel1 = lambda c: idb[:, 4 * c:4 * c + 4].unsqueeze(2).broadcast_to([128, 4, 128])
        sel2 = idb[:, :].unsqueeze(1).broadcast_to([128, 4, 128])
        cnt = [0]
        for stile in range(NST):
            t0 = stile * 512
            wo = [loadW(wout_d[:, hf * 512:(hf + 1) * 512]) for hf in range(2)]
            for t in range(4):
                r0 = t0 + t * 128
                if yg is None:
                    P.dma("pool", yb[:], y_d[r0:r0 + 128, :], writes=["yb"])
                else:
                    for r in range(2):
                        for q in range(2):
                            P.dma("sp", ysf[:, r * 2 + q, :], yg(r, q, r0), writes=["ysf"])
                    for r in range(2):
                        P.ts(junk[:, r * 512:(r + 1) * 512], ysf[:, r * 2, :], hsel[:, 0:1], None, ALU.mult, None,
                             reads=["ysf", "hsel"], writes=["junk"])
                        P.stt(yb[:, r * 512:(r + 1) * 512], ysf[:, r * 2 + 1, :], hsel[:, 1:2], junk[:, r * 512:(r + 1) * 512],
                              ALU.mult, ALU.add, reads=["ysf", "hsel", "junk"], writes=["yb"])
                P.dma("sp", xt[:], over["xtile"](r0) if "xtile" in over else x_d[r0:r0 + 128, :], writes=["xt"])
                for c in range(8):
                    P.tr(psT[:, c * 128:(c + 1) * 128], yb[:, c * 128:(c + 1) * 128], idb[:], reads=["yb", "idb"], writes=[psT.name])
                P.copy("act", yT[:].rearrange("p c t -> p (c t)"), psT[:, :], reads=[psT.name], writes=["yT"])
                for hf in range(2):
                    bk = psO[hf]
                    for c in range(8):
                        P.mm(bk[:, :], yT[:, c, :], wo[hf][0][:, c, :], c == 0, c == 7, reads=["yT", wo[hf][1]], writes=[bk.name])
                    P.tt(junk[:, hf * 512:(hf + 1) * 512], bk[:, :], G1[:, hf * 512:(hf + 1) * 512], ALU.mult,
                         reads=[bk.name, "mmod"], writes=["junk"])
                P.tt(x1[t][:], junk[:], xt[:], ALU.add, reads=["junk", "xt"], writes=["x1_%d" % t])
                norm_mod_T(P, x1[t][:], "x1_%d" % t, GM, SH, gk, hb, h2T[t], psT, idb, junk, small, "", act_copy=True)
                P.lastw["h2T_%d" % t] = P.lastw["hT"]
            for g in range(4):
                wq, wqk = loadW(wq_d[:, g * 512:(g + 1) * 512])
                for t in range(4):
                    bk = psZ[(g * 4 + t) % 2]
                    for hp in range(4):
                        for c in range(8):
                            P.mm(bk[:, hp * 128:(hp + 1) * 128], wq[:, c, hp * 128:(hp + 1) * 128], h2T[t][:, c, :], c == 0, c == 7,
                                 reads=[wqk, "h2T_%d" % t], writes=[bk.name])
                    P.copy("act", qTb[t][:, 4 * g:4 * g + 4, :].rearrange("p a t -> p (a t)"), bk[:, :], reads=[bk.name], writes=["qTb_%d" % t])
            for t in range(4):
                for g in range(4):
                    bk = psZ[g % 2]
                    for a in range(4):
                        hp = 4 * g + a
                        P.mm(bk[:, a * 128:(a + 1) * 128], qTb[t][:, hp, :], keysb[:, (hp % 2) * 128:(hp % 2 + 1) * 128], True, True,
                             reads=["qTb_%d" % t, "keysb"], writes=[bk.name])
                    P.copy("act", sbf[:, 4 * g:4 * g + 4, :].rearrange("p a n -> p (a n)"), bk[:, :], reads=[bk.name], writes=["sbf"])
                for hp in range(16):
                    P.op("dve", lambda: nc.vector.max(out=t16[:, hp, 0:8], in_=sbf[:, hp, :]), reads=["sbf"], writes=["t16"])
                    P.op("dve", lambda: nc.vector.match_replace(out=tmp128[:], in_to_replace=t16[:, hp, 0:8], in_values=sbf[:, hp, :], imm_value=-1e30),
                         reads=["sbf", "t16"], writes=["tmp128"])
                    P.op("dve", lambda: nc.vector.max(out=t16[:, hp, 8:16], in_=tmp128[:]), reads=["tmp128"], writes=["t16"])
                tv = t16[:].rearrange("p (h two) k -> p h two k", two=2)
                for h in range(8):
                    P.tt(cand[:, h, :].rearrange("p (a b) -> p a b", a=16), tv[:, h, 0, :].unsqueeze(2).broadcast_to([128, 16, 16]),
                         tv[:, h, 1, :].unsqueeze(1).broadcast_to([128, 16, 16]), ALU.add, reads=["t16"], writes=["cand"])
                for h in range(8):
                    P.op("dve", lambda: nc.vector.max(out=c16[:, h, 0:8], in_=cand[:, h, :]), reads=["cand"], writes=["c16"])
                    P.op("dve", lambda: nc.vector.match_replace(out=cand2[:], in_to_replace=c16[:, h, 0:8], in_values=cand[:, h, :], imm_value=-1e30),
                         reads=["cand", "c16"], writes=["cand2"])
                    P.op("dve", lambda: nc.vector.max(out=c16[:, h, 8:16], in_=cand2[:]), reads=["cand2"], writes=["c16"])
                P.copy("dve", thr[t][:], c16[:, :, 15], reads=["c16"], writes=["thr_%d" % t])
                P.tt(e16[:], c16[:], c16[:, :, 0:1].broadcast_to([128, 8, 16]), ALU.subtract, reads=["c16"], writes=["e16"])
                P.act(e16[:], e16[:], AF.Exp, reads=["e16"], writes=["e16"])
                P.op("dve", lambda: nc.vector.tensor_reduce(out=zs[:], in_=e16[:], axis=AX.X, op=ALU.add), reads=["e16"], writes=["zs"])
                P.act(zs[:], zs[:], AF.Ln, reads=["zs"], writes=["zs"])
                P.tt(zs[:], zs[:], c16[:, :, 0], ALU.add, reads=["zs", "c16"], writes=["zs"])
                P.ts(nb[t][:], zs[:], -1.0, None, ALU.mult, None, reads=["zs"], writes=["nb_%d" % t])
                P.tt(ethr[t][:], thr[t][:], nb[t][:], ALU.add, reads=["thr_%d" % t, "nb_%d" % t], writes=["ethr_%d" % t])
                P.act(ethr[t][:], ethr[t][:], AF.Exp, reads=["ethr_%d" % t], writes=["ethr_%d" % t])
                for half in range(2):
                    for a in range(8):
                        P.tr(psT[:, a * 128:(a + 1) * 128], sbf[:, half * 8 + a, :], idb[:], reads=["sbf", "idb"], writes=[psT.name])
                    P.copy("act", sT[t][:, half * 8:half * 8 + 8, :].rearrange("p a t -> p (a t)"), psT[:, :], reads=[psT.name], writes=["sT_%d" % t])
            def tail(pu):
                pk, pt_, pvi, pch = pu
                for es in range(4):
                    P.tr(psT[:, es * 128:(es + 1) * 128], Ab[pk][:, es * 128:(es + 1) * 128], idb[:], reads=["Ab%d" % pk, "idb"], writes=[psT.name])
                P.copy("act", AT[pk][:].rearrange("p a t -> p (a t)"), psT[:, 0:512], reads=[psT.name], writes=["AT%d" % pk])

            def tail2(pu):
                pk, pt_, pvi, pch = pu
                for hf in range(2):
                    for es in range(4):
                        P.mm(psO[hf][:, :], AT[pk][:, es, :], Vc[pvi][:, es, hf * 512:(hf + 1) * 512], es == 0, es == 3,
                             reads=["AT%d" % pk, "Vc%d" % pvi], writes=[psO[hf].name])
                    if pch == 0:
                        P.copy("dve", acc[pt_][:, hf * 512:(hf + 1) * 512], psO[hf][:, :], reads=[psO[hf].name], writes=["acc_%d" % pt_])
                    else:
                        P.tt(acc[pt_][:, hf * 512:(hf + 1) * 512], acc[pt_][:, hf * 512:(hf + 1) * 512], psO[hf][:, :], ALU.add,
                             reads=[psO[hf].name, "acc_%d" % pt_], writes=["acc_%d" % pt_])

            prev = None
            for ch in range(32):
                uw, uk = loadW(uT_d[:, ch * 512:(ch + 1) * 512])
                vi = vn[0] % 2; vn[0] += 1
                P.dma("pool", Vc[vi][:], v_d[ch * 512:(ch + 1) * 512, :].rearrange("(a p) n -> p a n", p=128), writes=["Vc%d" % vi])
                for t in range(4):
                    k = cnt[0] % 2
                    for h in range(8):
                        bz = psZ[zc[0] % 4]; ei = zc[0] % 3; zc[0] += 1
                        P.mm(bz[:, :], sT[t][:, 2 * h, :], sel1(ch), True, False, reads=["sT_%d" % t, "idb"], writes=[bz.name])
                        P.mm(bz[:, :], sT[t][:, 2 * h + 1, :], sel2, False, True, reads=["sT_%d" % t, "idb"], writes=[bz.name])
                        P.act(ef[ei][:], bz[:, :], AF.Exp, reads=[bz.name, "nb_%d" % t], writes=["ef%d" % ei], bias=nb[t][:, h:h + 1], scale=1.0)
                        if h == 0:
                            P.stt(Gs[k][:], ef[ei][:], ethr[t][:, h:h + 1], ef[ei][:], ALU.is_ge, ALU.mult,
                                  reads=["ef%d" % ei, "ethr_%d" % t], writes=["Gs%d" % k])
                        else:
                            P.stt(wb[h][:], ef[ei][:], ethr[t][:, h:h + 1], ef[ei][:], ALU.is_ge, ALU.mult,
                                  reads=["ef%d" % ei, "ethr_%d" % t], writes=["wbm%d" % h])
                            P.tt(Gs[k][:], Gs[k][:], wb[h][:], ALU.add, reads=["Gs%d" % k, "wbm%d" % h], writes=["Gs%d" % k], e="pool")
                    if prev is not None:
                        tail(prev)
                    for c in range(8):
                        P.mm(psH[:, :], h2T[t][:, c, :], uw[:, c, :], c == 0, c == 7, reads=["h2T_%d" % t, uk], writes=[psH.name])
                    P.act(gh[k][:], psH[:, :], AF.Gelu, reads=[psH.name], writes=["gh%d" % k])
                    P.tt(Ab[k][:], Gs[k][:], gh[k][:], ALU.mult, reads=["Gs%d" % k, "gh%d" % k], writes=["Ab%d" % k])
                    if prev is not None:
                        tail2(prev)
                    prev = (k, t, vi, ch)
                    cnt[0] += 1
            tail(prev); tail2(prev)
            for t in range(4):
                r0 = t0 + t * 128
                P.tt(ot[:], acc[t][:], G2, ALU.mult, reads=["acc_%d" % t, "mmod"], writes=["junk"], e="pool")
                P.tt(ot[:], ot[:], x1[t][:], ALU.add, reads=["junk", "x1_%d" % t], writes=["junk"], e="pool")
                P.dma("sp", over["otile"](r0) if "otile" in over else o_d[r0:r0 + 128, :], ot[:], reads=["junk"], writes=["oout"])
        for jx in range(len(P.dsem) if standalone else 0):
            if P.dcnt[jx] > 0:
                P._wait("sp", ("D%d" % jx, P.dsem[jx], 16 * P.dcnt[jx]))
        print("ffn ninst", P.ninst, "nwait", P.nwait)
    return nc


def prep_ffn(inp, layer, x_rows, y_rows, c_b, wout):
    return {
        "x": None if x_rows is None else np.ascontiguousarray(x_rows), "y": None if y_rows is None else np.ascontiguousarray(y_rows), "c": np.ascontiguousarray(c_b.reshape(8, 128).T),
        "adaw": np.ascontiguousarray(inp["ada_w"][layer][:, 2048:6144]), "adab": np.ascontiguousarray(inp["ada_b"][layer][None, 2048:6144]),
        "ng": np.ascontiguousarray(inp["norm_ffn_g"][layer][None, :]), "wout": np.ascontiguousarray(wout),
        "wq": np.ascontiguousarray(inp["peer_wq"][layer]),
        "keysT": np.ascontiguousarray(np.concatenate([inp["peer_keys"][layer][0].T, inp["peer_keys"][layer][1].T], axis=1)),
        "uT": np.ascontiguousarray(inp["peer_u"][layer].T), "v": np.ascontiguousarray(inp["peer_v"][layer]),
        "ident": np.eye(128, dtype=np.float32),
    }


def build_mix0(S, nc=None, P=None, pfx="", over=None):
    over = over or {}
    standalone = nc is None
    NT = S // 128
    if standalone:
        nc = bass.Bass("TRN2", target_bir_lowering=False)
    dt = lambda n, s: over[n] if n in over else nc.dram_tensor(pfx + n, s, F32, kind="ExternalInput").ap()
    x_d = dt("x", [S, D]); c_d = dt("c", [128, 8]); adaw_d = dt("adaw", [D, 2048]); adab_d = dt("adab", [1, 2048])
    ng_d = dt("ng", [1, D]); win_d = dt("win", [D, 1796]); convw_d = dt("convw", [128, 16]); gb_d = dt("gb", [1, 4])
    mg_d = dt("mg", [1, 256]); gqk_d = dt("gqk", [1, 512]); ident_d = dt("ident", [128, 128]); negm_d = dt("negm", [128, 128])
    posm_d = dt("posm", [128, 128]); trile_d = dt("trile", [128, 128]); sgt_d = dt("sgt", [128, 128])
    qab_d = dt("qab", [4, 4 * 128]); qad_d = dt("qad", [4, 4 * 128]); ka_d = dt("ka", [4, S]); kblk_d = dt("kblk", [32, S])
    fut_d = dt("fut", [33, 32]); own_d = dt("own", [33, 32])
    y_d = None if "ytile" in over else nc.dram_tensor("y", [S, 512], F32, kind="ExternalOutput").ap()
    with ExitStack() as st:
        if P is None:
            P = Prog(nc, st)
        P.st = st
        idf, idb = load_consts(P, ident_d)
        banks = [P.ps("bk%d" % i, [128, 512], F32) for i in range(7)]
        psT = P.ps("psT", [128, 1024], BF16)
        mod = compute_mod(P, c_d, adaw_d, adab_d, 2048, banks[0], "m")
        GM = P.sb("GM", [128, D], F32)
        P.dma("sp", GM[:], ng_d[0:1, :].partition_broadcast(128), writes=["GM"])
        P.stt(GM[:], mod[:, 1024:2048], 1.0, GM[:], ALU.add, ALU.mult, reads=["mmod", "GM"], writes=["GM"])
        SH = mod[:, 0:1024]
        gk = ["GM", "mmod"]
        cf = lambda name, src: (lambda t: (P.dma("sp", t[:], src, writes=[name]), t)[1])(P.sb(name, [128, 128], F32))
        negb = P.sb("negb", [128, 128], BF16)
        P.dma("pool", negb[:], negm_d[:, :], writes=["negb"])
        posm = cf("posm_s", posm_d[:, :]); trile = cf("trile_s", trile_d[:, :]); sgt = cf("sgt_s", sgt_d[:, :])
        onesf = P.sb("onesf", [128, 128], F32)
        P.memset("pool", onesf[:], 1.0, writes=["onesf"])
        convw = P.sb("convw_s", [128, 4, 4], F32)
        P.dma("sp", convw[:].rearrange("p a j -> p (a j)"), convw_d[:, :], writes=["convw"])
        GB = P.sb("GB", [128, 4], F32)
        P.dma("sp", GB[:], gb_d[0:1, :].partition_broadcast(128), writes=["GB"])
        MG = P.sb("MG", [128, 256], F32)
        P.dma("sp", MG[:], mg_d[0:1, :].partition_broadcast(128), writes=["MG"])
        Gqk = P.sb("Gqk", [128, 512], F32)
        P.dma("sp", Gqk[:], gqk_d[0:1, :].partition_broadcast(128), writes=["Gqk"])
        P.ts(Gqk[:, 0:256], Gqk[:, 0:256], 0.125, None, ALU.mult, None, reads=["Gqk"], writes=["Gqk"])
        wsb = P.sb("wsb", [128, 8, 1796], BF16)
        for c in range(8):
            P.dma("pool", wsb[:, c, :], win_d[c * 128:(c + 1) * 128, :], writes=["wsb"])
        KT = P.sb("KT", [100, 4, S], BF16)
        QT = P.sb("QT", [100, 4, 128], BF16)
        QTf = P.sb("QTf", [64, 4, 128], F32)
        qab = P.sb("qab_s", [100, 4, 128], F32)
        qad = P.sb("qad_s", [100, 4, 128], F32)
        P.dma("sp", qab[96:100, :, :], qab_d.rearrange("r (h t) -> r h t", h=4), writes=["qab"])
        P.dma("sp", qad[96:100, :, :], qad_d.rearrange("r (h t) -> r h t", h=4), writes=["qad"])
        for a in range(4):
            P.dma("pool", KT[64:96, a, :], kblk_d[:, :], writes=["KTaug"])
            P.dma("pool", KT[96:100, a, :], ka_d[:, :], writes=["KTaug"])
        Vm = P.sb("Vm", [128, NT, 4, 65], BF16)
        P.memset("pool", Vm[:, :, :, 64:65], 1.0, writes=["Vones"])
        kmT = P.sb("kmT", [64, 4, 32], F32)
        P.memset("pool", kmT[:], 0.0, writes=["kmT"])
        Bw = P.sb("Bw", [128, 4, 96], F32)
        P.memset("pool", Bw[:], 0.0, writes=["Bw"])
        FUT = P.sb("FUT", [128, 32], F32); OWN = P.sb("OWN", [128, 32], F32)
        xts = [P.sb("xt%d" % i, [128, D], F32) for i in range(2)]
        junk = P.sb("junk", [128, D], F32)
        hb = P.sb("hb", [128, D], BF16)
        hT = P.sb("hT", [128, 8, 128], BF16)
        small = P.sb("small", [128, 40], F32)
        cbuf = P.sb("cbuf", [128, 4, 131], F32)
        P.memset("pool", cbuf[:], 0.0, writes=["cbuf"])
        cacc = P.sb("cacc", [128, 4, 128], F32)
        ctmp = P.sb("ctmp", [128, 4, 128], F32)
        qkT = P.sb("qkT", [128, 4, 128], BF16)
        gi = P.sb("gi", [128, 8], F32)
        Lm = P.sb("Lm", [128, 128], F32)
        DT = P.sb("DT", [128, 128], F32)
        SmT = P.sb("SmT", [128, 128], BF16)
        vaug = P.sb("vaug", [128, 2, 129], BF16)
        P.memset("pool", vaug[:, :, 128:129], 1.0, writes=["vaug1"])
        kk = P.sb("kk", [128, 128], BF16)
        intra = P.sb("intra", [128, 129], F32)
        num = P.sb("num", [128, 129], F32)
        hm = P.sb("hm", [128, 128], F32)
        sig = P.sb("sig", [128, 128], F32)
        Cf = [P.sb("Cf%d" % m, [128, 129], F32) for m in range(2)]
        Cb = [P.sb("Cb%d" % m, [128, 129], BF16) for m in range(2)]
        for m in range(2):
            P.memset("pool", Cf[m][:], 0.0, writes=["Cf%d" % m])
            P.memset("pool", Cb[m][:], 0.0, writes=["Cb%d" % m])
        sq = P.sb("sq", [128, 512], F32)
        qkn = P.sb("qkn", [128, 512], F32)
        gm = P.sb("gm", [128, 32], F32)
        top8 = P.sb("top8", [128, 8], F32)
        PT = [P.sb("PT%d" % b, [128, 512], BF16) for b in range(2)]
        ymix = [P.sb("ymix%d" % b, [128, 512], F32) for b in range(2)]
        bkA, bkB, bkC, bkD, bkE, bkF, bkG = banks
        for i in range(NT):
            blk = i // 2
            xt = xts[i % 2]; xk = "xt%d" % (i % 2)
            ym = ymix[i % 2]; yk = "ymix%d" % (i % 2)
            P.dma("sp", xt[:], x_d[i * 128:(i + 1) * 128, :], writes=[xk])
            if i % 2 == 0:
                P.dma("sp", FUT[:], fut_d[blk:blk + 1, :].partition_broadcast(128), writes=["FUT"])
                P.dma("sp", OWN[:], own_d[blk:blk + 1, :].partition_broadcast(128), writes=["OWN"])
            norm_mod_T(P, xt[:], xk, GM, SH, gk, hb, hT, psT, idb, junk, small, "", act_copy=True)
            for a in range(4):
                for c in range(8):
                    P.mm(bkD[:, a * 128:(a + 1) * 128], wsb[:, c, a * 128:(a + 1) * 128], hT[:, c, :], c == 0, c == 7,
                         reads=["wsb", "hT"], writes=[bkD.name])
            for bk, c0, n in ((bkA, 512, 512), (bkB, 1024, 512), (bkC, 1536, 260)):
                for c in range(8):
                    P.mm(bk[:, 0:n], hT[:, c, :], wsb[:, c, c0:c0 + n], c == 0, c == 7, reads=["wsb", "hT"], writes=[bk.name])
            P.copy("pool", cbuf[:, :, 0:3], cbuf[:, :, 128:131], reads=["cbuf"], writes=["cbuf"])
            P.copy("act", cbuf[:, :, 3:131], bkD[:, :].rearrange("p (a t) -> p a t", a=4), reads=[bkD.name], writes=["cbuf"])
            for j in range(4):
                dst = cacc if j == 0 else ctmp
                P.tt(dst[:], cbuf[:, :, j:j + 128], convw[:, :, j:j + 1].broadcast_to([128, 4, 128]), ALU.mult,
                     reads=["cbuf", "convw"], writes=["cacc" if j == 0 else "ctmp"])
                if j > 0:
                    P.tt(cacc[:], cacc[:], ctmp[:], ALU.add, reads=["cacc", "ctmp"], writes=["cacc"])
            P.act(ctmp[:], cacc[:], AF.Silu, reads=["cacc"], writes=["ctmp"])
            P.copy("dve", qkT[:, 0:2, :], ctmp[:, 0:2, :], reads=["ctmp"], writes=["qkT"])
            P.ts(qkT[:, 2:4, :], ctmp[:, 2:4, :], 128.0 ** -0.5, None, ALU.mult, None, reads=["ctmp"], writes=["qkT"])
            P.tt(gi[:, 0:4], bkC[:, 256:260], GB[:], ALU.add, reads=[bkC.name, "GB"], writes=["gi"])
            P.act(gi[:, 4:6], gi[:, 2:4], AF.Exp, reads=["gi"], writes=["gi"], scale=-1.0)
            P.ts(gi[:, 4:6], gi[:, 4:6], 1.0, None, ALU.add, None, reads=["gi"], writes=["gi"])
            P.act(gi[:, 6:8], gi[:, 4:6], AF.Ln, reads=["gi"], writes=["gi"])
            nfl = gi[:, 6:8]
            P.mm(bkD[:, 0:2], trile[:], nfl, True, True, reads=["trile_s", "gi", "cbuf"], writes=[bkD.name])
            P.mm(bkD[:, 2:4], onesf[:], nfl, True, True, reads=["onesf", "gi"], writes=[bkD.name])
            P.act(small[:, 4:8], bkD[:, 0:4], AF.Exp, reads=[bkD.name], writes=["wd"], scale=-1.0)
            P.copy("act", vaug[:, :, 0:128], bkA[:, 0:256].rearrange("p (m d) -> p m d", m=2), reads=[bkA.name], writes=["vaug"])
            for m in range(2):
                P.ts(Lm[:], sgt[:], nfl[:, m:m + 1], None, ALU.mult, None, reads=["sgt_s", "gi"], writes=["Lm"])
                P.mm(bkE[:, 0:128], Lm[:], trile[:], True, False, reads=["Lm", "trile_s"], writes=[bkE.name])
                P.mm(bkE[:, 0:128], idf[:], posm[:], False, True, reads=["idf", "posm_s"], writes=[bkE.name])
                P.act(DT[:], bkE[:, 0:128], AF.Exp, reads=[bkE.name, "gi"], writes=["DT"], bias=gi[:, m:m + 1], scale=-1.0)
                P.mm(bkE[:, 128:256], qkT[:, 2 + m, :], qkT[:, m, :], True, True, reads=["qkT"], writes=[bkE.name])
                P.tt(SmT[:], bkE[:, 128:256], DT[:], ALU.mult, reads=[bkE.name, "DT"], writes=["SmT"])
                P.tr(psT[:, 0:128], qkT[:, 2 + m, :], idb[:], reads=["qkT", "idb"], writes=[psT.name])
                P.ts(kk[:], psT[:, 0:128], DT[:, 127:128], None, ALU.mult, None, reads=[psT.name, "DT"], writes=["kk"])
                P.mm(bkF[:, 0:129], qkT[:, m, :], Cb[m][:], True, True, reads=["qkT", "Cb%d" % m], writes=[bkF.name])
                P.mm(bkF[:, 136:265], SmT[:], vaug[:, m, :], True, True, reads=["SmT", "vaug", "vaug1"], writes=[bkF.name])
                P.mm(bkF[:, 272:401], kk[:], vaug[:, m, :], True, True, reads=["kk", "vaug", "vaug1"], writes=[bkF.name])
                P.copy("act", intra[:], bkF[:, 136:265], reads=[bkF.name], writes=["intra"])
                P.stt(num[:], bkF[:, 0:129], small[:, 4 + m:5 + m], intra[:], ALU.mult, ALU.add, reads=[bkF.name, "wd", "intra"], writes=["num"])
                P.stt(Cf[m][:], Cf[m][:], small[:, 6 + m:7 + m], bkF[:, 272:401], ALU.mult, ALU.add,
                      reads=["Cf%d" % m, "wd", bkF.name], writes=["Cf%d" % m])
                P.copy("pool", Cb[m][:], Cf[m][:], reads=["Cf%d" % m], writes=["Cb%d" % m])
                P.ts(small[:, 8:9], num[:, 128:129], 1.0, None, ALU.max, None, reads=["num"], writes=["den"])
                P.ts(small[:, 11:12], num[:, 128:129], -1.0, 1.0, ALU.mult, ALU.max, reads=["num"], writes=["den2"])
                P.tt(small[:, 8:9], small[:, 8:9], small[:, 11:12], ALU.max, reads=["den", "den2"], writes=["den"])
                P.op("dve", lambda: nc.vector.reciprocal(out=small[:, 8:9], in_=small[:, 8:9]), reads=["den"], writes=["den"])
                P.ts(hm[:], num[:, 0:128], small[:, 8:9], None, ALU.mult, None, reads=["num", "den"], writes=["hm"])
                P.act(sig[:], hm[:], AF.Square, reads=["hm"], writes=["sig", "ssm"], accum_out=small[:, 9:10])
                rstd_from_ss(P, small[:, 9:10], small[:, 10:11], 128, "ssm", "rstdm")
                P.act(sig[:], bkA[:, 256 + m * 128:256 + (m + 1) * 128], AF.Sigmoid, reads=[bkA.name], writes=["sig"])
                P.stt(hm[:], hm[:], small[:, 10:11], MG[:, m * 128:(m + 1) * 128], ALU.mult, ALU.mult, reads=["hm", "rstdm", "MG"], writes=["hm"])
                P.tt(ym[:, m * 128:(m + 1) * 128], hm[:], sig[:], ALU.mult, reads=["hm", "sig"], writes=[yk])
            P.act(sq[:], bkB[:, :], AF.Square, reads=[bkB.name], writes=["sq"])
            P.op("dve", lambda: nc.vector.tensor_reduce(out=small[:, 16:24], in_=sq[:].rearrange("p (g d) -> p g d", g=8), axis=AX.X, op=ALU.add),
                 reads=["sq"], writes=["ss8"])
            rstd_from_ss(P, small[:, 16:24], small[:, 24:32], 64, "ss8", "rstd8")
            P.tt(sq[:].rearrange("p (g d) -> p g d", g=8), bkB[:, :].rearrange("p (g d) -> p g d", g=8),
                 small[:, 24:32].unsqueeze(2).broadcast_to([128, 8, 64]), ALU.mult, reads=[bkB.name, "rstd8"], writes=["sq"])
            P.tt(qkn[:], sq[:], Gqk[:], ALU.mult, reads=["sq", "Gqk"], writes=["qkn"])
            P.copy("act", Vm[:, i, :, 0:64], bkC[:, 0:256].rearrange("p (a d) -> p a d", a=4), reads=[bkC.name], writes=["V%d" % i])
            for g in range(4):
                P.tr(bkB[0:64, g * 128:(g + 1) * 128], qkn[:, g * 64:(g + 1) * 64], idf[:], reads=["qkn", "idf"], writes=[bkB.name])
                P.tr(bkC[0:64, g * 128:(g + 1) * 128], qkn[:, 256 + g * 64:256 + (g + 1) * 64], idf[:], reads=["qkn", "idf"], writes=[bkC.name])
            P.copy("dve", QT[0:64, :, :], bkB[0:64, :].rearrange("p (a t) -> p a t", a=4), reads=[bkB.name], writes=["QT"])
            P.copy("dve", QTf[:], bkB[0:64, :].rearrange("p (a t) -> p a t", a=4), reads=[bkB.name], writes=["QTf"])
            P.copy("dve", KT[0:64, :, i * 128:(i + 1) * 128], bkC[0:64, :].rearrange("p (a t) -> p a t", a=4), reads=[bkC.name], writes=["K%d" % i])
            P.stt(QT[96:100, :, :], qad[96:100, :, :], float(i), qab[96:100, :, :], ALU.mult, ALU.add, reads=["qab", "qad"], writes=["QT"])
            for a in range(4):
                P.mm(bkG[:, a * 32:(a + 1) * 32], QTf[:, a, :], kmT[:, a, :], True, True, reads=["QTf", "kmT"], writes=[bkG.name])
            for a in range(4):
                P.tt(gm[:], bkG[:, a * 32:(a + 1) * 32], FUT[:], ALU.add, reads=[bkG.name, "FUT"], writes=["gm"])
                P.op("dve", lambda: nc.vector.max(out=top8[:], in_=gm[:]), reads=["gm"], writes=["top8"])
                P.ts(gm[:], gm[:], top8[:, 2:3], 30000.0, ALU.is_ge, ALU.mult, reads=["gm", "top8"], writes=["gm"])
                P.stt(Bw[:, a, 64:96], gm[:], -30000.0, OWN[:], ALU.add, ALU.max, reads=["gm", "OWN"], writes=["Bw"])
            for a in range(4):
                P.tr(bkB[0:96, a * 128:(a + 1) * 128], Bw[:, a, :], idf[:], reads=["Bw", "idf", "QT", "QTf"], writes=[bkB.name])
            P.copy("dve", QT[64:96, :, :], bkB[64:96, :].rearrange("p (a t) -> p a t", a=4), reads=[bkB.name], writes=["QT"])
            for a in range(4):
                P.mm(bkG[0:64, 128 + a:129 + a], qkn[:, 256 + a * 64:256 + (a + 1) * 64], onesf[:, 0:1], True, True,
                     reads=["qkn", "onesf"], writes=[bkG.name])
            if blk < 32:
                P.stt(kmT[:, :, blk], bkG[0:64, 128:132], 1.0 / 256.0, kmT[:, :, blk], ALU.mult, ALU.add, reads=[bkG.name, "kmT"], writes=["kmT"])
            ngrp = i // 4 + 1
            items = [(a, g) for a in range(4) for g in range(ngrp)]
            psOb = (bkA, bkD)

            def emit_S(a, g):
                kts = list(range(4 * g, min(4 * g + 4, i + 1)))
                ib = (a * ngrp + g) % 2
                bank = (bkE, bkF)[ib]
                for jj, kt in enumerate(kts):
                    P.mm(bank[:, jj * 128:(jj + 1) * 128], KT[0:100, a, kt * 128:(kt + 1) * 128], QT[0:100, a, :],
                         True, kt != i, reads=["K%d" % kt, "KTaug", "QT"], writes=[bank.name])
                    if kt == i:
                        P.mm(bank[:, jj * 128:(jj + 1) * 128], idb[:], negb[:], False, True, reads=["idb", "negb"], writes=[bank.name])
                P.act(PT[ib][:, 0:len(kts) * 128], bank[:, 0:len(kts) * 128], AF.Exp, reads=[bank.name], writes=["PT%d" % ib])

            def emit_PV(a, g):
                kts = list(range(4 * g, min(4 * g + 4, i + 1)))
                ib = (a * ngrp + g) % 2
                po = psOb[a % 2]
                for jj, kt in enumerate(kts):
                    P.mm(po[:, 0:65], PT[ib][:, jj * 128:(jj + 1) * 128], Vm[:, kt, a, :], kt == 0, kt == i,
                         reads=["PT%d" % ib, "V%d" % kt, "Vones"], writes=[po.name])
                if g == ngrp - 1:
                    P.op("dve", lambda: nc.vector.reciprocal(out=small[:, 12 + a:13 + a], in_=po[:, 64:65]), reads=[po.name], writes=["rdb%d" % a])
                    P.ts(ym[:, 256 + a * 64:256 + (a + 1) * 64], po[:, 0:64], small[:, 12 + a:13 + a], None, ALU.mult, None,
                         reads=[po.name, "rdb%d" % a], writes=[yk])

            emit_S(*items[0])
            for ix in range(len(items)):
                if ix + 1 < len(items):
                    emit_S(*items[ix + 1])
                emit_PV(*items[ix])
            P.dma("sp", over["ytile"](i) if "ytile" in over else y_d[i * 128:(i + 1) * 128, :], ym[:], reads=[yk], writes=["yout"])
        for jx in range(len(P.dsem) if standalone else 0):
            if P.dcnt[jx] > 0:
                P._wait("sp", ("D%d" % jx, P.dsem[jx], 16 * P.dcnt[jx]))
        print("mix0 ninst", P.ninst, "nwait", P.nwait)
    return nc


def prep_mix0(inp, layer, x_b, c_b, hh, S):
    e = layer // 2
    w = inp["ev_w_in"][e]
    mh = [2 * hh, 2 * hh + 1]; bh = [4 * hh + a for a in range(4)]
    cols = []
    for base in (0, 512):
        for m in mh:
            cols.append(w[:, base + m * 128:base + (m + 1) * 128])
    for base in (1024, 1536):
        for m in mh:
            cols.append(w[:, base + m * 128:base + (m + 1) * 128])
    for base in (2056, 2568):
        for a in bh:
            cols.append(w[:, base + a * 64:base + (a + 1) * 64])
    for a in bh:
        cols.append(w[:, 3080 + a * 64:3080 + (a + 1) * 64])
    cols.append(w[:, 2048 + mh[0]:2048 + mh[0] + 2])
    cols.append(w[:, 2052 + mh[0]:2052 + mh[0] + 2])
    win = np.ascontiguousarray(np.concatenate(cols, axis=1))
    cw = inp["ev_conv_w"][e]
    convw = np.zeros((128, 4, 4), np.float32)
    for ai, (base, m) in enumerate([(0, mh[0]), (0, mh[1]), (512, mh[0]), (512, mh[1])]):
        convw[:, ai, :] = cw[:, base + m * 128:base + (m + 1) * 128].T
    gb = np.concatenate([inp["ev_igate_b"][e][mh[0]:mh[0] + 2], inp["ev_fgate_b"][e][mh[0]:mh[0] + 2]])[None, :]
    mg = inp["ev_mnorm_g"][e][mh[0]:mh[0] + 2].reshape(1, 256)
    qn = inp["ev_qn_g"][e]; kn = inp["ev_kn_g"][e]
    gqk = np.concatenate([qn] * 4 + [kn] * 4)[None, :]
    ident, negm, ka = mix1_consts(S)
    sl = slopes8()[4 * hh:4 * hh + 4]
    qab, qad = alibi_q_rows(sl)
    li = np.arange(128)
    trile = (li[:, None] <= li[None, :]).astype(np.float32)
    sgt = (li[:, None] > li[None, :]).astype(np.float32)
    t = np.arange(S)
    kblk = (np.arange(32)[:, None] == (t // 256)[None, :]).astype(np.float32)
    nb = np.arange(32)
    fut = np.stack([np.where(nb >= b, NEG, 0.0) for b in range(33)]).astype(np.float32)
    own = np.stack([np.where(nb == b, 0.0, NEG) for b in range(33)]).astype(np.float32)
    f = np.ascontiguousarray
    return {
        "x": f(x_b), "c": f(c_b.reshape(8, 128).T), "adaw": f(inp["ada_w"][layer][:, 0:2048]), "adab": f(inp["ada_b"][layer][None, 0:2048]),
        "ng": f(inp["norm_mix_g"][layer][None, :]), "win": win, "convw": f(convw.reshape(128, 16)), "gb": f(gb.astype(np.float32)),
        "mg": f(mg), "gqk": f(gqk.astype(np.float32)), "ident": ident, "negm": negm, "posm": f(-negm), "trile": trile, "sgt": sgt,
        "qab": qab, "qad": qad, "ka": ka, "kblk": kblk, "fut": fut, "own": own,
    }


PAIRS = [[0, 1], [2, 3], [4, 5], [6, 7]]


def build_fused(S):
    NTOK = S // 2
    NCH = max(1, (S * 512 * 4) // (2 << 20))
    RY = S // NCH
    RX = NTOK // NCH
    nc = bass.Bass("TRN2", target_bir_lowering=False)
    dr = lambda n, shp: nc.dram_tensor(n, shp, F32).ap()
    y0c = [dr("y0c%d" % j, [RY, 512]) for j in range(NCH)]; yg0c = [dr("yg0c%d" % j, [2 * RY, 512]) for j in range(NCH)]
    y1c = [dr("y1c%d" % j, [RY, 512]) for j in range(NCH)]; yg1c = [dr("yg1c%d" % j, [2 * RY, 512]) for j in range(NCH)]
    x1c = [dr("x1c%d" % j, [RX, D]) for j in range(NCH)]; x1gc = [dr("x1gc%d" % j, [2 * RX, D]) for j in range(NCH)]

    def ytile(ch):
        return lambda i: ch[(i * 128) // RY][(i * 128) % RY:(i * 128) % RY + 128, :]

    def ygtile(ch):
        def f(r, q, r0):
            t = q * NTOK + r0
            return ch[t // RY][r * RY + t % RY:r * RY + t % RY + 128, :]
        return f

    def x1tile(r0):
        return x1c[r0 // RX][r0 % RX:r0 % RX + 128, :]

    def x1gtile(i):
        t = i * 128
        rank, loc = t // NTOK, t % NTOK
        return x1gc[loc // RX][rank * RX + loc % RX:rank * RX + loc % RX + 128, :]

    with ExitStack() as sems:
        Ps = [Prog(nc, None, 16, p, sems) for p in ("a_", "b_", "c_", "d_")]
        ccs = [sems.enter_context(nc.semaphore("cc%d" % i)) for i in range(3)]

        def exchange(Pprev, Pnext, srcs, dsts, cs):
            toks = Pprev.all_tokens()
            Pnext.wait_all(toks, engines=["pool"])
            for a_, d_ in zip(srcs, dsts):
                nc.gpsimd.collective_compute("AllGather", ALU.bypass, replica_groups=PAIRS, ins=[a_.opt()], outs=[d_.opt()]).then_inc(cs)
            Pnext.wait_all(toks + [("CC", cs, len(srcs))], engines=["pe", "act", "dve", "sp", "pool"])

        build_mix0(S, nc, Ps[0], "a_", over={"ytile": ytile(y0c)})
        exchange(Ps[0], Ps[1], y0c, yg0c, ccs[0])
        build_ffn(NTOK, nc, Ps[1], "b_", over={"yg": ygtile(yg0c), "otile": x1tile})
        exchange(Ps[1], Ps[2], x1c, x1gc, ccs[1])
        build_mix1(S, nc, Ps[2], "c_", over={"xtile": x1gtile, "ytile": ytile(y1c)})
        exchange(Ps[2], Ps[3], y1c, yg1c, ccs[2])
        build_ffn(NTOK, nc, Ps[3], "d_", over={"xtile": x1tile, "yg": ygtile(yg1c)})
        Ps[3].wait_all(Ps[3].all_tokens(), engines=["sp"])
        print("fused ninst", sum(p.ninst for p in Ps), "nwait", sum(p.nwait for p in Ps))
    return nc


_CACHE = {}


def _get(name, fn, *a):
    key = (name,) + a
    if key not in _CACHE:
        _CACHE[key] = fn(*a)
    return _CACHE[key]


def kernel(**inputs):
    inp = {k: np.asarray(v) for k, v in inputs.items()}
    x = inp["x"].astype(np.float32, copy=False)
    c = inp["c"]
    B, S, _ = x.shape
    NTOK = S // 2
    nc = _get("fused", build_fused, S)
    wo0 = inp["ev_w_out"][0]
    wo0p = np.concatenate([wo0[0:256], wo0[512:768], wo0[256:512], wo0[768:1024]], axis=0)
    maps = []
    for k in range(8):
        b, hh = k // 2, k % 2
        hsel = np.zeros((128, 2), np.float32); hsel[:, hh] = 1.0
        m = {}
        for pfx, d in (("a_", prep_mix0(inp, 0, x[b], c[b], hh, S)),
                       ("b_", prep_ffn(inp, 0, x[b, hh * NTOK:(hh + 1) * NTOK], None, c[b], wo0p)),
                       ("c_", prep_mix1(inp, 1, None, c[b], hh, S)),
                       ("d_", prep_ffn(inp, 1, None, None, c[b], inp["od_w_out"][0]))):
            for kk, v in d.items():
                if v is not None:
                    m[pfx + kk] = v
        m["b_hsel"] = hsel; m["d_hsel"] = hsel
        maps.append(m)
    res = run_bass_kernel_spmd(nc, maps, core_ids=list(range(8)))
    out = np.concatenate([res.results[k]["o"] for k in range(8)], axis=0).reshape(B, S, D)
    return out.astype(np.float32)
```

```python
import numpy as np
from contextlib import ExitStack
import concourse.bass as bass
import concourse.mybir as mybir
from concourse.bass_utils import run_bass_kernel_spmd

F32 = mybir.dt.float32
BF16 = mybir.dt.bfloat16
AF = mybir.ActivationFunctionType
ALU = mybir.AluOpType
AX = mybir.AxisListType

D = 1024
EPS = 1e-6
NEG = -30000.0
import os
STAGE = int(os.environ.get('KSTAGE', '9'))
SUB = int(os.environ.get('KSUB', '0'))
KTENG = os.environ.get('KTENG', 'dve')
NPE = int(os.environ.get('KNPE', '0'))


class Prog:
    def __init__(self, nc, stack, n_dma_sems=24, pfx="", sem_stack=None):
        self.nc = nc
        self.st = stack
        self.pfx = pfx
        sem_stack = sem_stack if sem_stack is not None else stack
        self.eng = {"pe": nc.tensor, "act": nc.scalar, "dve": nc.vector, "pool": nc.gpsimd, "sp": nc.sync}
        self.sem = {k: sem_stack.enter_context(nc.semaphore(pfx + "s_" + k)) for k in self.eng}
        self.cnt = {k: 0 for k in self.eng}
        self.dsem = [sem_stack.enter_context(nc.semaphore(pfx + "d%d" % i)) for i in range(n_dma_sems)]
        self.dcnt = [0] * n_dma_sems
        self.dnext = 0
        self.waited = {k: {} for k in self.eng}
        self.lastw = {}
        self.readers = {}
        self.ninst = 0
        self.nwait = 0

    def sb(self, name, shape, dt):
        return self.st.enter_context(self.nc.sbuf_tensor(self.pfx + name, shape, dt))

    def ps(self, name, shape, dt):
        return self.st.enter_context(self.nc.psum_tensor(self.pfx + name, shape, dt))

    def all_tokens(self):
        toks = [("E" + e, self.sem[e], self.cnt[e]) for e in self.eng if self.cnt[e] > 0]
        toks += [("D%d" % j, self.dsem[j], 16 * self.dcnt[j]) for j in range(len(self.dsem)) if self.dcnt[j] > 0]
        return toks

    def wait_all(self, toks, engines=None):
        for e in (engines or self.eng):
            for sid, sm, v in toks:
                self.eng[e].wait_ge(sm, v)
                self.nwait += 1

    def _wait(self, e, tok):
        if tok is None:
            return
        sid, s, v = tok
        if self.waited[e].get(sid, 0) >= v:
            return
        self.eng[e].wait_ge(s, v)
        self.nwait += 1
        self.waited[e][sid] = v

    def _deps(self, e, reads, writes):
        for k in reads:
            self._wait(e, self.lastw.get(k))
        for k in writes:
            self._wait(e, self.lastw.get(k))
            for t in self.readers.get(k, ()):
                self._wait(e, t)

    def _commit(self, tok, reads, writes):
        for k in writes:
            self.lastw[k] = tok
            self.readers[k] = []
        for k in reads:
            lst = self.readers.setdefault(k, [])
            lst.append(tok)
            if len(lst) > 16:
                best = {}
                for t in lst:
                    if t[0] not in best or best[t[0]][2] < t[2]:
                        best[t[0]] = t
                self.readers[k] = list(best.values())

    def op(self, e, ins_fn, reads=(), writes=()):
        self._deps(e, reads, writes)
        ins = ins_fn()
        self.cnt[e] += 1
        ins.then_inc(self.sem[e], 1)
        tok = ("E" + e, self.sem[e], self.cnt[e])
        self.waited[e]["E" + e] = self.cnt[e] - 1
        self._commit(tok, reads, writes)
        self.ninst += 1
        return tok

    def dma(self, q, out, in_, reads=(), writes=(), **kw):
        j = self.dnext
        self.dnext = (self.dnext + 1) % len(self.dsem)
        if self.dcnt[j] > 0:
            self._wait(q, ("D%d" % j, self.dsem[j], 16 * self.dcnt[j]))
        self._deps(q, reads, writes)
        ins = self.eng[q].dma_start(out=out, in_=in_, **kw)
        self.dcnt[j] += 1
        ins.then_inc(self.dsem[j], 16)
        tok = ("D%d" % j, self.dsem[j], 16 * self.dcnt[j])
        self.waited[q]["D%d" % j] = 16 * (self.dcnt[j] - 1)
        self._commit(tok, reads, writes)
        self.ninst += 1
        return tok

    def finish(self, keys, e="sp"):
        for k in keys:
            self._wait(e, self.lastw.get(k))

    def mm(self, out, lhsT, rhs, start, stop, reads, writes):
        nc = self.nc
        return self.op("pe", lambda: nc.tensor.matmul(out, lhsT=lhsT, rhs=rhs, start=start, stop=stop),
                       reads=reads, writes=writes)

    def tr(self, out, in_, ident, reads, writes):
        nc = self.nc
        return self.op("pe", lambda: nc.tensor.transpose(out=out, in_=in_, identity=ident), reads=reads, writes=writes)

    def act(self, out, in_, func, reads, writes, **kw):
        nc = self.nc
        return self.op("act", lambda: nc.scalar.activation(out=out, in_=in_, func=func, **kw), reads=reads, writes=writes)

    def tt(self, out, in0, in1, op, reads, writes, e="dve"):
        eng = self.eng[e]
        return self.op(e, lambda: eng.tensor_tensor(out=out, in0=in0, in1=in1, op=op), reads=reads, writes=writes)

    def ts(self, out, in0, s1, s2, op0, op1, reads, writes, e="dve", **kw):
        eng = self.eng[e]
        if op1 is None:
            return self.op(e, lambda: eng.tensor_scalar(out=out, in0=in0, scalar1=s1, scalar2=None, op0=op0, **kw),
                           reads=reads, writes=writes)
        return self.op(e, lambda: eng.tensor_scalar(out=out, in0=in0, scalar1=s1, scalar2=s2, op0=op0, op1=op1, **kw),
                       reads=reads, writes=writes)

    def stt(self, out, in0, scalar, in1, op0, op1, reads, writes):
        nc = self.nc
        return self.op("dve", lambda: nc.vector.scalar_tensor_tensor(out=out, in0=in0, scalar=scalar, in1=in1, op0=op0, op1=op1),
                       reads=reads, writes=writes)

    def copy(self, e, out, in_, reads, writes):
        nc = self.nc
        if e == "act":
            return self.op("act", lambda: nc.scalar.copy(out=out, in_=in_), reads=reads, writes=writes)
        eng = self.eng[e]
        return self.op(e, lambda: eng.tensor_copy(out=out, in_=in_), reads=reads, writes=writes)

    def memset(self, e, ap, val, writes):
        eng = self.eng[e]
        return self.op(e, lambda: eng.memset(ap, val), writes=writes)


def load_consts(P, ident_d):
    idf = P.sb("idf", [128, 128], F32)
    idb = P.sb("idb", [128, 128], BF16)
    P.dma("sp", idf[:], ident_d[:, :], writes=["idf"])
    P.dma("pool", idb[:], ident_d[:, :], writes=["idb"])
    return idf, idb


def compute_mod(P, c_d, adaw_d, adab_d, ncols, ps_bank, name):
    nc = P.nc
    CW = 256
    ccol = P.sb(name + "_ccol", [128, 8], F32)
    cact = P.sb(name + "_cact", [128, 8], F32)
    crep = P.sb(name + "_crep", [128, 8, 128], F32)
    mod = P.sb(name + "_mod", [128, ncols], F32)
    brep = P.sb(name + "_brep", [128, CW], F32)
    wch = P.sb(name + "_wch", [128, 8, CW], F32)
    P.dma("sp", ccol[:], c_d[:, :], writes=[name + "ccol"])
    P.act(cact[:], ccol[:], AF.Silu, reads=[name + "ccol"], writes=[name + "cact"])
    P.copy("dve", crep[:], cact[:].unsqueeze(2).broadcast_to([128, 8, 128]), reads=[name + "cact"], writes=[name + "crep"])
    for j in range(ncols // CW):
        P.dma("sp", wch[:], adaw_d[:, j * CW:(j + 1) * CW].rearrange("(c p) n -> p c n", p=128), writes=[name + "wch"])
        P.dma("sp", brep[:], adab_d[0:1, j * CW:(j + 1) * CW].partition_broadcast(128), writes=[name + "brep"])
        for c in range(8):
            P.mm(ps_bank[:, 0:CW], crep[:, c, :], wch[:, c, :], c == 0, c == 7,
                 reads=[name + "crep", name + "wch"], writes=[ps_bank.name])
        P.tt(mod[:, j * CW:(j + 1) * CW], ps_bank[:, 0:CW], brep[:], ALU.add,
             reads=[ps_bank.name, name + "brep"], writes=[name + "mod"])
    return mod


def rstd_from_ss(P, ss, rstd, n, key_in, key_out, width=1):
    nc = P.nc
    P.ts(rstd, ss, 1.0 / n, EPS, ALU.mult, ALU.add, reads=[key_in], writes=[key_out])
    P.act(rstd, rstd, AF.Sqrt, reads=[key_out], writes=[key_out])
    P.op("dve", lambda: nc.vector.reciprocal(out=rstd, in_=rstd), reads=[key_out], writes=[key_out])


def norm_mod_T(P, xt, xkey, GM, SH, gkeys, hb, hT, psT, idb, junk, small, tag, act_copy=True):
    nc = P.nc
    ss = small[:, 0:1]
    rstd = small[:, 1:2]
    P.act(junk[:], xt, AF.Square, reads=[xkey], writes=["junk" + tag, "ss" + tag], accum_out=ss)
    rstd_from_ss(P, ss, rstd, D, "ss" + tag, "rstd" + tag)
    P.stt(junk[:], xt, rstd, GM[:], ALU.mult, ALU.mult, reads=[xkey, "rstd" + tag] + gkeys, writes=["junk" + tag])
    P.tt(hb[:], junk[:], SH[:], ALU.add, reads=["junk" + tag] + gkeys, writes=["hb" + tag])
    for c in range(8):
        P.tr(psT[:, c * 128:(c + 1) * 128], hb[:, c * 128:(c + 1) * 128], idb[:], reads=["hb" + tag, "idb"], writes=[psT.name])
    P.copy("act" if act_copy else "dve", hT[:].rearrange("p c t -> p (c t)"), psT[:, :], reads=[psT.name], writes=["hT" + tag])


def build_mix1(S, nc=None, P=None, pfx="", over=None):
    over = over or {}
    standalone = nc is None
    NT = S // 128
    if standalone:
        nc = bass.Bass("TRN2", target_bir_lowering=False)
    dt = lambda n, s: over[n] if n in over else nc.dram_tensor(pfx + n, s, F32, kind="ExternalInput").ap()
    x_d = None if "xtile" in over else dt("x", [S, D])
    c_d = dt("c", [128, 8]); adaw_d = dt("adaw", [D, 2048]); adab_d = dt("adab", [1, 2048])
    ng_d = dt("ng", [1, D]); win_d = dt("win", [D, 4 * 384]); gqk_d = dt("gqk", [1, 256]); lam_d = dt("lam", [1, 256])
    ong_d = dt("ong", [1, 128]); ident_d = dt("ident", [128, 128]); negm_d = dt("negm", [128, 128])
    qab_d = dt("qab", [4, 4 * 128]); qad_d = dt("qad", [4, 4 * 128]); ka_d = dt("ka", [4, S])
    laminit_d = dt("laminit", [1, 2])
    y_d = None if "ytile" in over else nc.dram_tensor("y", [S, 512], F32, kind="ExternalOutput").ap()
    with ExitStack() as st:
        if P is None:
            P = Prog(nc, st)
        P.st = st
        idf, idb = load_consts(P, ident_d)
        banks = [P.ps("bk%d" % i, [128, 512], F32) for i in range(7)]
        psT = P.ps("psT", [128, 1024], BF16)
        mod = compute_mod(P, c_d, adaw_d, adab_d, 2048, banks[0], "m")
        GM = P.sb("GM", [128, D], F32)
        P.dma("sp", GM[:], ng_d[0:1, :].partition_broadcast(128), writes=["GM"])
        P.stt(GM[:], mod[:, 1024:2048], 1.0, GM[:], ALU.add, ALU.mult, reads=["mmod", "GM"], writes=["GM"])
        SH = mod[:, 0:1024]
        gk = ["GM", "mmod"]
        negb = P.sb("negb", [128, 128], BF16)
        P.dma("pool", negb[:], negm_d[:, :], writes=["negb"])
        Gqk = P.sb("Gqk", [128, 256], F32)
        P.dma("sp", Gqk[:], gqk_d[0:1, :].partition_broadcast(128), writes=["Gqk"])
        P.ts(Gqk[:, 0:128], Gqk[:, 0:128], 0.125, None, ALU.mult, None, reads=["Gqk"], writes=["Gqk"])
        ONG = P.sb("ONG", [128, 128], F32)
        P.dma("sp", ONG[:], ong_d[0:1, :].partition_broadcast(128), writes=["ONG"])
        lamt = P.sb("lamt_s", [128, 256], F32)
        lami = P.sb("lami", [128, 2], F32)
        lsm = P.sb("lsm", [128, 4], F32)
        P.dma("sp", lamt[:], lam_d[0:1, :].partition_broadcast(128), writes=["lamt"])
        P.dma("sp", lami[:], laminit_d[0:1, :].partition_broadcast(128), writes=["lami"])
        lv = lamt[:].rearrange("p (a d) -> p a d", a=4)
        P.tt(lamt[:, 0:64], lv[:, 0, :], lv[:, 1, :], ALU.mult, reads=["lamt"], writes=["lamt"])
        P.tt(lamt[:, 128:192], lv[:, 2, :], lv[:, 3, :], ALU.mult, reads=["lamt"], writes=["lamt"])
        P.op("dve", lambda: nc.vector.tensor_reduce(out=lsm[:, 0:1], in_=lamt[:, 0:64], axis=AX.X, op=ALU.add), reads=["lamt"], writes=["lsm"])
        P.op("dve", lambda: nc.vector.tensor_reduce(out=lsm[:, 1:2], in_=lamt[:, 128:192], axis=AX.X, op=ALU.add), reads=["lamt"], writes=["lsm"])
        P.act(lsm[:, 0:2], lsm[:, 0:2], AF.Exp, reads=["lsm"], writes=["lsm"])
        P.tt(lsm[:, 2:3], lsm[:, 1:2], lsm[:, 0:1], ALU.subtract, reads=["lsm"], writes=["lsm"])
        P.tt(lsm[:, 3:4], lsm[:, 2:3], lami[:, 0:1], ALU.subtract, reads=["lsm", "lami"], writes=["neglam"])
        neglam = lsm[:, 3:4]
        P.ts(ONG[:], ONG[:], lami[:, 1:2], None, ALU.mult, None, reads=["ONG", "lami"], writes=["ONG"])

        KT = P.sb("KT", [68, 2, S], BF16)
        QT = P.sb("QT", [68, 2, 128], BF16)
        qab = P.sb("qab_s", [68, 4, 128], F32)
        qad = P.sb("qad_s", [68, 4, 128], F32)
        P.dma("sp", qab[64:68, :, :], qab_d.rearrange("r (h t) -> r h t", h=4), writes=["qab"])
        P.dma("sp", qad[64:68, :, :], qad_d.rearrange("r (h t) -> r h t", h=4), writes=["qad"])
        for c in range(2):
            P.dma("pool", KT[64:68, c, :], ka_d[:, :], writes=["KTaug"])
        Vaug = P.sb("Vaug", [128, NT, 129], BF16)
        P.memset("pool", Vaug[:, :, 128:129], 1.0, writes=["Vones"])
        wsb = P.sb("wsb", [128, 8, 384], BF16)
        xts = [P.sb("xt%d" % i, [128, D], F32) for i in range(2)]
        junk = P.sb("junk", [128, D], F32)
        hb = P.sb("hb", [128, D], BF16)
        hT = P.sb("hT", [128, 8, 128], BF16)
        small = P.sb("small", [128, 16], F32)
        sq = P.sb("sq", [128, 256], F32)
        qkn = P.sb("qkn", [128, 256], BF16)
        PT = [[P.sb("PT%d%d" % (c, b), [128, 512], BF16) for b in range(2)] for c in range(2)]
        osb = P.sb("osb", [128, 128], F32)
        o2 = P.sb("o2", [128, 128], F32)
        yo = [P.sb("yo%d" % i, [128, 128], F32) for i in range(2)]
        psZ = banks[0]
        psS = [[banks[1], banks[2]], [banks[3], banks[4]]]
        psO = [banks[5], banks[6]]
        it = 0
        for j in range(4 if STAGE >= 1 else 0):
            P.dma("pool", wsb[:], win_d[:, j * 384:(j + 1) * 384].rearrange("(c p) n -> p c n", p=128),
                  writes=["wsb"])
            for i in range(NT):
                xt = xts[it % 2]; xk = "xt%d" % (it % 2)
                P.dma("sp", xt[:], over["xtile"](i) if "xtile" in over else x_d[i * 128:(i + 1) * 128, :], writes=[xk])
                norm_mod_T(P, xt[:], xk, GM, SH, gk, hb, hT, psT, idb, junk, small, "", act_copy=True)
                if STAGE < 2:
                    continue
                for c in range(8):
                    P.mm(psZ[:, 0:384], hT[:, c, :], wsb[:, c, :], c == 0, c == 7, reads=["hT", "wsb"], writes=[psZ.name])
                if STAGE < 3:
                    continue
                if SUB == 5:
                    continue
                P.act(sq[:], psZ[:, 0:256], AF.Square, reads=[psZ.name], writes=["sq"])
                P.op("dve", lambda: nc.vector.tensor_reduce(out=small[:, 4:8], in_=sq[:].rearrange("p (g d) -> p g d", g=4), axis=AX.X, op=ALU.add),
                     reads=["sq"], writes=["ss4"])
                rstd_from_ss(P, small[:, 4:8], small[:, 8:12], 64, "ss4", "rstd4")
                P.tt(sq[:].rearrange("p (g d) -> p g d", g=4), psZ[:, 0:256].rearrange("p (g d) -> p g d", g=4),
                     small[:, 8:12].unsqueeze(2).broadcast_to([128, 4, 64]), ALU.mult, reads=[psZ.name, "rstd4"], writes=["sq"])
                P.tt(qkn[:], sq[:], Gqk[:], ALU.mult, reads=["sq", "Gqk"], writes=["qkn"])
                if SUB == 2:
                    continue
                P.copy("act", Vaug[:, i, 0:128], psZ[:, 256:384], reads=[psZ.name], writes=["V%d" % i])
                if SUB == 3:
                    continue
                for g in range(4):
                    P.tr(psT[0:64, g * 128:(g + 1) * 128], qkn[:, g * 64:(g + 1) * 64], idb[:], reads=["qkn", "idb"], writes=[psT.name])
                if SUB == 4:
                    continue
                P.copy("dve", QT[0:64, :, :], psT[0:64, 0:256].rearrange("p (c t) -> p c t", c=2), reads=[psT.name], writes=["QT"])
                if SUB == 6:
                    continue
                for c in range(2):
                    P.copy(KTENG, KT[0:64, c, i * 128:(i + 1) * 128], psT[0:64, 256 + c * 128:256 + (c + 1) * 128],
                           reads=[psT.name], writes=["K%d" % i])
                if SUB == 7:
                    continue
                if SUB != 1:
                  P.stt(QT[64:68, :, :], qad[64:68, j:j + 1, :].broadcast_to([4, 2, 128]), float(i),
                        qab[64:68, j:j + 1, :].broadcast_to([4, 2, 128]), ALU.mult, ALU.add, reads=["qab", "qad"], writes=["QT"])
                if STAGE < 4:
                    continue
                ngrp = i // 4 + 1
                items = [(g, c) for g in range(ngrp) for c in range(2)]

                def emit_S(g, c):
                    kts = list(range(4 * g, min(4 * g + 4, i + 1)))
                    bank = psS[c][g % 2]
                    for jj, kt in enumerate(kts):
                        P.mm(bank[:, jj * 128:(jj + 1) * 128], KT[0:68, c, kt * 128:(kt + 1) * 128], QT[0:68, c, :],
                             True, kt != i, reads=["K%d" % kt, "KTaug", "QT"], writes=[bank.name])
                        if kt == i:
                            P.mm(bank[:, jj * 128:(jj + 1) * 128], idb[:], negb[:], False, True,
                                 reads=["idb", "negb"], writes=[bank.name])
                    pt = PT[c][g % 2]; pk = "PT%d%d" % (c, g % 2)
                    P.act(pt[:, 0:len(kts) * 128], bank[:, 0:len(kts) * 128], AF.Exp, reads=[bank.name], writes=[pk])

                def emit_PV(g, c):
                    kts = list(range(4 * g, min(4 * g + 4, i + 1)))
                    pt = PT[c][g % 2]; pk = "PT%d%d" % (c, g % 2)
                    for jj, kt in enumerate(kts):
                        P.mm(psO[c][:, 0:129], pt[:, jj * 128:(jj + 1) * 128], Vaug[:, kt, :], kt == 0, kt == i,
                             reads=[pk, "V%d" % kt, "Vones"], writes=[psO[c].name])

                emit_S(*items[0])
                for ix in range(len(items)):
                    if ix + 1 < len(items):
                        emit_S(*items[ix + 1])
                    emit_PV(*items[ix])
                if STAGE < 5:
                    continue
                P.op("dve", lambda: nc.vector.reciprocal(out=small[:, 12:13], in_=psO[0][:, 128:129]), reads=[psO[0].name], writes=["rd0"])
                P.op("dve", lambda: nc.vector.reciprocal(out=small[:, 13:14], in_=psO[1][:, 128:129]), reads=[psO[1].name], writes=["rd1"])
                P.ts(o2[:], psO[1][:, 0:128], small[:, 13:14], neglam, ALU.mult, ALU.mult, reads=[psO[1].name, "rd1", "neglam"], writes=["o2"])
                P.stt(osb[:], psO[0][:, 0:128], small[:, 12:13], o2[:], ALU.mult, ALU.add, reads=[psO[0].name, "rd0", "o2"], writes=["osb"])
                P.act(o2[:], osb[:], AF.Square, reads=["osb"], writes=["o2", "sso"], accum_out=small[:, 14:15])
                rstd_from_ss(P, small[:, 14:15], small[:, 15:16], 128, "sso", "rstdo")
                yt = yo[it % 2]; yk = "yo%d" % (it % 2)
                P.stt(yt[:], osb[:], small[:, 15:16], ONG[:], ALU.mult, ALU.mult, reads=["osb", "rstdo", "ONG"], writes=[yk])
                ydst = over["ytile"](i)[:, j * 128:(j + 1) * 128] if "ytile" in over else y_d[i * 128:(i + 1) * 128, j * 128:(j + 1) * 128]
                P.dma("sp", ydst, yt[:], reads=[yk], writes=["yout"])
                it += 1
        P.finish(["yout"])
        for k, v in P.lastw.items():
            pass
        for jx in range(len(P.dsem) if standalone else 0):
            if P.dcnt[jx] > 0:
                P._wait("sp", ("D%d" % jx, P.dsem[jx], 16 * P.dcnt[jx]))
        print("mix1 ninst", P.ninst, "nwait", P.nwait)
    return nc


def mix1_consts(S):
    ident = np.eye(128, dtype=np.float32)
    k_idx = np.arange(128)[:, None]; q_idx = np.arange(128)[None, :]
    negm = np.where(k_idx > q_idx, NEG, 0.0).astype(np.float32)
    ka = np.zeros((4, S), np.float32)
    t = np.arange(S)
    ka[0] = 1.0; ka[1] = t % 128; ka[2] = t // 128; ka[3] = 1.0
    return ident, negm, ka


def alibi_q_rows(slopes):
    nh = len(slopes)
    qab = np.zeros((4, nh, 128), np.float32); qad = np.zeros((4, nh, 128), np.float32)
    for h, s in enumerate(slopes):
        qab[0, h] = -s * np.arange(128); qab[1, h] = s; qab[2, h] = 128.0 * s
        qad[3, h] = -128.0 * s
    return qab.reshape(4, nh * 128), qad.reshape(4, nh * 128)


def slopes8():
    return [2.0 ** (-8.0 * (h + 1) / 8) for h in range(8)]


def prep_mix1(inp, layer, x_b, c_b, hh, S):
    o = layer // 2
    w = inp["od_w_in"][o]
    cols = []
    for j in range(4):
        h = 4 * hh + j
        cols.append(w[:, h * 128:(h + 1) * 128])
        cols.append(w[:, 1024 + h * 128:1024 + (h + 1) * 128])
        cols.append(w[:, 2048 + h * 128:2048 + (h + 1) * 128])
    win = np.ascontiguousarray(np.concatenate(cols, axis=1))
    ident, negm, ka = mix1_consts(S)
    sl = slopes8()[4 * hh:4 * hh + 4]
    qab, qad = alibi_q_rows(sl)
    qn = inp["od_qn_g"][o]; kn = inp["od_kn_g"][o]
    gqk = np.concatenate([qn, qn, kn, kn])[None, :].astype(np.float32)
    import math
    lam_init = 0.8 - 0.6 * math.exp(-0.3 * layer)
    return {
        "x": None if x_b is None else np.ascontiguousarray(x_b), "c": np.ascontiguousarray(c_b.reshape(8, 128).T),
        "adaw": np.ascontiguousarray(inp["ada_w"][layer][:, 0:2048]), "adab": np.ascontiguousarray(inp["ada_b"][layer][None, 0:2048]),
        "ng": np.ascontiguousarray(inp["norm_mix_g"][layer][None, :]), "win": win, "gqk": gqk,
        "lam": np.ascontiguousarray(inp["od_lam"][o].reshape(1, 256)), "ong": np.ascontiguousarray(inp["od_onorm_g"][o][None, :]),
        "ident": ident, "negm": negm, "qab": qab, "qad": qad, "ka": ka,
        "laminit": np.array([[lam_init, 1.0 - lam_init]], np.float32),
    }


def build_ffn(NTOK, nc=None, P=None, pfx="", over=None):
    over = over or {}
    standalone = nc is None
    NST = NTOK // 512
    if standalone:
        nc = bass.Bass("TRN2", target_bir_lowering=False)
    dt = lambda n, s: over[n] if n in over else nc.dram_tensor(pfx + n, s, F32, kind="ExternalInput").ap()
    yg = over.get("yg"); SEQ = 2 * NTOK
    x_d = None if "xtile" in over else dt("x", [NTOK, D])
    c_d = dt("c", [128, 8])
    y_d = dt("y", [NTOK, D]) if yg is None else None
    hsel_d = dt("hsel", [128, 2]) if yg is not None else None
    adaw_d = dt("adaw", [D, 4096]); adab_d = dt("adab", [1, 4096]); ng_d = dt("ng", [1, D])
    wout_d = dt("wout", [D, D]); wq_d = dt("wq", [D, 2048]); keysT_d = dt("keysT", [128, 256])
    uT_d = dt("uT", [D, 16384]); v_d = dt("v", [16384, D]); ident_d = dt("ident", [128, 128])
    o_d = None if "otile" in over else nc.dram_tensor("o", [NTOK, D], F32, kind="ExternalOutput").ap()
    with ExitStack() as st:
        if P is None:
            P = Prog(nc, st)
        P.st = st
        idf, idb = load_consts(P, ident_d)
        banks = [P.ps("bk%d" % i, [128, 512], F32) for i in range(7)]
        psT = P.ps("psT", [128, 1024], BF16)
        mod = compute_mod(P, c_d, adaw_d, adab_d, 4096, banks[0], "m")
        GM = P.sb("GM", [128, D], F32)
        P.dma("sp", GM[:], ng_d[0:1, :].partition_broadcast(128), writes=["GM"])
        P.stt(GM[:], mod[:, 2048:3072], 1.0, GM[:], ALU.add, ALU.mult, reads=["mmod", "GM"], writes=["GM"])
        G1 = mod[:, 0:1024]; SH = mod[:, 1024:2048]; G2 = mod[:, 3072:4096]
        gk = ["GM", "mmod"]
        keysb = P.sb("keysb", [128, 256], BF16)
        P.dma("pool", keysb[:], keysT_d[:, :], writes=["keysb"])
        W = [P.sb("W%d" % i, [128, 8, 512], BF16) for i in range(2)]
        Vc = [P.sb("Vc%d" % i, [128, 4, 1024], BF16) for i in range(2)]
        wn = [0]; vn = [0]

        def loadW(src):
            i = wn[0] % 2; wn[0] += 1
            P.dma("pool", W[i][:], src.rearrange("(c p) n -> p c n", p=128), writes=["W%d" % i])
            return W[i], "W%d" % i

        xt = P.sb("xt", [128, D], F32)
        if yg is not None:
            ysf = P.sb("ysf", [128, 2, 512], F32)
            hsel = P.sb("hsel_s", [128, 2], F32)
            P.dma("sp", hsel[:], hsel_d[:, :], writes=["hsel"])
        yb = P.sb("yb", [128, D], BF16)
        yT = P.sb("yT", [128, 8, 128], BF16)
        junk = P.sb("junk", [128, D], F32)
        hb = P.sb("hb", [128, D], BF16)
        small = P.sb("small", [128, 8], F32)
        x1 = [P.sb("x1_%d" % t, [128, D], F32) for t in range(4)]
        h2T = [P.sb("h2T_%d" % t, [128, 8, 128], BF16) for t in range(4)]
        qTb = [P.sb("qTb_%d" % t, [128, 16, 128], BF16) for t in range(4)]
        NA = 8 - NPE
        s1nb = [P.sb("s1nb_%d" % t, [128, max(NA, 1), 128], F32) for t in range(4)]
        s2b = [P.sb("s2b_%d" % t, [128, max(NA, 1), 128], BF16) for t in range(4)]
        sT = [P.sb("sT_%d" % t, [128, max(2 * NPE, 1), 128], BF16) for t in range(4)]
        thr = [P.sb("thr_%d" % t, [128, 8], F32) for t in range(4)]
        nb = [P.sb("nb_%d" % t, [128, 8], F32) for t in range(4)]
        acc = [P.sb("acc_%d" % t, [128, D], F32) for t in range(4)]
        sbf = P.sb("sbf", [128, 16, 128], BF16)
        t16 = P.sb("t16", [128, 16, 16], F32)
        tmp128 = P.sb("tmp128", [128, 128], BF16)
        cand = P.sb("cand", [128, 1, 256], F32)
        cand2 = P.sb("cand2", [128, 256], F32)
        c16 = P.sb("c16", [128, 8, 16], F32)
        e16 = P.sb("e16", [128, 8, 16], F32)
        zs = P.sb("zs", [128, 8], F32)
        ef = [P.sb("ef%d" % i, [128, 512], F32) for i in range(3)]
        wb = [None] + [P.sb("wbm%d" % i, [128, 512], BF16) for i in range(1, 8)]
        Gs = [P.sb("Gs%d" % i, [128, 512], F32) for i in range(2)]
        ethr = [P.sb("ethr_%d" % t, [128, 8], F32) for t in range(4)]
        gh = [P.sb("gh%d" % i, [128, 512], BF16) for i in range(8)]
        Ab = [P.sb("Ab%d" % i, [128, 512], BF16) for i in range(2)]
        AT = [P.sb("AT%d" % i, [128, 4, 128], BF16) for i in range(2)]
        ot = junk
        psZ = [banks[0], banks[1], banks[6], banks[2]]; psH = banks[3]; psO = [banks[4], banks[5]]; zc = [0]
        sel1 = lambda c: idb[:, 4 * c:4 * c + 4].unsqueeze(2).broadcast_to([128, 4, 128])
        sel2 = idb[:, :].unsqueeze(1).broadcast_to([128, 4, 128])
        cnt = [0]
        for stile in range(NST):
            t0 = stile * 512
            wo = [loadW(wout_d[:, hf * 512:(hf + 1) * 512]) for hf in range(2)]
            for t in range(4):
                r0 = t0 + t * 128
                if yg is None:
                    P.dma("pool", yb[:], y_d[r0:r0 + 128, :], writes=["yb"])
                else:
                    for r in range(2):
                        for q in range(2):
                            P.dma("sp", ysf[:, q, :], yg(r, q, r0), writes=["ysf%d" % q])
                        P.ts(junk[:, r * 512:(r + 1) * 512], ysf[:, 0, :], hsel[:, 0:1], None, ALU.mult, None,
                             reads=["ysf0", "hsel"], writes=["junk"])
                        P.stt(yb[:, r * 512:(r + 1) * 512], ysf[:, 1, :], hsel[:, 1:2], junk[:, r * 512:(r + 1) * 512],
                              ALU.mult, ALU.add, reads=["ysf1", "hsel", "junk"], writes=["yb"])
                P.dma("sp", xt[:], over["xtile"](r0) if "xtile" in over else x_d[r0:r0 + 128, :], writes=["xt"])
                for c in range(8):
                    P.tr(psT[:, c * 128:(c + 1) * 128], yb[:, c * 128:(c + 1) * 128], idb[:], reads=["yb", "idb"], writes=[psT.name])
                P.copy("act", yT[:].rearrange("p c t -> p (c t)"), psT[:, :], reads=[psT.name], writes=["yT"])
                for hf in range(2):
                    bk = psO[hf]
                    for c in range(8):
                        P.mm(bk[:, :], yT[:, c, :], wo[hf][0][:, c, :], c == 0, c == 7, reads=["yT", wo[hf][1]], writes=[bk.name])
                    P.tt(junk[:, hf * 512:(hf + 1) * 512], bk[:, :], G1[:, hf * 512:(hf + 1) * 512], ALU.mult,
                         reads=[bk.name, "mmod"], writes=["junk"])
                P.tt(x1[t][:], junk[:], xt[:], ALU.add, reads=["junk", "xt"], writes=["x1_%d" % t])
                norm_mod_T(P, x1[t][:], "x1_%d" % t, GM, SH, gk, hb, h2T[t], psT, idb, junk, small, "", act_copy=True)
                P.lastw["h2T_%d" % t] = P.lastw["hT"]
            for g in range(4):
                wq, wqk = loadW(wq_d[:, g * 512:(g + 1) * 512])
                for t in range(4):
                    bk = psZ[(g * 4 + t) % 2]
                    for hp in range(4):
                        for c in range(8):
                            P.mm(bk[:, hp * 128:(hp + 1) * 128], wq[:, c, hp * 128:(hp + 1) * 128], h2T[t][:, c, :], c == 0, c == 7,
                                 reads=[wqk, "h2T_%d" % t], writes=[bk.name])
                    P.copy("act", qTb[t][:, 4 * g:4 * g + 4, :].rearrange("p a t -> p (a t)"), bk[:, :], reads=[bk.name], writes=["qTb_%d" % t])
            for t in range(4):
                for g in range(4):
                    bk = psZ[g % 2]
                    for a in range(4):
                        hp = 4 * g + a
                        P.mm(bk[:, a * 128:(a + 1) * 128], qTb[t][:, hp, :], keysb[:, (hp % 2) * 128:(hp % 2 + 1) * 128], True, True,
                             reads=["qTb_%d" % t, "keysb"], writes=[bk.name])
                    P.copy("act", sbf[:, 4 * g:4 * g + 4, :].rearrange("p a n -> p (a n)"), bk[:, :], reads=[bk.name], writes=["sbf"])
                for hp in range(16):
                    P.op("dve", lambda: nc.vector.max(out=t16[:, hp, 0:8], in_=sbf[:, hp, :]), reads=["sbf"], writes=["t16"])
                    P.op("dve", lambda: nc.vector.match_replace(out=tmp128[:], in_to_replace=t16[:, hp, 0:8], in_values=sbf[:, hp, :], imm_value=-1e30),
                         reads=["sbf", "t16"], writes=["tmp128"])
                    P.op("dve", lambda: nc.vector.max(out=t16[:, hp, 8:16], in_=tmp128[:]), reads=["tmp128"], writes=["t16"])
                tv = t16[:].rearrange("p (h two) k -> p h two k", two=2)
                for h in range(8):
                    P.tt(cand[:, 0, :].rearrange("p (a b) -> p a b", a=16), tv[:, h, 0, :].unsqueeze(2).broadcast_to([128, 16, 16]),
                         tv[:, h, 1, :].unsqueeze(1).broadcast_to([128, 16, 16]), ALU.add, reads=["t16"], writes=["cand"])
                    P.op("dve", lambda: nc.vector.max(out=c16[:, h, 0:8], in_=cand[:, 0, :]), reads=["cand"], writes=["c16"])
                    P.op("dve", lambda: nc.vector.match_replace(out=cand2[:], in_to_replace=c16[:, h, 0:8], in_values=cand[:, 0, :], imm_value=-1e30),
                         reads=["cand", "c16"], writes=["cand2"])
                    P.op("dve", lambda: nc.vector.max(out=c16[:, h, 8:16], in_=cand2[:]), reads=["cand2"], writes=["c16"])
                P.copy("dve", thr[t][:], c16[:, :, 15], reads=["c16"], writes=["thr_%d" % t])
                P.tt(e16[:], c16[:], c16[:, :, 0:1].broadcast_to([128, 8, 16]), ALU.subtract, reads=["c16"], writes=["e16"])
                P.act(e16[:], e16[:], AF.Exp, reads=["e16"], writes=["e16"])
                P.op("dve", lambda: nc.vector.tensor_reduce(out=zs[:], in_=e16[:], axis=AX.X, op=ALU.add), reads=["e16"], writes=["zs"])
                P.act(zs[:], zs[:], AF.Ln, reads=["zs"], writes=["zs"])
                P.tt(zs[:], zs[:], c16[:, :, 0], ALU.add, reads=["zs", "c16"], writes=["zs"])
                P.ts(nb[t][:], zs[:], -1.0, None, ALU.mult, None, reads=["zs"], writes=["nb_%d" % t])
                P.stt(ethr[t][:], thr[t][:], -2e-5, nb[t][:], ALU.add, ALU.add, reads=["thr_%d" % t, "nb_%d" % t], writes=["ethr_%d" % t])
                P.act(ethr[t][:], ethr[t][:], AF.Exp, reads=["ethr_%d" % t], writes=["ethr_%d" % t])
                sv = sbf[:].rearrange("p (h two) n -> p h two n", two=2)
                if NA > 0:
                    P.tt(s1nb[t][:], sv[:, NPE:8, 0, :], nb[t][:, NPE:8].unsqueeze(2).broadcast_to([128, NA, 128]), ALU.add,
                         reads=["sbf", "nb_%d" % t], writes=["s1nb_%d" % t])
                    P.copy("pool", s2b[t][:], sv[:, NPE:8, 1, :], reads=["sbf"], writes=["s2b_%d" % t])
                for a0 in range(0, 2 * NPE, 8):
                    na = min(8, 2 * NPE - a0)
                    for a in range(na):
                        P.tr(psT[:, a * 128:(a + 1) * 128], sbf[:, a0 + a, :], idb[:], reads=["sbf", "idb"], writes=[psT.name])
                    P.copy("act", sT[t][:, a0:a0 + na, :].rearrange("p a t -> p (a t)"), psT[:, 0:na * 128], reads=[psT.name], writes=["sT_%d" % t])
            units = []

            def S_A(U):
                P.tt(Ab[U["k"]][:], Gs[U["k"]][:], gh[U["g"]][:], ALU.mult, reads=["Gs%d" % U["k"], "gh%d" % U["g"]], writes=["Ab%d" % U["k"]])

            def S_T(U):
                for es in range(4):
                    P.tr(psT[:, es * 128:(es + 1) * 128], Ab[U["k"]][:, es * 128:(es + 1) * 128], idb[:], reads=["Ab%d" % U["k"], "idb"], writes=[psT.name])

            def S_C(U):
                P.copy("dve", AT[U["k"]][:].rearrange("p a t -> p (a t)"), psT[:, 0:512], reads=[psT.name], writes=["AT%d" % U["k"]])

            def S_O(U):
                for hf in range(2):
                    for es in range(4):
                        P.mm(psO[hf][:, :], AT[U["k"]][:, es, :], Vc[U["vi"]][:, es, hf * 512:(hf + 1) * 512], es == 0, es == 3,
                             reads=["AT%d" % U["k"], "Vc%d" % U["vi"]], writes=[psO[hf].name])

            def S_R(U):
                pt_ = U["t"]
                for hf in range(2):
                    if U["ch"] == 0:
                        P.copy("dve", acc[pt_][:, hf * 512:(hf + 1) * 512], psO[hf][:, :], reads=[psO[hf].name], writes=["acc_%d" % pt_])
                    else:
                        P.tt(acc[pt_][:, hf * 512:(hf + 1) * 512], acc[pt_][:, hf * 512:(hf + 1) * 512], psO[hf][:, :], ALU.add,
                             reads=[psO[hf].name, "acc_%d" % pt_], writes=["acc_%d" % pt_])

            def older(v, d, fn):
                if 0 <= v - d < len(units):
                    fn(units[v - d])

            def heads(U):
                k, t, ch = U["k"], U["t"], U["ch"]
                v = U["idx"]
                order = []
                pe_h = list(range(NPE)); ac_h = list(range(NPE, 8))
                while pe_h or ac_h:
                    if pe_h:
                        order.append(pe_h.pop(0))
                    for _ in range(3 if NPE <= 2 else 1):
                        if ac_h:
                            order.append(ac_h.pop(0))
                for hx, h in enumerate(order):
                    ei = zc[0] % 3; zc[0] += 1
                    dst = Gs[k] if hx == 0 else wb[max(h, 1)]
                    dkey = ("Gs%d" % k) if hx == 0 else ("wbm%d" % max(h, 1))
                    if h < NPE:
                        bz = psZ[h % 4]
                        P.mm(bz[:, :], sT[t][:, 2 * h, :], sel1(ch), True, False, reads=["sT_%d" % t, "idb"], writes=[bz.name])
                        P.mm(bz[:, :], sT[t][:, 2 * h + 1, :], sel2, False, True, reads=["sT_%d" % t, "idb"], writes=[bz.name])
                        P.act(ef[ei][:], bz[:, :], AF.Exp, reads=[bz.name, "nb_%d" % t], writes=["ef%d" % ei], bias=nb[t][:, h:h + 1], scale=1.0)
                        P.stt(dst[:], bz[:, :], thr[t][:, h:h + 1], ef[ei][:], ALU.is_ge, ALU.mult,
                              reads=[bz.name, "ef%d" % ei, "thr_%d" % t], writes=[dkey])
                    else:
                        for ii in range(4):
                            P.act(ef[ei][:, ii * 128:(ii + 1) * 128], s2b[t][:, h - NPE, :], AF.Exp, reads=["s2b_%d" % t, "s1nb_%d" % t],
                                  writes=["ef%d" % ei], bias=s1nb[t][:, h - NPE, 4 * ch + ii:4 * ch + ii + 1], scale=1.0)
                        P.stt(dst[:], ef[ei][:], ethr[t][:, h:h + 1], ef[ei][:], ALU.is_ge, ALU.mult,
                              reads=["ef%d" % ei, "ethr_%d" % t], writes=[dkey])
                    if hx > 0:
                        P.tt(Gs[k][:], Gs[k][:], dst[:], ALU.add, reads=["Gs%d" % k, dkey], writes=["Gs%d" % k], e="pool")
                    if hx == 0:
                        older(v, 2, S_C)
                    elif hx == 2:
                        older(v, 1, S_A)
                    elif hx == 4:
                        older(v, 3, S_R)

            assert NPE == 0
            psHb = [banks[0], banks[1], banks[6], banks[2]]
            wts = {}

            def emit_H(ch_, t_):
                uw_, uk_ = wts[ch_]
                for c in range(8):
                    P.mm(psHb[t_][:, :], h2T[t_][:, c, :], uw_[:, c, :], c == 0, c == 7, reads=["h2T_%d" % t_, uk_], writes=[psHb[t_].name])

            def emit_gelus(ch_):
                for t_ in range(4):
                    g_ = (ch_ % 2) * 4 + t_
                    P.act(gh[g_][:], psHb[t_][:, :], AF.Gelu, reads=[psHb[t_].name], writes=["gh%d" % g_])

            wts[0] = loadW(uT_d[:, 0:512])
            for t in range(4):
                emit_H(0, t)
            emit_gelus(0)
            for ch in range(32):
                vi = vn[0] % 2; vn[0] += 1
                P.dma("pool", Vc[vi][:], v_d[ch * 512:(ch + 1) * 512, :].rearrange("(a p) n -> p a n", p=128), writes=["Vc%d" % vi])
                if ch + 1 < 32:
                    wts[ch + 1] = loadW(uT_d[:, (ch + 1) * 512:(ch + 2) * 512])
                for t in range(4):
                    U = {"k": len(units) % 2, "t": t, "vi": vi, "ch": ch, "idx": len(units), "g": (ch % 2) * 4 + t}
                    units.append(U)
                    v = U["idx"]
                    if ch + 1 < 32:
                        emit_H(ch + 1, t)
                    heads(U)
                    if t == 3 and ch + 1 < 32:
                        emit_gelus(ch + 1)
                    older(v, 1, S_T)
                    older(v, 2, S_O)
            n = len(units)
            for v in range(n, n + 3):
                older(v, 2, S_C)
                older(v, 1, S_A)
                older(v, 3, S_R)
                older(v, 1, S_T)
                older(v, 2, S_O)
            older(n + 3, 3, S_R) if False else None
            for t in range(4):
                r0 = t0 + t * 128
                P.tt(ot[:], acc[t][:], G2, ALU.mult, reads=["acc_%d" % t, "mmod"], writes=["junk"], e="pool")
                P.tt(ot[:], ot[:], x1[t][:], ALU.add, reads=["junk", "x1_%d" % t], writes=["junk"], e="pool")
                P.dma("sp", over["otile"](r0) if "otile" in over else o_d[r0:r0 + 128, :], ot[:], reads=["junk"], writes=["oout"])
        for jx in range(len(P.dsem) if standalone else 0):
            if P.dcnt[jx] > 0:
                P._wait("sp", ("D%d" % jx, P.dsem[jx], 16 * P.dcnt[jx]))
        print("ffn ninst", P.ninst, "nwait", P.nwait)
    return nc


def prep_ffn(inp, layer, x_rows, y_rows, c_b, wout):
    return {
        "x": None if x_rows is None else np.ascontiguousarray(x_rows), "y": None if y_rows is None else np.ascontiguousarray(y_rows), "c": np.ascontiguousarray(c_b.reshape(8, 128).T),
        "adaw": np.ascontiguousarray(inp["ada_w"][layer][:, 2048:6144]), "adab": np.ascontiguousarray(inp["ada_b"][layer][None, 2048:6144]),
        "ng": np.ascontiguousarray(inp["norm_ffn_g"][layer][None, :]), "wout": np.ascontiguousarray(wout),
        "wq": np.ascontiguousarray(inp["peer_wq"][layer]),
        "keysT": np.ascontiguousarray(np.concatenate([inp["peer_keys"][layer][0].T, inp["peer_keys"][layer][1].T], axis=1)),
        "uT": np.ascontiguousarray(inp["peer_u"][layer].T), "v": np.ascontiguousarray(inp["peer_v"][layer]),
        "ident": np.eye(128, dtype=np.float32),
    }


def build_mix0(S, nc=None, P=None, pfx="", over=None):
    over = over or {}
    standalone = nc is None
    NT = S // 128
    if standalone:
        nc = bass.Bass("TRN2", target_bir_lowering=False)
    dt = lambda n, s: over[n] if n in over else nc.dram_tensor(pfx + n, s, F32, kind="ExternalInput").ap()
    x_d = dt("x", [S, D]); c_d = dt("c", [128, 8]); adaw_d = dt("adaw", [D, 2048]); adab_d = dt("adab", [1, 2048])
    ng_d = dt("ng", [1, D]); win_d = dt("win", [D, 1796]); convw_d = dt("convw", [128, 16]); gb_d = dt("gb", [1, 4])
    mg_d = dt("mg", [1, 256]); gqk_d = dt("gqk", [1, 512]); ident_d = dt("ident", [128, 128]); negm_d = dt("negm", [128, 128])
    posm_d = dt("posm", [128, 128]); trile_d = dt("trile", [128, 128]); sgt_d = dt("sgt", [128, 128])
    qab_d = dt("qab", [4, 4 * 128]); qad_d = dt("qad", [4, 4 * 128]); ka_d = dt("ka", [4, S]); kblk_d = dt("kblk", [32, S])
    fut_d = dt("fut", [33, 32]); own_d = dt("own", [33, 32])
    y_d = None if "ytile" in over else nc.dram_tensor("y", [S, 512], F32, kind="ExternalOutput").ap()
    with ExitStack() as st:
        if P is None:
            P = Prog(nc, st)
        P.st = st
        idf, idb = load_consts(P, ident_d)
        banks = [P.ps("bk%d" % i, [128, 512], F32) for i in range(7)]
        psT = P.ps("psT", [128, 1024], BF16)
        mod = compute_mod(P, c_d, adaw_d, adab_d, 2048, banks[0], "m")
        GM = P.sb("GM", [128, D], F32)
        P.dma("sp", GM[:], ng_d[0:1, :].partition_broadcast(128), writes=["GM"])
        P.stt(GM[:], mod[:, 1024:2048], 1.0, GM[:], ALU.add, ALU.mult, reads=["mmod", "GM"], writes=["GM"])
        SH = mod[:, 0:1024]
        gk = ["GM", "mmod"]
        cf = lambda name, src: (lambda t: (P.dma("sp", t[:], src, writes=[name]), t)[1])(P.sb(name, [128, 128], F32))
        negb = P.sb("negb", [128, 128], BF16)
        P.dma("pool", negb[:], negm_d[:, :], writes=["negb"])
        posm = cf("posm_s", posm_d[:, :]); trile = cf("trile_s", trile_d[:, :]); sgt = cf("sgt_s", sgt_d[:, :])
        onesf = P.sb("onesf", [128, 128], F32)
        P.memset("pool", onesf[:], 1.0, writes=["onesf"])
        convw = P.sb("convw_s", [128, 4, 4], F32)
        P.dma("sp", convw[:].rearrange("p a j -> p (a j)"), convw_d[:, :], writes=["convw"])
        GB = P.sb("GB", [128, 4], F32)
        P.dma("sp", GB[:], gb_d[0:1, :].partition_broadcast(128), writes=["GB"])
        MG = P.sb("MG", [128, 256], F32)
        P.dma("sp", MG[:], mg_d[0:1, :].partition_broadcast(128), writes=["MG"])
        Gqk = P.sb("Gqk", [128, 512], F32)
        P.dma("sp", Gqk[:], gqk_d[0:1, :].partition_broadcast(128), writes=["Gqk"])
        P.ts(Gqk[:, 0:256], Gqk[:, 0:256], 0.125, None, ALU.mult, None, reads=["Gqk"], writes=["Gqk"])
        wsb = P.sb("wsb", [128, 8, 1796], BF16)
        for c in range(8):
            P.dma("pool", wsb[:, c, :], win_d[c * 128:(c + 1) * 128, :], writes=["wsb"])
        KT = P.sb("KT", [100, 4, S], BF16)
        QT = P.sb("QT", [100, 4, 128], BF16)
        QTf = P.sb("QTf", [64, 4, 128], F32)
        qab = P.sb("qab_s", [100, 4, 128], F32)
        qad = P.sb("qad_s", [100, 4, 128], F32)
        P.dma("sp", qab[96:100, :, :], qab_d.rearrange("r (h t) -> r h t", h=4), writes=["qab"])
        P.dma("sp", qad[96:100, :, :], qad_d.rearrange("r (h t) -> r h t", h=4), writes=["qad"])
        for a in range(4):
            P.dma("pool", KT[64:96, a, :], kblk_d[:, :], writes=["KTaug"])
            P.dma("pool", KT[96:100, a, :], ka_d[:, :], writes=["KTaug"])
        Vm = P.sb("Vm", [128, NT, 4, 65], BF16)
        P.memset("pool", Vm[:, :, :, 64:65], 1.0, writes=["Vones"])
        kmT = P.sb("kmT", [64, 4, 32], F32)
        P.memset("pool", kmT[:], 0.0, writes=["kmT"])
        Bw = P.sb("Bw", [128, 4, 96], F32)
        P.memset("pool", Bw[:], 0.0, writes=["Bw"])
        FUT = P.sb("FUT", [128, 32], F32); OWN = P.sb("OWN", [128, 32], F32)
        xts = [P.sb("xt%d" % i, [128, D], F32) for i in range(2)]
        junk = P.sb("junk", [128, D], F32)
        hb = P.sb("hb", [128, D], BF16)
        hT = P.sb("hT", [128, 8, 128], BF16)
        small = P.sb("small", [128, 40], F32)
        cbuf = P.sb("cbuf", [128, 4, 131], F32)
        P.memset("pool", cbuf[:], 0.0, writes=["cbuf"])
        cacc = P.sb("cacc", [128, 4, 128], F32)
        ctmp = P.sb("ctmp", [128, 4, 128], F32)
        qkT = P.sb("qkT", [128, 4, 128], BF16)
        gi = P.sb("gi", [128, 8], F32)
        Lm = P.sb("Lm", [128, 128], F32)
        DT = P.sb("DT", [128, 128], F32)
        SmT = P.sb("SmT", [128, 128], BF16)
        vaug = P.sb("vaug", [128, 2, 129], BF16)
        P.memset("pool", vaug[:, :, 128:129], 1.0, writes=["vaug1"])
        kk = P.sb("kk", [128, 128], BF16)
        intra = P.sb("intra", [128, 129], F32)
        num = P.sb("num", [128, 129], F32)
        hm = P.sb("hm", [128, 128], F32)
        sig = P.sb("sig", [128, 128], F32)
        Cf = [P.sb("Cf%d" % m, [128, 129], F32) for m in range(2)]
        Cb = [P.sb("Cb%d" % m, [128, 129], BF16) for m in range(2)]
        for m in range(2):
            P.memset("pool", Cf[m][:], 0.0, writes=["Cf%d" % m])
            P.memset("pool", Cb[m][:], 0.0, writes=["Cb%d" % m])
        sq = P.sb("sq", [128, 512], F32)
        qkn = P.sb("qkn", [128, 512], F32)
        gm = P.sb("gm", [128, 32], F32)
        top8 = P.sb("top8", [128, 8], F32)
        PT = [P.sb("PT%d" % b, [128, 512], BF16) for b in range(2)]
        ymix = [P.sb("ymix%d" % b, [128, 512], F32) for b in range(2)]
        bkA, bkB, bkC, bkD, bkE, bkF, bkG = banks
        for i in range(NT):
            blk = i // 2
            xt = xts[i % 2]; xk = "xt%d" % (i % 2)
            ym = ymix[i % 2]; yk = "ymix%d" % (i % 2)
            P.dma("sp", xt[:], x_d[i * 128:(i + 1) * 128, :], writes=[xk])
            if i % 2 == 0:
                P.dma("sp", FUT[:], fut_d[blk:blk + 1, :].partition_broadcast(128), writes=["FUT"])
                P.dma("sp", OWN[:], own_d[blk:blk + 1, :].partition_broadcast(128), writes=["OWN"])
            norm_mod_T(P, xt[:], xk, GM, SH, gk, hb, hT, psT, idb, junk, small, "", act_copy=True)
            for a in range(4):
                for c in range(8):
                    P.mm(bkD[:, a * 128:(a + 1) * 128], wsb[:, c, a * 128:(a + 1) * 128], hT[:, c, :], c == 0, c == 7,
                         reads=["wsb", "hT"], writes=[bkD.name])
            for bk, c0, n in ((bkA, 512, 512), (bkB, 1024, 512), (bkC, 1536, 260)):
                for c in range(8):
                    P.mm(bk[:, 0:n], hT[:, c, :], wsb[:, c, c0:c0 + n], c == 0, c == 7, reads=["wsb", "hT"], writes=[bk.name])
            P.copy("pool", cbuf[:, :, 0:3], cbuf[:, :, 128:131], reads=["cbuf"], writes=["cbuf"])
            P.copy("act", cbuf[:, :, 3:131], bkD[:, :].rearrange("p (a t) -> p a t", a=4), reads=[bkD.name], writes=["cbuf"])
            for j in range(4):
                dst = cacc if j == 0 else ctmp
                P.tt(dst[:], cbuf[:, :, j:j + 128], convw[:, :, j:j + 1].broadcast_to([128, 4, 128]), ALU.mult,
                     reads=["cbuf", "convw"], writes=["cacc" if j == 0 else "ctmp"])
                if j > 0:
                    P.tt(cacc[:], cacc[:], ctmp[:], ALU.add, reads=["cacc", "ctmp"], writes=["cacc"])
            P.act(ctmp[:], cacc[:], AF.Silu, reads=["cacc"], writes=["ctmp"])
            P.copy("dve", qkT[:, 0:2, :], ctmp[:, 0:2, :], reads=["ctmp"], writes=["qkT"])
            P.ts(qkT[:, 2:4, :], ctmp[:, 2:4, :], 128.0 ** -0.5, None, ALU.mult, None, reads=["ctmp"], writes=["qkT"])
            P.tt(gi[:, 0:4], bkC[:, 256:260], GB[:], ALU.add, reads=[bkC.name, "GB"], writes=["gi"])
            P.act(gi[:, 4:6], gi[:, 2:4], AF.Exp, reads=["gi"], writes=["gi"], scale=-1.0)
            P.ts(gi[:, 4:6], gi[:, 4:6], 1.0, None, ALU.add, None, reads=["gi"], writes=["gi"])
            P.act(gi[:, 6:8], gi[:, 4:6], AF.Ln, reads=["gi"], writes=["gi"])
            nfl = gi[:, 6:8]
            P.mm(bkD[:, 0:2], trile[:], nfl, True, True, reads=["trile_s", "gi", "cbuf"], writes=[bkD.name])
            P.mm(bkD[:, 2:4], onesf[:], nfl, True, True, reads=["onesf", "gi"], writes=[bkD.name])
            P.act(small[:, 4:8], bkD[:, 0:4], AF.Exp, reads=[bkD.name], writes=["wd"], scale=-1.0)
            P.copy("act", vaug[:, :, 0:128], bkA[:, 0:256].rearrange("p (m d) -> p m d", m=2), reads=[bkA.name], writes=["vaug"])
            for m in range(2):
                P.ts(Lm[:], sgt[:], nfl[:, m:m + 1], None, ALU.mult, None, reads=["sgt_s", "gi"], writes=["Lm"])
                P.mm(bkE[:, 0:128], Lm[:], trile[:], True, False, reads=["Lm", "trile_s"], writes=[bkE.name])
                P.mm(bkE[:, 0:128], idf[:], posm[:], False, True, reads=["idf", "posm_s"], writes=[bkE.name])
                P.act(DT[:], bkE[:, 0:128], AF.Exp, reads=[bkE.name, "gi"], writes=["DT"], bias=gi[:, m:m + 1], scale=-1.0)
                P.mm(bkE[:, 128:256], qkT[:, 2 + m, :], qkT[:, m, :], True, True, reads=["qkT"], writes=[bkE.name])
                P.tt(SmT[:], bkE[:, 128:256], DT[:], ALU.mult, reads=[bkE.name, "DT"], writes=["SmT"])
                P.tr(psT[:, 0:128], qkT[:, 2 + m, :], idb[:], reads=["qkT", "idb"], writes=[psT.name])
                P.ts(kk[:], psT[:, 0:128], DT[:, 127:128], None, ALU.mult, None, reads=[psT.name, "DT"], writes=["kk"])
                P.mm(bkF[:, 0:129], qkT[:, m, :], Cb[m][:], True, True, reads=["qkT", "Cb%d" % m], writes=[bkF.name])
                P.mm(bkF[:, 136:265], SmT[:], vaug[:, m, :], True, True, reads=["SmT", "vaug", "vaug1"], writes=[bkF.name])
                P.mm(bkF[:, 272:401], kk[:], vaug[:, m, :], True, True, reads=["kk", "vaug", "vaug1"], writes=[bkF.name])
                P.copy("act", intra[:], bkF[:, 136:265], reads=[bkF.name], writes=["intra"])
                P.stt(num[:], bkF[:, 0:129], small[:, 4 + m:5 + m], intra[:], ALU.mult, ALU.add, reads=[bkF.name, "wd", "intra"], writes=["num"])
                P.stt(Cf[m][:], Cf[m][:], small[:, 6 + m:7 + m], bkF[:, 272:401], ALU.mult, ALU.add,
                      reads=["Cf%d" % m, "wd", bkF.name], writes=["Cf%d" % m])
                P.copy("pool", Cb[m][:], Cf[m][:], reads=["Cf%d" % m], writes=["Cb%d" % m])
                P.ts(small[:, 8:9], num[:, 128:129], 1.0, None, ALU.max, None, reads=["num"], writes=["den"])
                P.ts(small[:, 11:12], num[:, 128:129], -1.0, 1.0, ALU.mult, ALU.max, reads=["num"], writes=["den2"])
                P.tt(small[:, 8:9], small[:, 8:9], small[:, 11:12], ALU.max, reads=["den", "den2"], writes=["den"])
                P.op("dve", lambda: nc.vector.reciprocal(out=small[:, 8:9], in_=small[:, 8:9]), reads=["den"], writes=["den"])
                P.ts(hm[:], num[:, 0:128], small[:, 8:9], None, ALU.mult, None, reads=["num", "den"], writes=["hm"])
                P.act(sig[:], hm[:], AF.Square, reads=["hm"], writes=["sig", "ssm"], accum_out=small[:, 9:10])
                rstd_from_ss(P, small[:, 9:10], small[:, 10:11], 128, "ssm", "rstdm")
                P.act(sig[:], bkA[:, 256 + m * 128:256 + (m + 1) * 128], AF.Sigmoid, reads=[bkA.name], writes=["sig"])
                P.stt(hm[:], hm[:], small[:, 10:11], MG[:, m * 128:(m + 1) * 128], ALU.mult, ALU.mult, reads=["hm", "rstdm", "MG"], writes=["hm"])
                P.tt(ym[:, m * 128:(m + 1) * 128], hm[:], sig[:], ALU.mult, reads=["hm", "sig"], writes=[yk])
            P.act(sq[:], bkB[:, :], AF.Square, reads=[bkB.name], writes=["sq"])
            P.op("dve", lambda: nc.vector.tensor_reduce(out=small[:, 16:24], in_=sq[:].rearrange("p (g d) -> p g d", g=8), axis=AX.X, op=ALU.add),
                 reads=["sq"], writes=["ss8"])
            rstd_from_ss(P, small[:, 16:24], small[:, 24:32], 64, "ss8", "rstd8")
            P.tt(sq[:].rearrange("p (g d) -> p g d", g=8), bkB[:, :].rearrange("p (g d) -> p g d", g=8),
                 small[:, 24:32].unsqueeze(2).broadcast_to([128, 8, 64]), ALU.mult, reads=[bkB.name, "rstd8"], writes=["sq"])
            P.tt(qkn[:], sq[:], Gqk[:], ALU.mult, reads=["sq", "Gqk"], writes=["qkn"])
            P.copy("act", Vm[:, i, :, 0:64], bkC[:, 0:256].rearrange("p (a d) -> p a d", a=4), reads=[bkC.name], writes=["V%d" % i])
            for g in range(4):
                P.tr(bkB[0:64, g * 128:(g + 1) * 128], qkn[:, g * 64:(g + 1) * 64], idf[:], reads=["qkn", "idf"], writes=[bkB.name])
                P.tr(bkC[0:64, g * 128:(g + 1) * 128], qkn[:, 256 + g * 64:256 + (g + 1) * 64], idf[:], reads=["qkn", "idf"], writes=[bkC.name])
            P.copy("dve", QT[0:64, :, :], bkB[0:64, :].rearrange("p (a t) -> p a t", a=4), reads=[bkB.name], writes=["QT"])
            P.copy("dve", QTf[:], bkB[0:64, :].rearrange("p (a t) -> p a t", a=4), reads=[bkB.name], writes=["QTf"])
            P.copy("dve", KT[0:64, :, i * 128:(i + 1) * 128], bkC[0:64, :].rearrange("p (a t) -> p a t", a=4), reads=[bkC.name], writes=["K%d" % i])
            P.stt(QT[96:100, :, :], qad[96:100, :, :], float(i), qab[96:100, :, :], ALU.mult, ALU.add, reads=["qab", "qad"], writes=["QT"])
            for a in range(4):
                P.mm(bkG[:, a * 32:(a + 1) * 32], QTf[:, a, :], kmT[:, a, :], True, True, reads=["QTf", "kmT"], writes=[bkG.name])
            for a in range(4):
                P.tt(gm[:], bkG[:, a * 32:(a + 1) * 32], FUT[:], ALU.add, reads=[bkG.name, "FUT"], writes=["gm"])
                P.op("dve", lambda: nc.vector.max(out=top8[:], in_=gm[:]), reads=["gm"], writes=["top8"])
                P.ts(gm[:], gm[:], top8[:, 2:3], 30000.0, ALU.is_ge, ALU.mult, reads=["gm", "top8"], writes=["gm"])
                P.stt(Bw[:, a, 64:96], gm[:], -30000.0, OWN[:], ALU.add, ALU.max, reads=["gm", "OWN"], writes=["Bw"])
            for a in range(4):
                P.tr(bkB[0:96, a * 128:(a + 1) * 128], Bw[:, a, :], idf[:], reads=["Bw", "idf", "QT", "QTf"], writes=[bkB.name])
            P.copy("dve", QT[64:96, :, :], bkB[64:96, :].rearrange("p (a t) -> p a t", a=4), reads=[bkB.name], writes=["QT"])
            for a in range(4):
                P.mm(bkG[0:64, 128 + a:129 + a], qkn[:, 256 + a * 64:256 + (a + 1) * 64], onesf[:, 0:1], True, True,
                     reads=["qkn", "onesf"], writes=[bkG.name])
            if blk < 32:
                P.stt(kmT[:, :, blk], bkG[0:64, 128:132], 1.0 / 256.0, kmT[:, :, blk], ALU.mult, ALU.add, reads=[bkG.name, "kmT"], writes=["kmT"])
            ngrp = i // 4 + 1
            items = [(a, g) for a in range(4) for g in range(ngrp)]
            psOb = (bkA, bkD)

            def emit_S(a, g):
                kts = list(range(4 * g, min(4 * g + 4, i + 1)))
                ib = (a * ngrp + g) % 2
                bank = (bkE, bkF)[ib]
                for jj, kt in enumerate(kts):
                    P.mm(bank[:, jj * 128:(jj + 1) * 128], KT[0:100, a, kt * 128:(kt + 1) * 128], QT[0:100, a, :],
                         True, kt != i, reads=["K%d" % kt, "KTaug", "QT"], writes=[bank.name])
                    if kt == i:
                        P.mm(bank[:, jj * 128:(jj + 1) * 128], idb[:], negb[:], False, True, reads=["idb", "negb"], writes=[bank.name])
                P.act(PT[ib][:, 0:len(kts) * 128], bank[:, 0:len(kts) * 128], AF.Exp, reads=[bank.name], writes=["PT%d" % ib])

            def emit_PV(a, g):
                kts = list(range(4 * g, min(4 * g + 4, i + 1)))
                ib = (a * ngrp + g) % 2
                po = psOb[a % 2]
                for jj, kt in enumerate(kts):
                    P.mm(po[:, 0:65], PT[ib][:, jj * 128:(jj + 1) * 128], Vm[:, kt, a, :], kt == 0, kt == i,
                         reads=["PT%d" % ib, "V%d" % kt, "Vones"], writes=[po.name])
                if g == ngrp - 1:
                    P.op("dve", lambda: nc.vector.reciprocal(out=small[:, 12 + a:13 + a], in_=po[:, 64:65]), reads=[po.name], writes=["rdb%d" % a])
                    P.ts(ym[:, 256 + a * 64:256 + (a + 1) * 64], po[:, 0:64], small[:, 12 + a:13 + a], None, ALU.mult, None,
                         reads=[po.name, "rdb%d" % a], writes=[yk])

            emit_S(*items[0])
            for ix in range(len(items)):
                if ix + 1 < len(items):
                    emit_S(*items[ix + 1])
                emit_PV(*items[ix])
            P.dma("sp", over["ytile"](i) if "ytile" in over else y_d[i * 128:(i + 1) * 128, :], ym[:], reads=[yk], writes=["yout"])
        for jx in range(len(P.dsem) if standalone else 0):
            if P.dcnt[jx] > 0:
                P._wait("sp", ("D%d" % jx, P.dsem[jx], 16 * P.dcnt[jx]))
        print("mix0 ninst", P.ninst, "nwait", P.nwait)
    return nc


def prep_mix0(inp, layer, x_b, c_b, hh, S):
    e = layer // 2
    w = inp["ev_w_in"][e]
    mh = [2 * hh, 2 * hh + 1]; bh = [4 * hh + a for a in range(4)]
    cols = []
    for base in (0, 512):
        for m in mh:
            cols.append(w[:, base + m * 128:base + (m + 1) * 128])
    for base in (1024, 1536):
        for m in mh:
            cols.append(w[:, base + m * 128:base + (m + 1) * 128])
    for base in (2056, 2568):
        for a in bh:
            cols.append(w[:, base + a * 64:base + (a + 1) * 64])
    for a in bh:
        cols.append(w[:, 3080 + a * 64:3080 + (a + 1) * 64])
    cols.append(w[:, 2048 + mh[0]:2048 + mh[0] + 2])
    cols.append(w[:, 2052 + mh[0]:2052 + mh[0] + 2])
    win = np.ascontiguousarray(np.concatenate(cols, axis=1))
    cw = inp["ev_conv_w"][e]
    convw = np.zeros((128, 4, 4), np.float32)
    for ai, (base, m) in enumerate([(0, mh[0]), (0, mh[1]), (512, mh[0]), (512, mh[1])]):
        convw[:, ai, :] = cw[:, base + m * 128:base + (m + 1) * 128].T
    gb = np.concatenate([inp["ev_igate_b"][e][mh[0]:mh[0] + 2], inp["ev_fgate_b"][e][mh[0]:mh[0] + 2]])[None, :]
    mg = inp["ev_mnorm_g"][e][mh[0]:mh[0] + 2].reshape(1, 256)
    qn = inp["ev_qn_g"][e]; kn = inp["ev_kn_g"][e]
    gqk = np.concatenate([qn] * 4 + [kn] * 4)[None, :]
    ident, negm, ka = mix1_consts(S)
    sl = slopes8()[4 * hh:4 * hh + 4]
    qab, qad = alibi_q_rows(sl)
    li = np.arange(128)
    trile = (li[:, None] <= li[None, :]).astype(np.float32)
    sgt = (li[:, None] > li[None, :]).astype(np.float32)
    t = np.arange(S)
    kblk = (np.arange(32)[:, None] == (t // 256)[None, :]).astype(np.float32)
    nb = np.arange(32)
    fut = np.stack([np.where(nb >= b, NEG, 0.0) for b in range(33)]).astype(np.float32)
    own = np.stack([np.where(nb == b, 0.0, NEG) for b in range(33)]).astype(np.float32)
    f = np.ascontiguousarray
    return {
        "x": f(x_b), "c": f(c_b.reshape(8, 128).T), "adaw": f(inp["ada_w"][layer][:, 0:2048]), "adab": f(inp["ada_b"][layer][None, 0:2048]),
        "ng": f(inp["norm_mix_g"][layer][None, :]), "win": win, "convw": f(convw.reshape(128, 16)), "gb": f(gb.astype(np.float32)),
        "mg": f(mg), "gqk": f(gqk.astype(np.float32)), "ident": ident, "negm": negm, "posm": f(-negm), "trile": trile, "sgt": sgt,
        "qab": qab, "qad": qad, "ka": ka, "kblk": kblk, "fut": fut, "own": own,
    }


PAIRS = [[0, 1], [2, 3], [4, 5], [6, 7]]


def build_fused(S):
    NTOK = S // 2
    NCH = max(1, (S * 512 * 4) // (2 << 20))
    RY = S // NCH
    RX = NTOK // NCH
    nc = bass.Bass("TRN2", target_bir_lowering=False)
    dr = lambda n, shp: nc.dram_tensor(n, shp, F32).ap()
    y0c = [dr("y0c%d" % j, [RY, 512]) for j in range(NCH)]; yg0c = [dr("yg0c%d" % j, [2 * RY, 512]) for j in range(NCH)]
    y1c = [dr("y1c%d" % j, [RY, 512]) for j in range(NCH)]; yg1c = [dr("yg1c%d" % j, [2 * RY, 512]) for j in range(NCH)]
    x1c = [dr("x1c%d" % j, [RX, D]) for j in range(NCH)]; x1gc = [dr("x1gc%d" % j, [2 * RX, D]) for j in range(NCH)]

    def ytile(ch):
        return lambda i: ch[(i * 128) // RY][(i * 128) % RY:(i * 128) % RY + 128, :]

    def ygtile(ch):
        def f(r, q, r0):
            t = q * NTOK + r0
            return ch[t // RY][r * RY + t % RY:r * RY + t % RY + 128, :]
        return f

    def x1tile(r0):
        return x1c[r0 // RX][r0 % RX:r0 % RX + 128, :]

    def x1gtile(i):
        t = i * 128
        rank, loc = t // NTOK, t % NTOK
        return x1gc[loc // RX][rank * RX + loc % RX:rank * RX + loc % RX + 128, :]

    with ExitStack() as sems:
        Ps = [Prog(nc, None, 16, p, sems) for p in ("a_", "b_", "c_", "d_")]
        ccs = [sems.enter_context(nc.semaphore("cc%d" % i)) for i in range(3)]

        def exchange(Pprev, Pnext, srcs, dsts, cs):
            toks = Pprev.all_tokens()
            Pnext.wait_all(toks, engines=["pool"])
            for a_, d_ in zip(srcs, dsts):
                nc.gpsimd.collective_compute("AllGather", ALU.bypass, replica_groups=PAIRS, ins=[a_.opt()], outs=[d_.opt()]).then_inc(cs)
            Pnext.wait_all(toks + [("CC", cs, len(srcs))], engines=["pe", "act", "dve", "sp", "pool"])

        build_mix0(S, nc, Ps[0], "a_", over={"ytile": ytile(y0c)})
        exchange(Ps[0], Ps[1], y0c, yg0c, ccs[0])
        build_ffn(NTOK, nc, Ps[1], "b_", over={"yg": ygtile(yg0c), "otile": x1tile})
        exchange(Ps[1], Ps[2], x1c, x1gc, ccs[1])
        build_mix1(S, nc, Ps[2], "c_", over={"xtile": x1gtile, "ytile": ytile(y1c)})
        exchange(Ps[2], Ps[3], y1c, yg1c, ccs[2])
        build_ffn(NTOK, nc, Ps[3], "d_", over={"xtile": x1tile, "yg": ygtile(yg1c)})
        Ps[3].wait_all(Ps[3].all_tokens(), engines=["sp"])
        print("fused ninst", sum(p.ninst for p in Ps), "nwait", sum(p.nwait for p in Ps))
    return nc


_CACHE = {}


def _get(name, fn, *a):
    key = (name,) + a
    if key not in _CACHE:
        _CACHE[key] = fn(*a)
    return _CACHE[key]


def kernel(**inputs):
    inp = {k: np.asarray(v) for k, v in inputs.items()}
    x = inp["x"].astype(np.float32, copy=False)
    c = inp["c"]
    B, S, _ = x.shape
    NTOK = S // 2
    nc = _get("fused", build_fused, S)
    wo0 = inp["ev_w_out"][0]
    wo0p = np.concatenate([wo0[0:256], wo0[512:768], wo0[256:512], wo0[768:1024]], axis=0)
    maps = []
    for k in range(8):
        b, hh = k // 2, k % 2
        hsel = np.zeros((128, 2), np.float32); hsel[:, hh] = 1.0
        m = {}
        for pfx, d in (("a_", prep_mix0(inp, 0, x[b], c[b], hh, S)),
                       ("b_", prep_ffn(inp, 0, x[b, hh * NTOK:(hh + 1) * NTOK], None, c[b], wo0p)),
                       ("c_", prep_mix1(inp, 1, None, c[b], hh, S)),
                       ("d_", prep_ffn(inp, 1, None, None, c[b], inp["od_w_out"][0]))):
            for kk, v in d.items():
                if v is not None:
                    m[pfx + kk] = v
        m["b_hsel"] = hsel; m["d_hsel"] = hsel
        maps.append(m)
    res = run_bass_kernel_spmd(nc, maps, core_ids=list(range(8)))
    out = np.concatenate([res.results[k]["o"] for k in range(8)], axis=0).reshape(B, S, D)
    return out.astype(np.float32)
```

```python
import numpy as np
from contextlib import ExitStack
import concourse.bass as bass
import concourse.mybir as mybir
from concourse.bass_utils import run_bass_kernel_spmd

F32 = mybir.dt.float32
BF16 = mybir.dt.bfloat16
AF = mybir.ActivationFunctionType
ALU = mybir.AluOpType
AX = mybir.AxisListType

D = 1024
EPS = 1e-6
NEG = -30000.0
import os
STAGE = int(os.environ.get('KSTAGE', '9'))
SUB = int(os.environ.get('KSUB', '0'))
KTENG = os.environ.get('KTENG', 'dve')
NPE = int(os.environ.get('KNPE', '0'))


class Prog:
    def __init__(self, nc, stack, n_dma_sems=24, pfx="", sem_stack=None):
        self.nc = nc
        self.st = stack
        self.pfx = pfx
        sem_stack = sem_stack if sem_stack is not None else stack
        self.eng = {"pe": nc.tensor, "act": nc.scalar, "dve": nc.vector, "pool": nc.gpsimd, "sp": nc.sync}
        self.sem = {k: sem_stack.enter_context(nc.semaphore(pfx + "s_" + k)) for k in self.eng}
        self.cnt = {k: 0 for k in self.eng}
        self.dsem = [sem_stack.enter_context(nc.semaphore(pfx + "d%d" % i)) for i in range(n_dma_sems)]
        self.dcnt = [0] * n_dma_sems
        self.dnext = 0
        self.waited = {k: {} for k in self.eng}
        self.lastw = {}
        self.readers = {}
        self.ninst = 0
        self.nwait = 0

    def sb(self, name, shape, dt):
        return self.st.enter_context(self.nc.sbuf_tensor(self.pfx + name, shape, dt))

    def ps(self, name, shape, dt):
        return self.st.enter_context(self.nc.psum_tensor(self.pfx + name, shape, dt))

    def all_tokens(self):
        toks = [("E" + e, self.sem[e], self.cnt[e]) for e in self.eng if self.cnt[e] > 0]
        toks += [("D%d" % j, self.dsem[j], 16 * self.dcnt[j]) for j in range(len(self.dsem)) if self.dcnt[j] > 0]
        return toks

    def wait_all(self, toks, engines=None):
        for e in (engines or self.eng):
            for sid, sm, v in toks:
                self.eng[e].wait_ge(sm, v)
                self.nwait += 1

    def _wait(self, e, tok):
        if tok is None:
            return
        sid, s, v = tok
        if self.waited[e].get(sid, 0) >= v:
            return
        self.eng[e].wait_ge(s, v)
        self.nwait += 1
        self.waited[e][sid] = v

    def _deps(self, e, reads, writes):
        for k in reads:
            self._wait(e, self.lastw.get(k))
        for k in writes:
            self._wait(e, self.lastw.get(k))
            for t in self.readers.get(k, ()):
                self._wait(e, t)

    def _commit(self, tok, reads, writes):
        for k in writes:
            self.lastw[k] = tok
            self.readers[k] = []
        for k in reads:
            lst = self.readers.setdefault(k, [])
            lst.append(tok)
            if len(lst) > 16:
                best = {}
                for t in lst:
                    if t[0] not in best or best[t[0]][2] < t[2]:
                        best[t[0]] = t
                self.readers[k] = list(best.values())

    def op(self, e, ins_fn, reads=(), writes=()):
        self._deps(e, reads, writes)
        ins = ins_fn()
        self.cnt[e] += 1
        ins.then_inc(self.sem[e], 1)
        tok = ("E" + e, self.sem[e], self.cnt[e])
        self.waited[e]["E" + e] = self.cnt[e] - 1
        self._commit(tok, reads, writes)
        self.ninst += 1
        return tok

    def dma(self, q, out, in_, reads=(), writes=(), **kw):
        j = self.dnext
        self.dnext = (self.dnext + 1) % len(self.dsem)
        if self.dcnt[j] > 0:
            self._wait(q, ("D%d" % j, self.dsem[j], 16 * self.dcnt[j]))
        self._deps(q, reads, writes)
        ins = self.eng[q].dma_start(out=out, in_=in_, **kw)
        self.dcnt[j] += 1
        ins.then_inc(self.dsem[j], 16)
        tok = ("D%d" % j, self.dsem[j], 16 * self.dcnt[j])
        self.waited[q]["D%d" % j] = 16 * (self.dcnt[j] - 1)
        self._commit(tok, reads, writes)
        self.ninst += 1
        return tok

    def finish(self, keys, e="sp"):
        for k in keys:
            self._wait(e, self.lastw.get(k))

    def mm(self, out, lhsT, rhs, start, stop, reads, writes):
        nc = self.nc
        return self.op("pe", lambda: nc.tensor.matmul(out, lhsT=lhsT, rhs=rhs, start=start, stop=stop),
                       reads=reads, writes=writes)

    def tr(self, out, in_, ident, reads, writes):
        nc = self.nc
        return self.op("pe", lambda: nc.tensor.transpose(out=out, in_=in_, identity=ident), reads=reads, writes=writes)

    def act(self, out, in_, func, reads, writes, **kw):
        nc = self.nc
        return self.op("act", lambda: nc.scalar.activation(out=out, in_=in_, func=func, **kw), reads=reads, writes=writes)

    def tt(self, out, in0, in1, op, reads, writes, e="dve"):
        eng = self.eng[e]
        return self.op(e, lambda: eng.tensor_tensor(out=out, in0=in0, in1=in1, op=op), reads=reads, writes=writes)

    def ts(self, out, in0, s1, s2, op0, op1, reads, writes, e="dve", **kw):
        eng = self.eng[e]
        if op1 is None:
            return self.op(e, lambda: eng.tensor_scalar(out=out, in0=in0, scalar1=s1, scalar2=None, op0=op0, **kw),
                           reads=reads, writes=writes)
        return self.op(e, lambda: eng.tensor_scalar(out=out, in0=in0, scalar1=s1, scalar2=s2, op0=op0, op1=op1, **kw),
                       reads=reads, writes=writes)

    def stt(self, out, in0, scalar, in1, op0, op1, reads, writes):
        nc = self.nc
        return self.op("dve", lambda: nc.vector.scalar_tensor_tensor(out=out, in0=in0, scalar=scalar, in1=in1, op0=op0, op1=op1),
                       reads=reads, writes=writes)

    def copy(self, e, out, in_, reads, writes):
        nc = self.nc
        if e == "act":
            return self.op("act", lambda: nc.scalar.copy(out=out, in_=in_), reads=reads, writes=writes)
        eng = self.eng[e]
        return self.op(e, lambda: eng.tensor_copy(out=out, in_=in_), reads=reads, writes=writes)

    def memset(self, e, ap, val, writes):
        eng = self.eng[e]
        return self.op(e, lambda: eng.memset(ap, val), writes=writes)


def load_consts(P, ident_d):
    idf = P.sb("idf", [128, 128], F32)
    idb = P.sb("idb", [128, 128], BF16)
    P.dma("sp", idf[:], ident_d[:, :], writes=["idf"])
    P.dma("pool", idb[:], ident_d[:, :], writes=["idb"])
    return idf, idb


def compute_mod(P, c_d, adaw_d, adab_d, ncols, ps_bank, name):
    nc = P.nc
    CW = 256
    ccol = P.sb(name + "_ccol", [128, 8], F32)
    cact = P.sb(name + "_cact", [128, 8], F32)
    crep = P.sb(name + "_crep", [128, 8, 128], F32)
    mod = P.sb(name + "_mod", [128, ncols], F32)
    brep = P.sb(name + "_brep", [128, CW], F32)
    wch = P.sb(name + "_wch", [128, 8, CW], F32)
    P.dma("sp", ccol[:], c_d[:, :], writes=[name + "ccol"])
    P.act(cact[:], ccol[:], AF.Silu, reads=[name + "ccol"], writes=[name + "cact"])
    P.copy("dve", crep[:], cact[:].unsqueeze(2).broadcast_to([128, 8, 128]), reads=[name + "cact"], writes=[name + "crep"])
    for j in range(ncols // CW):
        P.dma("sp", wch[:], adaw_d[:, j * CW:(j + 1) * CW].rearrange("(c p) n -> p c n", p=128), writes=[name + "wch"])
        P.dma("sp", brep[:], adab_d[0:1, j * CW:(j + 1) * CW].partition_broadcast(128), writes=[name + "brep"])
        for c in range(8):
            P.mm(ps_bank[:, 0:CW], crep[:, c, :], wch[:, c, :], c == 0, c == 7,
                 reads=[name + "crep", name + "wch"], writes=[ps_bank.name])
        P.tt(mod[:, j * CW:(j + 1) * CW], ps_bank[:, 0:CW], brep[:], ALU.add,
             reads=[ps_bank.name, name + "brep"], writes=[name + "mod"])
    return mod


def rstd_from_ss(P, ss, rstd, n, key_in, key_out, width=1):
    nc = P.nc
    P.ts(rstd, ss, 1.0 / n, EPS, ALU.mult, ALU.add, reads=[key_in], writes=[key_out])
    P.act(rstd, rstd, AF.Sqrt, reads=[key_out], writes=[key_out])
    P.op("dve", lambda: nc.vector.reciprocal(out=rstd, in_=rstd), reads=[key_out], writes=[key_out])


def norm_mod_T(P, xt, xkey, GM, SH, gkeys, hb, hT, psT, idb, junk, small, tag, act_copy=True):
    nc = P.nc
    ss = small[:, 0:1]
    rstd = small[:, 1:2]
    P.act(junk[:], xt, AF.Square, reads=[xkey], writes=["junk" + tag, "ss" + tag], accum_out=ss)
    rstd_from_ss(P, ss, rstd, D, "ss" + tag, "rstd" + tag)
    P.stt(junk[:], xt, rstd, GM[:], ALU.mult, ALU.mult, reads=[xkey, "rstd" + tag] + gkeys, writes=["junk" + tag])
    P.tt(hb[:], junk[:], SH[:], ALU.add, reads=["junk" + tag] + gkeys, writes=["hb" + tag])
    for c in range(8):
        P.tr(psT[:, c * 128:(c + 1) * 128], hb[:, c * 128:(c + 1) * 128], idb[:], reads=["hb" + tag, "idb"], writes=[psT.name])
    P.copy("act" if act_copy else "dve", hT[:].rearrange("p c t -> p (c t)"), psT[:, :], reads=[psT.name], writes=["hT" + tag])


def build_mix1(S, nc=None, P=None, pfx="", over=None):
    over = over or {}
    standalone = nc is None
    NT = S // 128
    if standalone:
        nc = bass.Bass("TRN2", target_bir_lowering=False)
    dt = lambda n, s: over[n] if n in over else nc.dram_tensor(pfx + n, s, F32, kind="ExternalInput").ap()
    x_d = None if "xtile" in over else dt("x", [S, D])
    c_d = dt("c", [128, 8]); adaw_d = dt("adaw", [D, 2048]); adab_d = dt("adab", [1, 2048])
    ng_d = dt("ng", [1, D]); win_d = dt("win", [D, 4 * 384]); gqk_d = dt("gqk", [1, 256]); lam_d = dt("lam", [1, 256])
    ong_d = dt("ong", [1, 128]); ident_d = dt("ident", [128, 128]); negm_d = dt("negm", [128, 128])
    qab_d = dt("qab", [4, 4 * 128]); qad_d = dt("qad", [4, 4 * 128]); ka_d = dt("ka", [4, S])
    laminit_d = dt("laminit", [1, 2])
    y_d = None if "ytile" in over else nc.dram_tensor("y", [S, 512], F32, kind="ExternalOutput").ap()
    with ExitStack() as st:
        if P is None:
            P = Prog(nc, st)
        P.st = st
        idf, idb = load_consts(P, ident_d)
        banks = [P.ps("bk%d" % i, [128, 512], F32) for i in range(7)]
        psT = P.ps("psT", [128, 1024], BF16)
        mod = compute_mod(P, c_d, adaw_d, adab_d, 2048, banks[0], "m")
        GM = P.sb("GM", [128, D], F32)
        P.dma("sp", GM[:], ng_d[0:1, :].partition_broadcast(128), writes=["GM"])
        P.stt(GM[:], mod[:, 1024:2048], 1.0, GM[:], ALU.add, ALU.mult, reads=["mmod", "GM"], writes=["GM"])
        SH = mod[:, 0:1024]
        gk = ["GM", "mmod"]
        negb = P.sb("negb", [128, 128], BF16)
        P.dma("pool", negb[:], negm_d[:, :], writes=["negb"])
        Gqk = P.sb("Gqk", [128, 256], F32)
        P.dma("sp", Gqk[:], gqk_d[0:1, :].partition_broadcast(128), writes=["Gqk"])
        P.ts(Gqk[:, 0:128], Gqk[:, 0:128], 0.125, None, ALU.mult, None, reads=["Gqk"], writes=["Gqk"])
        ONG = P.sb("ONG", [128, 128], F32)
        P.dma("sp", ONG[:], ong_d[0:1, :].partition_broadcast(128), writes=["ONG"])
        lamt = P.sb("lamt_s", [128, 256], F32)
        lami = P.sb("lami", [128, 2], F32)
        lsm = P.sb("lsm", [128, 4], F32)
        P.dma("sp", lamt[:], lam_d[0:1, :].partition_broadcast(128), writes=["lamt"])
        P.dma("sp", lami[:], laminit_d[0:1, :].partition_broadcast(128), writes=["lami"])
        lv = lamt[:].rearrange("p (a d) -> p a d", a=4)
        P.tt(lamt[:, 0:64], lv[:, 0, :], lv[:, 1, :], ALU.mult, reads=["lamt"], writes=["lamt"])
        P.tt(lamt[:, 128:192], lv[:, 2, :], lv[:, 3, :], ALU.mult, reads=["lamt"], writes=["lamt"])
        P.op("dve", lambda: nc.vector.tensor_reduce(out=lsm[:, 0:1], in_=lamt[:, 0:64], axis=AX.X, op=ALU.add), reads=["lamt"], writes=["lsm"])
        P.op("dve", lambda: nc.vector.tensor_reduce(out=lsm[:, 1:2], in_=lamt[:, 128:192], axis=AX.X, op=ALU.add), reads=["lamt"], writes=["lsm"])
        P.act(lsm[:, 0:2], lsm[:, 0:2], AF.Exp, reads=["lsm"], writes=["lsm"])
        P.tt(lsm[:, 2:3], lsm[:, 1:2], lsm[:, 0:1], ALU.subtract, reads=["lsm"], writes=["lsm"])
        P.tt(lsm[:, 3:4], lsm[:, 2:3], lami[:, 0:1], ALU.subtract, reads=["lsm", "lami"], writes=["neglam"])
        neglam = lsm[:, 3:4]
        P.ts(ONG[:], ONG[:], lami[:, 1:2], None, ALU.mult, None, reads=["ONG", "lami"], writes=["ONG"])

        KT = P.sb("KT", [68, 2, S], BF16)
        QT = P.sb("QT", [68, 2, 128], BF16)
        qab = P.sb("qab_s", [68, 4, 128], F32)
        qad = P.sb("qad_s", [68, 4, 128], F32)
        P.dma("sp", qab[64:68, :, :], qab_d.rearrange("r (h t) -> r h t", h=4), writes=["qab"])
        P.dma("sp", qad[64:68, :, :], qad_d.rearrange("r (h t) -> r h t", h=4), writes=["qad"])
        for c in range(2):
            P.dma("pool", KT[64:68, c, :], ka_d[:, :], writes=["KTaug"])
        Vaug = P.sb("Vaug", [128, NT, 129], BF16)
        P.memset("pool", Vaug[:, :, 128:129], 1.0, writes=["Vones"])
        wsb = P.sb("wsb", [128, 8, 384], BF16)
        xts = [P.sb("xt%d" % i, [128, D], F32) for i in range(2)]
        junk = P.sb("junk", [128, D], F32)
        hb = P.sb("hb", [128, D], BF16)
        hT = P.sb("hT", [128, 8, 128], BF16)
        small = P.sb("small", [128, 16], F32)
        sq = P.sb("sq", [128, 256], F32)
        qkn = P.sb("qkn", [128, 256], BF16)
        PT = [[P.sb("PT%d%d" % (c, b), [128, 512], BF16) for b in range(2)] for c in range(2)]
        osb = P.sb("osb", [128, 128], F32)
        o2 = P.sb("o2", [128, 128], F32)
        yo = [P.sb("yo%d" % i, [128, 128], F32) for i in range(2)]
        psZ = banks[0]
        psS = [[banks[1], banks[2]], [banks[3], banks[4]]]
        psO = [banks[5], banks[6]]
        it = 0
        for j in range(4 if STAGE >= 1 else 0):
            P.dma("pool", wsb[:], win_d[:, j * 384:(j + 1) * 384].rearrange("(c p) n -> p c n", p=128),
                  writes=["wsb"])
            for i in range(NT):
                xt = xts[it % 2]; xk = "xt%d" % (it % 2)
                P.dma("sp", xt[:], over["xtile"](i) if "xtile" in over else x_d[i * 128:(i + 1) * 128, :], writes=[xk])
                norm_mod_T(P, xt[:], xk, GM, SH, gk, hb, hT, psT, idb, junk, small, "", act_copy=True)
                if STAGE < 2:
                    continue
                for c in range(8):
                    P.mm(psZ[:, 0:384], hT[:, c, :], wsb[:, c, :], c == 0, c == 7, reads=["hT", "wsb"], writes=[psZ.name])
                if STAGE < 3:
                    continue
                if SUB == 5:
                    continue
                P.act(sq[:], psZ[:, 0:256], AF.Square, reads=[psZ.name], writes=["sq"])
                P.op("dve", lambda: nc.vector.tensor_reduce(out=small[:, 4:8], in_=sq[:].rearrange("p (g d) -> p g d", g=4), axis=AX.X, op=ALU.add),
                     reads=["sq"], writes=["ss4"])
                rstd_from_ss(P, small[:, 4:8], small[:, 8:12], 64, "ss4", "rstd4")
                P.tt(sq[:].rearrange("p (g d) -> p g d", g=4), psZ[:, 0:256].rearrange("p (g d) -> p g d", g=4),
                     small[:, 8:12].unsqueeze(2).broadcast_to([128, 4, 64]), ALU.mult, reads=[psZ.name, "rstd4"], writes=["sq"])
                P.tt(qkn[:], sq[:], Gqk[:], ALU.mult, reads=["sq", "Gqk"], writes=["qkn"])
                if SUB == 2:
                    continue
                P.copy("act", Vaug[:, i, 0:128], psZ[:, 256:384], reads=[psZ.name], writes=["V%d" % i])
                if SUB == 3:
                    continue
                for g in range(4):
                    P.tr(psT[0:64, g * 128:(g + 1) * 128], qkn[:, g * 64:(g + 1) * 64], idb[:], reads=["qkn", "idb"], writes=[psT.name])
                if SUB == 4:
                    continue
                P.copy("dve", QT[0:64, :, :], psT[0:64, 0:256].rearrange("p (c t) -> p c t", c=2), reads=[psT.name], writes=["QT"])
                if SUB == 6:
                    continue
                for c in range(2):
                    P.copy(KTENG, KT[0:64, c, i * 128:(i + 1) * 128], psT[0:64, 256 + c * 128:256 + (c + 1) * 128],
                           reads=[psT.name], writes=["K%d" % i])
                if SUB == 7:
                    continue
                if SUB != 1:
                  P.stt(QT[64:68, :, :], qad[64:68, j:j + 1, :].broadcast_to([4, 2, 128]), float(i),
                        qab[64:68, j:j + 1, :].broadcast_to([4, 2, 128]), ALU.mult, ALU.add, reads=["qab", "qad"], writes=["QT"])
                if STAGE < 4:
                    continue
                ngrp = i // 4 + 1
                items = [(g, c) for g in range(ngrp) for c in range(2)]

                def emit_S(g, c):
                    kts = list(range(4 * g, min(4 * g + 4, i + 1)))
                    bank = psS[c][g % 2]
                    for jj, kt in enumerate(kts):
                        P.mm(bank[:, jj * 128:(jj + 1) * 128], KT[0:68, c, kt * 128:(kt + 1) * 128], QT[0:68, c, :],
                             True, kt != i, reads=["K%d" % kt, "KTaug", "QT"], writes=[bank.name])
                        if kt == i:
                            P.mm(bank[:, jj * 128:(jj + 1) * 128], idb[:], negb[:], False, True,
                                 reads=["idb", "negb"], writes=[bank.name])
                    pt = PT[c][g % 2]; pk = "PT%d%d" % (c, g % 2)
                    P.act(pt[:, 0:len(kts) * 128], bank[:, 0:len(kts) * 128], AF.Exp, reads=[bank.name], writes=[pk])

                def emit_PV(g, c):
                    kts = list(range(4 * g, min(4 * g + 4, i + 1)))
                    pt = PT[c][g % 2]; pk = "PT%d%d" % (c, g % 2)
                    for jj, kt in enumerate(kts):
                        P.mm(psO[c][:, 0:129], pt[:, jj * 128:(jj + 1) * 128], Vaug[:, kt, :], kt == 0, kt == i,
                             reads=[pk, "V%d" % kt, "Vones"], writes=[psO[c].name])

                emit_S(*items[0])
                for ix in range(len(items)):
                    if ix + 1 < len(items):
                        emit_S(*items[ix + 1])
                    emit_PV(*items[ix])
                if STAGE < 5:
                    continue
                P.op("dve", lambda: nc.vector.reciprocal(out=small[:, 12:13], in_=psO[0][:, 128:129]), reads=[psO[0].name], writes=["rd0"])
                P.op("dve", lambda: nc.vector.reciprocal(out=small[:, 13:14], in_=psO[1][:, 128:129]), reads=[psO[1].name], writes=["rd1"])
                P.ts(o2[:], psO[1][:, 0:128], small[:, 13:14], neglam, ALU.mult, ALU.mult, reads=[psO[1].name, "rd1", "neglam"], writes=["o2"])
                P.stt(osb[:], psO[0][:, 0:128], small[:, 12:13], o2[:], ALU.mult, ALU.add, reads=[psO[0].name, "rd0", "o2"], writes=["osb"])
                P.act(o2[:], osb[:], AF.Square, reads=["osb"], writes=["o2", "sso"], accum_out=small[:, 14:15])
                rstd_from_ss(P, small[:, 14:15], small[:, 15:16], 128, "sso", "rstdo")
                yt = yo[it % 2]; yk = "yo%d" % (it % 2)
                P.stt(yt[:], osb[:], small[:, 15:16], ONG[:], ALU.mult, ALU.mult, reads=["osb", "rstdo", "ONG"], writes=[yk])
                ydst = over["ytile"](i)[:, j * 128:(j + 1) * 128] if "ytile" in over else y_d[i * 128:(i + 1) * 128, j * 128:(j + 1) * 128]
                P.dma("sp", ydst, yt[:], reads=[yk], writes=["yout"])
                it += 1
        P.finish(["yout"])
        for k, v in P.lastw.items():
            pass
        for jx in range(len(P.dsem) if standalone else 0):
            if P.dcnt[jx] > 0:
                P._wait("sp", ("D%d" % jx, P.dsem[jx], 16 * P.dcnt[jx]))
        print("mix1 ninst", P.ninst, "nwait", P.nwait)
    return nc


def mix1_consts(S):
    ident = np.eye(128, dtype=np.float32)
    k_idx = np.arange(128)[:, None]; q_idx = np.arange(128)[None, :]
    negm = np.where(k_idx > q_idx, NEG, 0.0).astype(np.float32)
    ka = np.zeros((4, S), np.float32)
    t = np.arange(S)
    ka[0] = 1.0; ka[1] = t % 128; ka[2] = t // 128; ka[3] = 1.0
    return ident, negm, ka


def alibi_q_rows(slopes):
    nh = len(slopes)
    qab = np.zeros((4, nh, 128), np.float32); qad = np.zeros((4, nh, 128), np.float32)
    for h, s in enumerate(slopes):
        qab[0, h] = -s * np.arange(128); qab[1, h] = s; qab[2, h] = 128.0 * s
        qad[3, h] = -128.0 * s
    return qab.reshape(4, nh * 128), qad.reshape(4, nh * 128)


def slopes8():
    return [2.0 ** (-8.0 * (h + 1) / 8) for h in range(8)]


def prep_mix1(inp, layer, x_b, c_b, hh, S):
    o = layer // 2
    w = inp["od_w_in"][o]
    cols = []
    for j in range(4):
        h = 4 * hh + j
        cols.append(w[:, h * 128:(h + 1) * 128])
        cols.append(w[:, 1024 + h * 128:1024 + (h + 1) * 128])
        cols.append(w[:, 2048 + h * 128:2048 + (h + 1) * 128])
    win = np.ascontiguousarray(np.concatenate(cols, axis=1))
    ident, negm, ka = mix1_consts(S)
    sl = slopes8()[4 * hh:4 * hh + 4]
    qab, qad = alibi_q_rows(sl)
    qn = inp["od_qn_g"][o]; kn = inp["od_kn_g"][o]
    gqk = np.concatenate([qn, qn, kn, kn])[None, :].astype(np.float32)
    import math
    lam_init = 0.8 - 0.6 * math.exp(-0.3 * layer)
    return {
        "x": None if x_b is None else np.ascontiguousarray(x_b), "c": np.ascontiguousarray(c_b.reshape(8, 128).T),
        "adaw": np.ascontiguousarray(inp["ada_w"][layer][:, 0:2048]), "adab": np.ascontiguousarray(inp["ada_b"][layer][None, 0:2048]),
        "ng": np.ascontiguousarray(inp["norm_mix_g"][layer][None, :]), "win": win, "gqk": gqk,
        "lam": np.ascontiguousarray(inp["od_lam"][o].reshape(1, 256)), "ong": np.ascontiguousarray(inp["od_onorm_g"][o][None, :]),
        "ident": ident, "negm": negm, "qab": qab, "qad": qad, "ka": ka,
        "laminit": np.array([[lam_init, 1.0 - lam_init]], np.float32),
    }


def build_ffn(NTOK, nc=None, P=None, pfx="", over=None):
    over = over or {}
    standalone = nc is None
    NST = NTOK // 512
    if standalone:
        nc = bass.Bass("TRN2", target_bir_lowering=False)
    dt = lambda n, s: over[n] if n in over else nc.dram_tensor(pfx + n, s, F32, kind="ExternalInput").ap()
    yg = over.get("yg"); SEQ = 2 * NTOK
    x_d = None if "xtile" in over else dt("x", [NTOK, D])
    c_d = dt("c", [128, 8])
    y_d = dt("y", [NTOK, D]) if yg is None else None
    hsel_d = dt("hsel", [128, 2]) if yg is not None else None
    adaw_d = dt("adaw", [D, 4096]); adab_d = dt("adab", [1, 4096]); ng_d = dt("ng", [1, D])
    wout_d = dt("wout", [D, D]); wq_d = dt("wq", [D, 2048]); keysT_d = dt("keysT", [128, 256])
    uT_d = dt("uT", [D, 16384]); v_d = dt("v", [16384, D]); ident_d = dt("ident", [128, 128])
    o_d = None if "otile" in over else nc.dram_tensor("o", [NTOK, D], F32, kind="ExternalOutput").ap()
    with ExitStack() as st:
        if P is None:
            P = Prog(nc, st)
        P.st = st
        idf, idb = load_consts(P, ident_d)
        banks = [P.ps("bk%d" % i, [128, 512], F32) for i in range(7)]
        psT = P.ps("psT", [128, 1024], BF16)
        mod = compute_mod(P, c_d, adaw_d, adab_d, 4096, banks[0], "m")
        GM = P.sb("GM", [128, D], F32)
        P.dma("sp", GM[:], ng_d[0:1, :].partition_broadcast(128), writes=["GM"])
        P.stt(GM[:], mod[:, 2048:3072], 1.0, GM[:], ALU.add, ALU.mult, reads=["mmod", "GM"], writes=["GM"])
        G1 = mod[:, 0:1024]; SH = mod[:, 1024:2048]; G2 = mod[:, 3072:4096]
        gk = ["GM", "mmod"]
        keysb = P.sb("keysb", [128, 256], BF16)
        P.dma("pool", keysb[:], keysT_d[:, :], writes=["keysb"])
        W = [P.sb("W%d" % i, [128, 8, 512], BF16) for i in range(2)]
        Vc = [P.sb("Vc%d" % i, [128, 4, 1024], BF16) for i in range(2)]
        wn = [0]; vn = [0]

        def loadW(src):
            i = wn[0] % 2; wn[0] += 1
            P.dma("pool", W[i][:], src.rearrange("(c p) n -> p c n", p=128), writes=["W%d" % i])
            return W[i], "W%d" % i

        xt = P.sb("xt", [128, D], F32)
        if yg is not None:
            ysf = P.sb("ysf", [128, 2, 512], F32)
            hsel = P.sb("hsel_s", [128, 2], F32)
            P.dma("sp", hsel[:], hsel_d[:, :], writes=["hsel"])
        yb = P.sb("yb", [128, D], BF16)
        yT = P.sb("yT", [128, 8, 128], BF16)
        junk = P.sb("junk", [128, D], F32)
        hb = P.sb("hb", [128, D], BF16)
        small = P.sb("small", [128, 8], F32)
        x1 = [P.sb("x1_%d" % t, [128, D], F32) for t in range(4)]
        h2T = [P.sb("h2T_%d" % t, [128, 8, 128], BF16) for t in range(4)]
        qTb = [P.sb("qTb_%d" % t, [128, 16, 128], BF16) for t in range(4)]
        NA = 8 - NPE
        s1nb = [P.sb("s1nb_%d" % t, [128, max(NA, 1), 128], F32) for t in range(4)]
        s2b = [P.sb("s2b_%d" % t, [128, max(NA, 1), 128], BF16) for t in range(4)]
        sT = [P.sb("sT_%d" % t, [128, max(2 * NPE, 1), 128], BF16) for t in range(4)]
        thr = [P.sb("thr_%d" % t, [128, 8], F32) for t in range(4)]
        nb = [P.sb("nb_%d" % t, [128, 8], F32) for t in range(4)]
        acc = [P.sb("acc_%d" % t, [128, D], F32) for t in range(4)]
        sbf = P.sb("sbf", [128, 16, 128], BF16)
        t16 = P.sb("t16", [128, 16, 16], F32)
        tmp128 = P.sb("tmp128", [128, 128], BF16)
        cand = P.sb("cand", [128, 1, 256], F32)
        cand2 = P.sb("cand2", [128, 256], F32)
        c16 = P.sb("c16", [128, 8, 16], F32)
        e16 = P.sb("e16", [128, 8, 16], F32)
        zs = P.sb("zs", [128, 8], F32)
        ef = [P.sb("ef%d" % i, [128, 512], F32) for i in range(5)]
        wb = [None] + [P.sb("wbm%d" % i, [128, 512], BF16) for i in range(1, 8)]
        Gs = [P.sb("Gs%d" % i, [128, 512], F32) for i in range(2)]
        ethr = [P.sb("ethr_%d" % t, [128, 8], F32) for t in range(4)]
        gh = [P.sb("gh%d" % i, [128, 512], BF16) for i in range(8)]
        Ab = [P.sb("Ab%d" % i, [128, 512], BF16) for i in range(2)]
        AT = [P.sb("AT%d" % i, [128, 4, 128], BF16) for i in range(2)]
        ot = junk
        psZ = [banks[0], banks[1], banks[6], banks[2]]; psH = banks[3]; psO = [banks[4], banks[5]]; zc = [0]
        sel1 = lambda c: idb[:, 4 * c:4 * c + 4].unsqueeze(2).broadcast_to([128, 4, 128])
        sel2 = idb[:, :].unsqueeze(1).broadcast_to([128, 4, 128])
        cnt = [0]
        for stile in range(NST):
            t0 = stile * 512
            wo = [loadW(wout_d[:, hf * 512:(hf + 1) * 512]) for hf in range(2)]
            for t in range(4):
                r0 = t0 + t * 128
                if yg is None:
                    P.dma("pool", yb[:], y_d[r0:r0 + 128, :], writes=["yb"])
                else:
                    for r in range(2):
                        for q in range(2):
                            P.dma("sp", ysf[:, q, :], yg(r, q, r0), writes=["ysf%d" % q])
                        P.ts(junk[:, r * 512:(r + 1) * 512], ysf[:, 0, :], hsel[:, 0:1], None, ALU.mult, None,
                             reads=["ysf0", "hsel"], writes=["junk"])
                        P.stt(yb[:, r * 512:(r + 1) * 512], ysf[:, 1, :], hsel[:, 1:2], junk[:, r * 512:(r + 1) * 512],
                              ALU.mult, ALU.add, reads=["ysf1", "hsel", "junk"], writes=["yb"])
                P.dma("sp", xt[:], over["xtile"](r0) if "xtile" in over else x_d[r0:r0 + 128, :], writes=["xt"])
                for c in range(8):
                    P.tr(psT[:, c * 128:(c + 1) * 128], yb[:, c * 128:(c + 1) * 128], idb[:], reads=["yb", "idb"], writes=[psT.name])
                P.copy("act", yT[:].rearrange("p c t -> p (c t)"), psT[:, :], reads=[psT.name], writes=["yT"])
                for hf in range(2):
                    bk = psO[hf]
                    for c in range(8):
                        P.mm(bk[:, :], yT[:, c, :], wo[hf][0][:, c, :], c == 0, c == 7, reads=["yT", wo[hf][1]], writes=[bk.name])
                    P.tt(junk[:, hf * 512:(hf + 1) * 512], bk[:, :], G1[:, hf * 512:(hf + 1) * 512], ALU.mult,
                         reads=[bk.name, "mmod"], writes=["junk"])
                P.tt(x1[t][:], junk[:], xt[:], ALU.add, reads=["junk", "xt"], writes=["x1_%d" % t])
                norm_mod_T(P, x1[t][:], "x1_%d" % t, GM, SH, gk, hb, h2T[t], psT, idb, junk, small, "", act_copy=True)
                P.lastw["h2T_%d" % t] = P.lastw["hT"]
            for g in range(4):
                wq, wqk = loadW(wq_d[:, g * 512:(g + 1) * 512])
                for t in range(4):
                    bk = psZ[(g * 4 + t) % 2]
                    for hp in range(4):
                        for c in range(8):
                            P.mm(bk[:, hp * 128:(hp + 1) * 128], wq[:, c, hp * 128:(hp + 1) * 128], h2T[t][:, c, :], c == 0, c == 7,
                                 reads=[wqk, "h2T_%d" % t], writes=[bk.name])
                    P.copy("act", qTb[t][:, 4 * g:4 * g + 4, :].rearrange("p a t -> p (a t)"), bk[:, :], reads=[bk.name], writes=["qTb_%d" % t])
            for t in range(4):
                for g in range(4):
                    bk = psZ[g % 2]
                    for a in range(4):
                        hp = 4 * g + a
                        P.mm(bk[:, a * 128:(a + 1) * 128], qTb[t][:, hp, :], keysb[:, (hp % 2) * 128:(hp % 2 + 1) * 128], True, True,
                             reads=["qTb_%d" % t, "keysb"], writes=[bk.name])
                    P.copy("act", sbf[:, 4 * g:4 * g + 4, :].rearrange("p a n -> p (a n)"), bk[:, :], reads=[bk.name], writes=["sbf"])
                for hp in range(16):
                    P.op("dve", lambda: nc.vector.max(out=t16[:, hp, 0:8], in_=sbf[:, hp, :]), reads=["sbf"], writes=["t16"])
                    P.op("dve", lambda: nc.vector.match_replace(out=tmp128[:], in_to_replace=t16[:, hp, 0:8], in_values=sbf[:, hp, :], imm_value=-1e30),
                         reads=["sbf", "t16"], writes=["tmp128"])
                    P.op("dve", lambda: nc.vector.max(out=t16[:, hp, 8:16], in_=tmp128[:]), reads=["tmp128"], writes=["t16"])
                tv = t16[:].rearrange("p (h two) k -> p h two k", two=2)
                for h in range(8):
                    P.tt(cand[:, 0, :].rearrange("p (a b) -> p a b", a=16), tv[:, h, 0, :].unsqueeze(2).broadcast_to([128, 16, 16]),
                         tv[:, h, 1, :].unsqueeze(1).broadcast_to([128, 16, 16]), ALU.add, reads=["t16"], writes=["cand"])
                    P.op("dve", lambda: nc.vector.max(out=c16[:, h, 0:8], in_=cand[:, 0, :]), reads=["cand"], writes=["c16"])
                    P.op("dve", lambda: nc.vector.match_replace(out=cand2[:], in_to_replace=c16[:, h, 0:8], in_values=cand[:, 0, :], imm_value=-1e30),
                         reads=["cand", "c16"], writes=["cand2"])
                    P.op("dve", lambda: nc.vector.max(out=c16[:, h, 8:16], in_=cand2[:]), reads=["cand2"], writes=["c16"])
                P.copy("dve", thr[t][:], c16[:, :, 15], reads=["c16"], writes=["thr_%d" % t])
                P.tt(e16[:], c16[:], c16[:, :, 0:1].broadcast_to([128, 8, 16]), ALU.subtract, reads=["c16"], writes=["e16"])
                P.act(e16[:], e16[:], AF.Exp, reads=["e16"], writes=["e16"])
                P.op("dve", lambda: nc.vector.tensor_reduce(out=zs[:], in_=e16[:], axis=AX.X, op=ALU.add), reads=["e16"], writes=["zs"])
                P.act(zs[:], zs[:], AF.Ln, reads=["zs"], writes=["zs"])
                P.tt(zs[:], zs[:], c16[:, :, 0], ALU.add, reads=["zs", "c16"], writes=["zs"])
                P.ts(nb[t][:], zs[:], -1.0, None, ALU.mult, None, reads=["zs"], writes=["nb_%d" % t])
                P.stt(ethr[t][:], thr[t][:], -2e-5, nb[t][:], ALU.add, ALU.add, reads=["thr_%d" % t, "nb_%d" % t], writes=["ethr_%d" % t])
                P.act(ethr[t][:], ethr[t][:], AF.Exp, reads=["ethr_%d" % t], writes=["ethr_%d" % t])
                sv = sbf[:].rearrange("p (h two) n -> p h two n", two=2)
                if NA > 0:
                    P.tt(s1nb[t][:], sv[:, NPE:8, 0, :], nb[t][:, NPE:8].unsqueeze(2).broadcast_to([128, NA, 128]), ALU.add,
                         reads=["sbf", "nb_%d" % t], writes=["s1nb_%d" % t])
                    P.copy("pool", s2b[t][:], sv[:, NPE:8, 1, :], reads=["sbf"], writes=["s2b_%d" % t])
                for a0 in range(0, 2 * NPE, 8):
                    na = min(8, 2 * NPE - a0)
                    for a in range(na):
                        P.tr(psT[:, a * 128:(a + 1) * 128], sbf[:, a0 + a, :], idb[:], reads=["sbf", "idb"], writes=[psT.name])
                    P.copy("act", sT[t][:, a0:a0 + na, :].rearrange("p a t -> p (a t)"), psT[:, 0:na * 128], reads=[psT.name], writes=["sT_%d" % t])
            units = []

            def S_A(U):
                P.tt(Ab[U["k"]][:], Gs[U["k"]][:], gh[U["g"]][:], ALU.mult, reads=["Gs%d" % U["k"], "gh%d" % U["g"]], writes=["Ab%d" % U["k"]])

            def S_T(U):
                for es in range(4):
                    P.tr(psT[:, es * 128:(es + 1) * 128], Ab[U["k"]][:, es * 128:(es + 1) * 128], idb[:], reads=["Ab%d" % U["k"], "idb"], writes=[psT.name])

            def S_C(U):
                P.copy("dve", AT[U["k"]][:].rearrange("p a t -> p (a t)"), psT[:, 0:512], reads=[psT.name], writes=["AT%d" % U["k"]])

            def S_O(U):
                for hf in range(2):
                    for es in range(4):
                        P.mm(psO[hf][:, :], AT[U["k"]][:, es, :], Vc[U["vi"]][:, es, hf * 512:(hf + 1) * 512], es == 0, es == 3,
                             reads=["AT%d" % U["k"], "Vc%d" % U["vi"]], writes=[psO[hf].name])

            def S_R(U):
                pt_ = U["t"]
                for hf in range(2):
                    if U["ch"] == 0:
                        P.copy("dve", acc[pt_][:, hf * 512:(hf + 1) * 512], psO[hf][:, :], reads=[psO[hf].name], writes=["acc_%d" % pt_])
                    else:
                        P.tt(acc[pt_][:, hf * 512:(hf + 1) * 512], acc[pt_][:, hf * 512:(hf + 1) * 512], psO[hf][:, :], ALU.add,
                             reads=[psO[hf].name, "acc_%d" % pt_], writes=["acc_%d" % pt_])

            def older(v, d, fn):
                if 0 <= v - d < len(units):
                    fn(units[v - d])

            def heads(U):
                k, t, ch = U["k"], U["t"], U["ch"]
                v = U["idx"]
                order = []
                pe_h = list(range(NPE)); ac_h = list(range(NPE, 8))
                while pe_h or ac_h:
                    if pe_h:
                        order.append(pe_h.pop(0))
                    for _ in range(3 if NPE <= 2 else 1):
                        if ac_h:
                            order.append(ac_h.pop(0))
                for hx, h in enumerate(order):
                    ei = zc[0] % 5; zc[0] += 1
                    dst = Gs[k] if hx == 0 else wb[max(h, 1)]
                    dkey = ("Gs%d" % k) if hx == 0 else ("wbm%d" % max(h, 1))
                    if h < NPE:
                        bz = psZ[h % 4]
                        P.mm(bz[:, :], sT[t][:, 2 * h, :], sel1(ch), True, False, reads=["sT_%d" % t, "idb"], writes=[bz.name])
                        P.mm(bz[:, :], sT[t][:, 2 * h + 1, :], sel2, False, True, reads=["sT_%d" % t, "idb"], writes=[bz.name])
                        P.act(ef[ei][:], bz[:, :], AF.Exp, reads=[bz.name, "nb_%d" % t], writes=["ef%d" % ei], bias=nb[t][:, h:h + 1], scale=1.0)
                        P.stt(dst[:], bz[:, :], thr[t][:, h:h + 1], ef[ei][:], ALU.is_ge, ALU.mult,
                              reads=[bz.name, "ef%d" % ei, "thr_%d" % t], writes=[dkey])
                    else:
                        for ii in range(4):
                            P.act(ef[ei][:, ii * 128:(ii + 1) * 128], s2b[t][:, h - NPE, :], AF.Exp, reads=["s2b_%d" % t, "s1nb_%d" % t],
                                  writes=["ef%d" % ei], bias=s1nb[t][:, h - NPE, 4 * ch + ii:4 * ch + ii + 1], scale=1.0)
                        P.stt(dst[:], ef[ei][:], ethr[t][:, h:h + 1], ef[ei][:], ALU.is_ge, ALU.mult,
                              reads=["ef%d" % ei, "ethr_%d" % t], writes=[dkey])
                    if hx > 0:
                        P.tt(Gs[k][:], Gs[k][:], dst[:], ALU.add, reads=["Gs%d" % k, dkey], writes=["Gs%d" % k], e="pool")
                    if hx == 0:
                        older(v, 2, S_C)
                    elif hx == 2:
                        older(v, 1, S_A)
                    elif hx == 4:
                        older(v, 3, S_R)

            assert NPE == 0
            psHb = [banks[0], banks[1], banks[6], banks[2]]
            wts = {}

            def emit_H(ch_, t_):
                uw_, uk_ = wts[ch_]
                for c in range(8):
                    P.mm(psHb[t_][:, :], h2T[t_][:, c, :], uw_[:, c, :], c == 0, c == 7, reads=["h2T_%d" % t_, uk_], writes=[psHb[t_].name])

            def emit_gelus(ch_):
                for t_ in range(4):
                    g_ = (ch_ % 2) * 4 + t_
                    P.act(gh[g_][:], psHb[t_][:, :], AF.Gelu, reads=[psHb[t_].name], writes=["gh%d" % g_])

            wts[0] = loadW(uT_d[:, 0:512])
            for t in range(4):
                emit_H(0, t)
            emit_gelus(0)
            for ch in range(32):
                vi = vn[0] % 2; vn[0] += 1
                P.dma("pool", Vc[vi][:], v_d[ch * 512:(ch + 1) * 512, :].rearrange("(a p) n -> p a n", p=128), writes=["Vc%d" % vi])
                if ch + 1 < 32:
                    wts[ch + 1] = loadW(uT_d[:, (ch + 1) * 512:(ch + 2) * 512])
                for t in range(4):
                    U = {"k": len(units) % 2, "t": t, "vi": vi, "ch": ch, "idx": len(units), "g": (ch % 2) * 4 + t}
                    units.append(U)
                    v = U["idx"]
                    if ch + 1 < 32:
                        emit_H(ch + 1, t)
                    heads(U)
                    if t == 3 and ch + 1 < 32:
                        emit_gelus(ch + 1)
                    older(v, 1, S_T)
                    older(v, 2, S_O)
            n = len(units)
            for v in range(n, n + 3):
                older(v, 2, S_C)
                older(v, 1, S_A)
                older(v, 3, S_R)
                older(v, 1, S_T)
                older(v, 2, S_O)
            older(n + 3, 3, S_R) if False else None
            for t in range(4):
                r0 = t0 + t * 128
                P.tt(ot[:], acc[t][:], G2, ALU.mult, reads=["acc_%d" % t, "mmod"], writes=["junk"], e="pool")
                P.tt(ot[:], ot[:], x1[t][:], ALU.add, reads=["junk", "x1_%d" % t], writes=["junk"], e="pool")
                P.dma("sp", over["otile"](r0) if "otile" in over else o_d[r0:r0 + 128, :], ot[:], reads=["junk"], writes=["oout"])
        for jx in range(len(P.dsem) if standalone else 0):
            if P.dcnt[jx] > 0:
                P._wait("sp", ("D%d" % jx, P.dsem[jx], 16 * P.dcnt[jx]))
        print("ffn ninst", P.ninst, "nwait", P.nwait)
    return nc


def prep_ffn(inp, layer, x_rows, y_rows, c_b, wout):
    return {
        "x": None if x_rows is None else np.ascontiguousarray(x_rows), "y": None if y_rows is None else np.ascontiguousarray(y_rows), "c": np.ascontiguousarray(c_b.reshape(8, 128).T),
        "adaw": np.ascontiguousarray(inp["ada_w"][layer][:, 2048:6144]), "adab": np.ascontiguousarray(inp["ada_b"][layer][None, 2048:6144]),
        "ng": np.ascontiguousarray(inp["norm_ffn_g"][layer][None, :]), "wout": np.ascontiguousarray(wout),
        "wq": np.ascontiguousarray(inp["peer_wq"][layer]),
        "keysT": np.ascontiguousarray(np.concatenate([inp["peer_keys"][layer][0].T, inp["peer_keys"][layer][1].T], axis=1)),
        "uT": np.ascontiguousarray(inp["peer_u"][layer].T), "v": np.ascontiguousarray(inp["peer_v"][layer]),
        "ident": np.eye(128, dtype=np.float32),
    }


def build_mix0(S, nc=None, P=None, pfx="", over=None):
    over = over or {}
    standalone = nc is None
    NT = S // 128
    if standalone:
        nc = bass.Bass("TRN2", target_bir_lowering=False)
    dt = lambda n, s: over[n] if n in over else nc.dram_tensor(pfx + n, s, F32, kind="ExternalInput").ap()
    x_d = dt("x", [S, D]); c_d = dt("c", [128, 8]); adaw_d = dt("adaw", [D, 2048]); adab_d = dt("adab", [1, 2048])
    ng_d = dt("ng", [1, D]); win_d = dt("win", [D, 1796]); convw_d = dt("convw", [128, 16]); gb_d = dt("gb", [1, 4])
    mg_d = dt("mg", [1, 256]); gqk_d = dt("gqk", [1, 512]); ident_d = dt("ident", [128, 128]); negm_d = dt("negm", [128, 128])
    posm_d = dt("posm", [128, 128]); trile_d = dt("trile", [128, 128]); sgt_d = dt("sgt", [128, 128])
    qab_d = dt("qab", [4, 4 * 128]); qad_d = dt("qad", [4, 4 * 128]); ka_d = dt("ka", [4, S]); kblk_d = dt("kblk", [32, S])
    fut_d = dt("fut", [33, 32]); own_d = dt("own", [33, 32])
    y_d = None if "ytile" in over else nc.dram_tensor("y", [S, 512], F32, kind="ExternalOutput").ap()
    with ExitStack() as st:
        if P is None:
            P = Prog(nc, st)
        P.st = st
        idf, idb = load_consts(P, ident_d)
        banks = [P.ps("bk%d" % i, [128, 512], F32) for i in range(7)]
        psT = P.ps("psT", [128, 1024], BF16)
        mod = compute_mod(P, c_d, adaw_d, adab_d, 2048, banks[0], "m")
        GM = P.sb("GM", [128, D], F32)
        P.dma("sp", GM[:], ng_d[0:1, :].partition_broadcast(128), writes=["GM"])
        P.stt(GM[:], mod[:, 1024:2048], 1.0, GM[:], ALU.add, ALU.mult, reads=["mmod", "GM"], writes=["GM"])
        SH = mod[:, 0:1024]
        gk = ["GM", "mmod"]
        cf = lambda name, src: (lambda t: (P.dma("sp", t[:], src, writes=[name]), t)[1])(P.sb(name, [128, 128], F32))
        negb = P.sb("negb", [128, 128], BF16)
        P.dma("pool", negb[:], negm_d[:, :], writes=["negb"])
        posm = cf("posm_s", posm_d[:, :]); trile = cf("trile_s", trile_d[:, :]); sgt = cf("sgt_s", sgt_d[:, :])
        onesf = P.sb("onesf", [128, 128], F32)
        P.memset("pool", onesf[:], 1.0, writes=["onesf"])
        convw = P.sb("convw_s", [128, 4, 4], F32)
        P.dma("sp", convw[:].rearrange("p a j -> p (a j)"), convw_d[:, :], writes=["convw"])
        GB = P.sb("GB", [128, 4], F32)
        P.dma("sp", GB[:], gb_d[0:1, :].partition_broadcast(128), writes=["GB"])
        MG = P.sb("MG", [128, 256], F32)
        P.dma("sp", MG[:], mg_d[0:1, :].partition_broadcast(128), writes=["MG"])
        Gqk = P.sb("Gqk", [128, 512], F32)
        P.dma("sp", Gqk[:], gqk_d[0:1, :].partition_broadcast(128), writes=["Gqk"])
        P.ts(Gqk[:, 0:256], Gqk[:, 0:256], 0.125, None, ALU.mult, None, reads=["Gqk"], writes=["Gqk"])
        wsb = P.sb("wsb", [128, 8, 1796], BF16)
        for c in range(8):
            P.dma("pool", wsb[:, c, :], win_d[c * 128:(c + 1) * 128, :], writes=["wsb"])
        KT = P.sb("KT", [100, 4, S], BF16)
        QT = P.sb("QT", [100, 4, 128], BF16)
        QTf = P.sb("QTf", [64, 4, 128], F32)
        qab = P.sb("qab_s", [100, 4, 128], F32)
        qad = P.sb("qad_s", [100, 4, 128], F32)
        P.dma("sp", qab[96:100, :, :], qab_d.rearrange("r (h t) -> r h t", h=4), writes=["qab"])
        P.dma("sp", qad[96:100, :, :], qad_d.rearrange("r (h t) -> r h t", h=4), writes=["qad"])
        for a in range(4):
            P.dma("pool", KT[64:96, a, :], kblk_d[:, :], writes=["KTaug"])
            P.dma("pool", KT[96:100, a, :], ka_d[:, :], writes=["KTaug"])
        Vm = P.sb("Vm", [128, NT, 4, 65], BF16)
        P.memset("pool", Vm[:, :, :, 64:65], 1.0, writes=["Vones"])
        kmT = P.sb("kmT", [64, 4, 32], F32)
        P.memset("pool", kmT[:], 0.0, writes=["kmT"])
        Bw = P.sb("Bw", [128, 4, 96], F32)
        P.memset("pool", Bw[:], 0.0, writes=["Bw"])
        FUT = P.sb("FUT", [128, 32], F32); OWN = P.sb("OWN", [128, 32], F32)
        xts = [P.sb("xt%d" % i, [128, D], F32) for i in range(2)]
        junk = P.sb("junk", [128, D], F32)
        hb = P.sb("hb", [128, D], BF16)
        hT = P.sb("hT", [128, 8, 128], BF16)
        small = P.sb("small", [128, 40], F32)
        cbuf = P.sb("cbuf", [128, 4, 131], F32)
        P.memset("pool", cbuf[:], 0.0, writes=["cbuf"])
        cacc = P.sb("cacc", [128, 4, 128], F32)
        ctmp = P.sb("ctmp", [128, 4, 128], F32)
        qkT = P.sb("qkT", [128, 4, 128], BF16)
        gi = P.sb("gi", [128, 8], F32)
        Lm = P.sb("Lm", [128, 128], F32)
        DT = P.sb("DT", [128, 128], F32)
        SmT = P.sb("SmT", [128, 128], BF16)
        vaug = P.sb("vaug", [128, 2, 129], BF16)
        P.memset("pool", vaug[:, :, 128:129], 1.0, writes=["vaug1"])
        kk = P.sb("kk", [128, 128], BF16)
        intra = P.sb("intra", [128, 129], F32)
        num = P.sb("num", [128, 129], F32)
        hm = P.sb("hm", [128, 128], F32)
        sig = P.sb("sig", [128, 128], F32)
        Cf = [P.sb("Cf%d" % m, [128, 129], F32) for m in range(2)]
        Cb = [P.sb("Cb%d" % m, [128, 129], BF16) for m in range(2)]
        for m in range(2):
            P.memset("pool", Cf[m][:], 0.0, writes=["Cf%d" % m])
            P.memset("pool", Cb[m][:], 0.0, writes=["Cb%d" % m])
        sq = P.sb("sq", [128, 512], F32)
        qkn = P.sb("qkn", [128, 512], F32)
        gm = P.sb("gm", [128, 32], F32)
        top8 = P.sb("top8", [128, 8], F32)
        PT = [P.sb("PT%d" % b, [128, 512], BF16) for b in range(2)]
        ymix = [P.sb("ymix%d" % b, [128, 512], F32) for b in range(2)]
        bkA, bkB, bkC, bkD, bkE, bkF, bkG = banks
        for i in range(NT):
            blk = i // 2
            xt = xts[i % 2]; xk = "xt%d" % (i % 2)
            ym = ymix[i % 2]; yk = "ymix%d" % (i % 2)
            P.dma("sp", xt[:], x_d[i * 128:(i + 1) * 128, :], writes=[xk])
            if i % 2 == 0:
                P.dma("sp", FUT[:], fut_d[blk:blk + 1, :].partition_broadcast(128), writes=["FUT"])
                P.dma("sp", OWN[:], own_d[blk:blk + 1, :].partition_broadcast(128), writes=["OWN"])
            norm_mod_T(P, xt[:], xk, GM, SH, gk, hb, hT, psT, idb, junk, small, "", act_copy=True)
            for a in range(4):
                for c in range(8):
                    P.mm(bkD[:, a * 128:(a + 1) * 128], wsb[:, c, a * 128:(a + 1) * 128], hT[:, c, :], c == 0, c == 7,
                         reads=["wsb", "hT"], writes=[bkD.name])
            for bk, c0, n in ((bkA, 512, 512), (bkB, 1024, 512), (bkC, 1536, 260)):
                for c in range(8):
                    P.mm(bk[:, 0:n], hT[:, c, :], wsb[:, c, c0:c0 + n], c == 0, c == 7, reads=["wsb", "hT"], writes=[bk.name])
            P.copy("pool", cbuf[:, :, 0:3], cbuf[:, :, 128:131], reads=["cbuf"], writes=["cbuf"])
            P.copy("act", cbuf[:, :, 3:131], bkD[:, :].rearrange("p (a t) -> p a t", a=4), reads=[bkD.name], writes=["cbuf"])
            for j in range(4):
                dst = cacc if j == 0 else ctmp
                P.tt(dst[:], cbuf[:, :, j:j + 128], convw[:, :, j:j + 1].broadcast_to([128, 4, 128]), ALU.mult,
                     reads=["cbuf", "convw"], writes=["cacc" if j == 0 else "ctmp"])
                if j > 0:
                    P.tt(cacc[:], cacc[:], ctmp[:], ALU.add, reads=["cacc", "ctmp"], writes=["cacc"])
            P.act(ctmp[:], cacc[:], AF.Silu, reads=["cacc"], writes=["ctmp"])
            P.copy("dve", qkT[:, 0:2, :], ctmp[:, 0:2, :], reads=["ctmp"], writes=["qkT"])
            P.ts(qkT[:, 2:4, :], ctmp[:, 2:4, :], 128.0 ** -0.5, None, ALU.mult, None, reads=["ctmp"], writes=["qkT"])
            P.tt(gi[:, 0:4], bkC[:, 256:260], GB[:], ALU.add, reads=[bkC.name, "GB"], writes=["gi"])
            P.act(gi[:, 4:6], gi[:, 2:4], AF.Exp, reads=["gi"], writes=["gi"], scale=-1.0)
            P.ts(gi[:, 4:6], gi[:, 4:6], 1.0, None, ALU.add, None, reads=["gi"], writes=["gi"])
            P.act(gi[:, 6:8], gi[:, 4:6], AF.Ln, reads=["gi"], writes=["gi"])
            nfl = gi[:, 6:8]
            P.mm(bkD[:, 0:2], trile[:], nfl, True, True, reads=["trile_s", "gi", "cbuf"], writes=[bkD.name])
            P.mm(bkD[:, 2:4], onesf[:], nfl, True, True, reads=["onesf", "gi"], writes=[bkD.name])
            P.act(small[:, 4:8], bkD[:, 0:4], AF.Exp, reads=[bkD.name], writes=["wd"], scale=-1.0)
            P.copy("act", vaug[:, :, 0:128], bkA[:, 0:256].rearrange("p (m d) -> p m d", m=2), reads=[bkA.name], writes=["vaug"])
            for m in range(2):
                P.ts(Lm[:], sgt[:], nfl[:, m:m + 1], None, ALU.mult, None, reads=["sgt_s", "gi"], writes=["Lm"])
                P.mm(bkE[:, 0:128], Lm[:], trile[:], True, False, reads=["Lm", "trile_s"], writes=[bkE.name])
                P.mm(bkE[:, 0:128], idf[:], posm[:], False, True, reads=["idf", "posm_s"], writes=[bkE.name])
                P.act(DT[:], bkE[:, 0:128], AF.Exp, reads=[bkE.name, "gi"], writes=["DT"], bias=gi[:, m:m + 1], scale=-1.0)
                P.mm(bkE[:, 128:256], qkT[:, 2 + m, :], qkT[:, m, :], True, True, reads=["qkT"], writes=[bkE.name])
                P.tt(SmT[:], bkE[:, 128:256], DT[:], ALU.mult, reads=[bkE.name, "DT"], writes=["SmT"])
                P.tr(psT[:, 0:128], qkT[:, 2 + m, :], idb[:], reads=["qkT", "idb"], writes=[psT.name])
                P.ts(kk[:], psT[:, 0:128], DT[:, 127:128], None, ALU.mult, None, reads=[psT.name, "DT"], writes=["kk"])
                P.mm(bkF[:, 0:129], qkT[:, m, :], Cb[m][:], True, True, reads=["qkT", "Cb%d" % m], writes=[bkF.name])
                P.mm(bkF[:, 136:265], SmT[:], vaug[:, m, :], True, True, reads=["SmT", "vaug", "vaug1"], writes=[bkF.name])
                P.mm(bkF[:, 272:401], kk[:], vaug[:, m, :], True, True, reads=["kk", "vaug", "vaug1"], writes=[bkF.name])
                P.copy("act", intra[:], bkF[:, 136:265], reads=[bkF.name], writes=["intra"])
                P.stt(num[:], bkF[:, 0:129], small[:, 4 + m:5 + m], intra[:], ALU.mult, ALU.add, reads=[bkF.name, "wd", "intra"], writes=["num"])
                P.stt(Cf[m][:], Cf[m][:], small[:, 6 + m:7 + m], bkF[:, 272:401], ALU.mult, ALU.add,
                      reads=["Cf%d" % m, "wd", bkF.name], writes=["Cf%d" % m])
                P.copy("pool", Cb[m][:], Cf[m][:], reads=["Cf%d" % m], writes=["Cb%d" % m])
                P.ts(small[:, 8:9], num[:, 128:129], 1.0, None, ALU.max, None, reads=["num"], writes=["den"])
                P.ts(small[:, 11:12], num[:, 128:129], -1.0, 1.0, ALU.mult, ALU.max, reads=["num"], writes=["den2"])
                P.tt(small[:, 8:9], small[:, 8:9], small[:, 11:12], ALU.max, reads=["den", "den2"], writes=["den"])
                P.op("dve", lambda: nc.vector.reciprocal(out=small[:, 8:9], in_=small[:, 8:9]), reads=["den"], writes=["den"])
                P.ts(hm[:], num[:, 0:128], small[:, 8:9], None, ALU.mult, None, reads=["num", "den"], writes=["hm"])
                P.act(sig[:], hm[:], AF.Square, reads=["hm"], writes=["sig", "ssm"], accum_out=small[:, 9:10])
                rstd_from_ss(P, small[:, 9:10], small[:, 10:11], 128, "ssm", "rstdm")
                P.act(sig[:], bkA[:, 256 + m * 128:256 + (m + 1) * 128], AF.Sigmoid, reads=[bkA.name], writes=["sig"])
                P.stt(hm[:], hm[:], small[:, 10:11], MG[:, m * 128:(m + 1) * 128], ALU.mult, ALU.mult, reads=["hm", "rstdm", "MG"], writes=["hm"])
                P.tt(ym[:, m * 128:(m + 1) * 128], hm[:], sig[:], ALU.mult, reads=["hm", "sig"], writes=[yk])
            P.act(sq[:], bkB[:, :], AF.Square, reads=[bkB.name], writes=["sq"])
            P.op("dve", lambda: nc.vector.tensor_reduce(out=small[:, 16:24], in_=sq[:].rearrange("p (g d) -> p g d", g=8), axis=AX.X, op=ALU.add),
                 reads=["sq"], writes=["ss8"])
            rstd_from_ss(P, small[:, 16:24], small[:, 24:32], 64, "ss8", "rstd8")
            P.tt(sq[:].rearrange("p (g d) -> p g d", g=8), bkB[:, :].rearrange("p (g d) -> p g d", g=8),
                 small[:, 24:32].unsqueeze(2).broadcast_to([128, 8, 64]), ALU.mult, reads=[bkB.name, "rstd8"], writes=["sq"])
            P.tt(qkn[:], sq[:], Gqk[:], ALU.mult, reads=["sq", "Gqk"], writes=["qkn"])
            P.copy("act", Vm[:, i, :, 0:64], bkC[:, 0:256].rearrange("p (a d) -> p a d", a=4), reads=[bkC.name], writes=["V%d" % i])
            for g in range(4):
                P.tr(bkB[0:64, g * 128:(g + 1) * 128], qkn[:, g * 64:(g + 1) * 64], idf[:], reads=["qkn", "idf"], writes=[bkB.name])
                P.tr(bkC[0:64, g * 128:(g + 1) * 128], qkn[:, 256 + g * 64:256 + (g + 1) * 64], idf[:], reads=["qkn", "idf"], writes=[bkC.name])
            P.copy("dve", QT[0:64, :, :], bkB[0:64, :].rearrange("p (a t) -> p a t", a=4), reads=[bkB.name], writes=["QT"])
            P.copy("dve", QTf[:], bkB[0:64, :].rearrange("p (a t) -> p a t", a=4), reads=[bkB.name], writes=["QTf"])
            P.copy("dve", KT[0:64, :, i * 128:(i + 1) * 128], bkC[0:64, :].rearrange("p (a t) -> p a t", a=4), reads=[bkC.name], writes=["K%d" % i])
            P.stt(QT[96:100, :, :], qad[96:100, :, :], float(i), qab[96:100, :, :], ALU.mult, ALU.add, reads=["qab", "qad"], writes=["QT"])
            for a in range(4):
                P.mm(bkG[:, a * 32:(a + 1) * 32], QTf[:, a, :], kmT[:, a, :], True, True, reads=["QTf", "kmT"], writes=[bkG.name])
            for a in range(4):
                P.tt(gm[:], bkG[:, a * 32:(a + 1) * 32], FUT[:], ALU.add, reads=[bkG.name, "FUT"], writes=["gm"])
                P.op("dve", lambda: nc.vector.max(out=top8[:], in_=gm[:]), reads=["gm"], writes=["top8"])
                P.ts(gm[:], gm[:], top8[:, 2:3], 30000.0, ALU.is_ge, ALU.mult, reads=["gm", "top8"], writes=["gm"])
                P.stt(Bw[:, a, 64:96], gm[:], -30000.0, OWN[:], ALU.add, ALU.max, reads=["gm", "OWN"], writes=["Bw"])
            for a in range(4):
                P.tr(bkB[0:96, a * 128:(a + 1) * 128], Bw[:, a, :], idf[:], reads=["Bw", "idf", "QT", "QTf"], writes=[bkB.name])
            P.copy("dve", QT[64:96, :, :], bkB[64:96, :].rearrange("p (a t) -> p a t", a=4), reads=[bkB.name], writes=["QT"])
            for a in range(4):
                P.mm(bkG[0:64, 128 + a:129 + a], qkn[:, 256 + a * 64:256 + (a + 1) * 64], onesf[:, 0:1], True, True,
                     reads=["qkn", "onesf"], writes=[bkG.name])
            if blk < 32:
                P.stt(kmT[:, :, blk], bkG[0:64, 128:132], 1.0 / 256.0, kmT[:, :, blk], ALU.mult, ALU.add, reads=[bkG.name, "kmT"], writes=["kmT"])
            ngrp = i // 4 + 1
            items = [(a, g) for a in range(4) for g in range(ngrp)]
            psOb = (bkA, bkD)

            def emit_S(a, g):
                kts = list(range(4 * g, min(4 * g + 4, i + 1)))
                ib = (a * ngrp + g) % 2
                bank = (bkE, bkF)[ib]
                for jj, kt in enumerate(kts):
                    P.mm(bank[:, jj * 128:(jj + 1) * 128], KT[0:100, a, kt * 128:(kt + 1) * 128], QT[0:100, a, :],
                         True, kt != i, reads=["K%d" % kt, "KTaug", "QT"], writes=[bank.name])
                    if kt == i:
                        P.mm(bank[:, jj * 128:(jj + 1) * 128], idb[:], negb[:], False, True, reads=["idb", "negb"], writes=[bank.name])
                P.act(PT[ib][:, 0:len(kts) * 128], bank[:, 0:len(kts) * 128], AF.Exp, reads=[bank.name], writes=["PT%d" % ib])

            def emit_PV(a, g):
                kts = list(range(4 * g, min(4 * g + 4, i + 1)))
                ib = (a * ngrp + g) % 2
                po = psOb[a % 2]
                for jj, kt in enumerate(kts):
                    P.mm(po[:, 0:65], PT[ib][:, jj * 128:(jj + 1) * 128], Vm[:, kt, a, :], kt == 0, kt == i,
                         reads=["PT%d" % ib, "V%d" % kt, "Vones"], writes=[po.name])
                if g == ngrp - 1:
                    P.op("dve", lambda: nc.vector.reciprocal(out=small[:, 12 + a:13 + a], in_=po[:, 64:65]), reads=[po.name], writes=["rdb%d" % a])
                    P.ts(ym[:, 256 + a * 64:256 + (a + 1) * 64], po[:, 0:64], small[:, 12 + a:13 + a], None, ALU.mult, None,
                         reads=[po.name, "rdb%d" % a], writes=[yk])

            emit_S(*items[0])
            for ix in range(len(items)):
                if ix + 1 < len(items):
                    emit_S(*items[ix + 1])
                emit_PV(*items[ix])
            P.dma("sp", over["ytile"](i) if "ytile" in over else y_d[i * 128:(i + 1) * 128, :], ym[:], reads=[yk], writes=["yout"])
        for jx in range(len(P.dsem) if standalone else 0):
            if P.dcnt[jx] > 0:
                P._wait("sp", ("D%d" % jx, P.dsem[jx], 16 * P.dcnt[jx]))
        print("mix0 ninst", P.ninst, "nwait", P.nwait)
    return nc


def prep_mix0(inp, layer, x_b, c_b, hh, S):
    e = layer // 2
    w = inp["ev_w_in"][e]
    mh = [2 * hh, 2 * hh + 1]; bh = [4 * hh + a for a in range(4)]
    cols = []
    for base in (0, 512):
        for m in mh:
            cols.append(w[:, base + m * 128:base + (m + 1) * 128])
    for base in (1024, 1536):
        for m in mh:
            cols.append(w[:, base + m * 128:base + (m + 1) * 128])
    for base in (2056, 2568):
        for a in bh:
            cols.append(w[:, base + a * 64:base + (a + 1) * 64])
    for a in bh:
        cols.append(w[:, 3080 + a * 64:3080 + (a + 1) * 64])
    cols.append(w[:, 2048 + mh[0]:2048 + mh[0] + 2])
    cols.append(w[:, 2052 + mh[0]:2052 + mh[0] + 2])
    win = np.ascontiguousarray(np.concatenate(cols, axis=1))
    cw = inp["ev_conv_w"][e]
    convw = np.zeros((128, 4, 4), np.float32)
    for ai, (base, m) in enumerate([(0, mh[0]), (0, mh[1]), (512, mh[0]), (512, mh[1])]):
        convw[:, ai, :] = cw[:, base + m * 128:base + (m + 1) * 128].T
    gb = np.concatenate([inp["ev_igate_b"][e][mh[0]:mh[0] + 2], inp["ev_fgate_b"][e][mh[0]:mh[0] + 2]])[None, :]
    mg = inp["ev_mnorm_g"][e][mh[0]:mh[0] + 2].reshape(1, 256)
    qn = inp["ev_qn_g"][e]; kn = inp["ev_kn_g"][e]
    gqk = np.concatenate([qn] * 4 + [kn] * 4)[None, :]
    ident, negm, ka = mix1_consts(S)
    sl = slopes8()[4 * hh:4 * hh + 4]
    qab, qad = alibi_q_rows(sl)
    li = np.arange(128)
    trile = (li[:, None] <= li[None, :]).astype(np.float32)
    sgt = (li[:, None] > li[None, :]).astype(np.float32)
    t = np.arange(S)
    kblk = (np.arange(32)[:, None] == (t // 256)[None, :]).astype(np.float32)
    nb = np.arange(32)
    fut = np.stack([np.where(nb >= b, NEG, 0.0) for b in range(33)]).astype(np.float32)
    own = np.stack([np.where(nb == b, 0.0, NEG) for b in range(33)]).astype(np.float32)
    f = np.ascontiguousarray
    return {
        "x": f(x_b), "c": f(c_b.reshape(8, 128).T), "adaw": f(inp["ada_w"][layer][:, 0:2048]), "adab": f(inp["ada_b"][layer][None, 0:2048]),
        "ng": f(inp["norm_mix_g"][layer][None, :]), "win": win, "convw": f(convw.reshape(128, 16)), "gb": f(gb.astype(np.float32)),
        "mg": f(mg), "gqk": f(gqk.astype(np.float32)), "ident": ident, "negm": negm, "posm": f(-negm), "trile": trile, "sgt": sgt,
        "qab": qab, "qad": qad, "ka": ka, "kblk": kblk, "fut": fut, "own": own,
    }


PAIRS = [[0, 1], [2, 3], [4, 5], [6, 7]]


def build_fused(S):
    NTOK = S // 2
    NCH = max(1, (S * 512 * 4) // (2 << 20))
    RY = S // NCH
    RX = NTOK // NCH
    nc = bass.Bass("TRN2", target_bir_lowering=False)
    dr = lambda n, shp: nc.dram_tensor(n, shp, F32).ap()
    y0c = [dr("y0c%d" % j, [RY, 512]) for j in range(NCH)]; yg0c = [dr("yg0c%d" % j, [2 * RY, 512]) for j in range(NCH)]
    y1c = [dr("y1c%d" % j, [RY, 512]) for j in range(NCH)]; yg1c = [dr("yg1c%d" % j, [2 * RY, 512]) for j in range(NCH)]
    x1c = [dr("x1c%d" % j, [RX, D]) for j in range(NCH)]; x1gc = [dr("x1gc%d" % j, [2 * RX, D]) for j in range(NCH)]

    def ytile(ch):
        return lambda i: ch[(i * 128) // RY][(i * 128) % RY:(i * 128) % RY + 128, :]

    def ygtile(ch):
        def f(r, q, r0):
            t = q * NTOK + r0
            return ch[t // RY][r * RY + t % RY:r * RY + t % RY + 128, :]
        return f

    def x1tile(r0):
        return x1c[r0 // RX][r0 % RX:r0 % RX + 128, :]

    def x1gtile(i):
        t = i * 128
        rank, loc = t // NTOK, t % NTOK
        return x1gc[loc // RX][rank * RX + loc % RX:rank * RX + loc % RX + 128, :]

    with ExitStack() as sems:
        Ps = [Prog(nc, None, 16, p, sems) for p in ("a_", "b_", "c_", "d_")]
        ccs = [sems.enter_context(nc.semaphore("cc%d" % i)) for i in range(3)]

        def exchange(Pprev, Pnext, srcs, dsts, cs):
            toks = Pprev.all_tokens()
            Pnext.wait_all(toks, engines=["pool"])
            for a_, d_ in zip(srcs, dsts):
                nc.gpsimd.collective_compute("AllGather", ALU.bypass, replica_groups=PAIRS, ins=[a_.opt()], outs=[d_.opt()]).then_inc(cs)
            Pnext.wait_all(toks + [("CC", cs, len(srcs))], engines=["pe", "act", "dve", "sp", "pool"])

        build_mix0(S, nc, Ps[0], "a_", over={"ytile": ytile(y0c)})
        exchange(Ps[0], Ps[1], y0c, yg0c, ccs[0])
        build_ffn(NTOK, nc, Ps[1], "b_", over={"yg": ygtile(yg0c), "otile": x1tile})
        exchange(Ps[1], Ps[2], x1c, x1gc, ccs[1])
        build_mix1(S, nc, Ps[2], "c_", over={"xtile": x1gtile, "ytile": ytile(y1c)})
        exchange(Ps[2], Ps[3], y1c, yg1c, ccs[2])
        build_ffn(NTOK, nc, Ps[3], "d_", over={"xtile": x1tile, "yg": ygtile(yg1c)})
        Ps[3].wait_all(Ps[3].all_tokens(), engines=["sp"])
        print("fused ninst", sum(p.ninst for p in Ps), "nwait", sum(p.nwait for p in Ps))
    return nc


_CACHE = {}


def _get(name, fn, *a):
    key = (name,) + a
    if key not in _CACHE:
        _CACHE[key] = fn(*a)
    return _CACHE[key]


def kernel(**inputs):
    inp = {k: np.asarray(v) for k, v in inputs.items()}
    x = inp["x"].astype(np.float32, copy=False)
    c = inp["c"]
    B, S, _ = x.shape
    NTOK = S // 2
    nc = _get("fused", build_fused, S)
    wo0 = inp["ev_w_out"][0]
    wo0p = np.concatenate([wo0[0:256], wo0[512:768], wo0[256:512], wo0[768:1024]], axis=0)
    maps = []
    for k in range(8):
        b, hh = k // 2, k % 2
        hsel = np.zeros((128, 2), np.float32); hsel[:, hh] = 1.0
        m = {}
        for pfx, d in (("a_", prep_mix0(inp, 0, x[b], c[b], hh, S)),
                       ("b_", prep_ffn(inp, 0, x[b, hh * NTOK:(hh + 1) * NTOK], None, c[b], wo0p)),
                       ("c_", prep_mix1(inp, 1, None, c[b], hh, S)),
                       ("d_", prep_ffn(inp, 1, None, None, c[b], inp["od_w_out"][0]))):
            for kk, v in d.items():
                if v is not None:
                    m[pfx + kk] = v
        m["b_hsel"] = hsel; m["d_hsel"] = hsel
        maps.append(m)
    res = run_bass_kernel_spmd(nc, maps, core_ids=list(range(8)))
    out = np.concatenate([res.results[k]["o"] for k in range(8)], axis=0).reshape(B, S, D)
    return out.astype(np.float32)
```
